# Optimizing a Trainium2 kernel written in Bass

```python
import math
import jax
import jax.numpy as jnp
from jax import lax
import numpy as np

D_MODEL = 1024
BATCH = 8
SEQ = 2048
DEPTH = 4

CTX_LEN = 256
GRID_W = 64

N_MOD = 6
EPS = 1e-6

N_HEADS = 8
N_KV_HEADS = 2
HEAD_DIM = 64
Q_REP = N_HEADS // N_KV_HEADS
ATTN_W = N_HEADS * HEAD_DIM
KV_W = N_KV_HEADS * HEAD_DIM
WINDOW = 128
ATTN_BLOCK = 128
ATTN_HALO = WINDOW // ATTN_BLOCK
ATTN_SCALE = HEAD_DIM ** -0.5
ROPE_BASE = 10000.0
ROPE_AXIS_DIM = HEAD_DIM // 2

SSM_W = D_MODEL // 4
SSM_GROUP = 16
SSM_GROUPS = SSM_W // SSM_GROUP
SSM_STATE = 64
DT_MIN = 1e-3
DT_MAX = 1e-1

POOL_W = D_MODEL // 4
POOL_WINDOWS = (2, 4, 8, 16)
POOL_GROUP = POOL_W // len(POOL_WINDOWS)

Q_END = ATTN_W
K_END = Q_END + KV_W
V_END = K_END + KV_W
U_END = V_END + SSM_W
P_END = U_END + POOL_W
IN_W = P_END
MIX_W = ATTN_W + SSM_W + POOL_W

N_EXPERTS = 32
TOP_K = 4
D_FF_EXPERT = D_MODEL
SWIGLU_LIMIT = 7.0
SWIGLU_ALPHA = 1.702
MOE_BLOCK = 128

kernel_name = 'hybrid_diffusion_trunk'


def _rmsnorm(x, w):
    xf = x.astype(jnp.float32)
    y = xf * lax.rsqrt(jnp.mean(xf * xf, axis=-1, keepdims=True) + EPS)
    return (y * w.astype(jnp.float32)).astype(x.dtype)


def _modulate(h, shift, scale):
    return h * (1 + scale) + shift


def _cols(p, lo, a, b):
    return p[..., a - lo:b - lo]


def _axial_rope_tables(length):
    rows = length // GRID_W
    row = jnp.repeat(jnp.arange(rows), GRID_W).astype(jnp.float32)
    col = jnp.tile(jnp.arange(GRID_W), rows).astype(jnp.float32)
    inv = ROPE_BASE ** (-jnp.arange(0, ROPE_AXIS_DIM, 2, dtype=jnp.float32) / ROPE_AXIS_DIM)
    ang_r = row[:, None] * inv
    ang_c = col[:, None] * inv
    ang = jnp.concatenate([ang_r, ang_r, ang_c, ang_c], axis=-1)
    return jnp.cos(ang), jnp.sin(ang)


def _apply_rope(t, cos, sin):
    tf = t.astype(jnp.float32)
    s = tf.reshape(tf.shape[:-1] + (2, 2, ROPE_AXIS_DIM // 2))
    rot = jnp.stack([-s[..., 1, :], s[..., 0, :]], axis=-2).reshape(tf.shape)
    return (tf * cos[None, :, None, :] + rot * sin[None, :, None, :]).astype(t.dtype)


def _window_attention(q, k, v, kc, vc, sink):
    B, L = q.shape[:2]
    Lc = kc.shape[1]
    nb = L // ATTN_BLOCK
    span = (2 * ATTN_HALO + 1) * ATTN_BLOCK
    qb = q.reshape(B, nb, ATTN_BLOCK, N_KV_HEADS, Q_REP, HEAD_DIM)
    pad = ((0, 0), (ATTN_HALO * ATTN_BLOCK, ATTN_HALO * ATTN_BLOCK), (0, 0), (0, 0))
    kpb = jnp.pad(k, pad).reshape(B, nb + 2 * ATTN_HALO, ATTN_BLOCK, N_KV_HEADS, HEAD_DIM)
    vpb = jnp.pad(v, pad).reshape(B, nb + 2 * ATTN_HALO, ATTN_BLOCK, N_KV_HEADS, HEAD_DIM)
    kband = jnp.concatenate([kpb[:, j:j + nb] for j in range(2 * ATTN_HALO + 1)], axis=2)
    vband = jnp.concatenate([vpb[:, j:j + nb] for j in range(2 * ATTN_HALO + 1)], axis=2)
    qpos = jnp.arange(L).reshape(nb, ATTN_BLOCK)
    kpos = (jnp.arange(nb)[:, None] - ATTN_HALO) * ATTN_BLOCK + jnp.arange(span)[None, :]
    valid = ((jnp.abs(qpos[:, :, None] - kpos[:, None, :]) <= WINDOW)
             & (kpos[:, None, :] >= 0) & (kpos[:, None, :] < L))
    s_loc = jnp.einsum('bnqgrd,bnkgd->bngrqk', qb, kband, preferred_element_type=jnp.float32) * ATTN_SCALE
    s_loc = jnp.where(valid[None, :, None, None], s_loc, -jnp.inf)
    s_ctx = jnp.einsum('bnqgrd,bcgd->bngrqc', qb, kc, preferred_element_type=jnp.float32) * ATTN_SCALE
    s_sink = jnp.broadcast_to(sink.astype(jnp.float32).reshape(1, 1, N_KV_HEADS, Q_REP, 1, 1),
                              s_ctx.shape[:-1] + (1,))
    p = jax.nn.softmax(jnp.concatenate([s_loc, s_ctx, s_sink], axis=-1), axis=-1)
    o = (jnp.einsum('bngrqk,bnkgd->bnqgrd', p[..., :span], vband.astype(jnp.float32))
         + jnp.einsum('bngrqc,bcgd->bnqgrd', p[..., span:span + Lc], vc.astype(jnp.float32)))
    return o.reshape(B, L, ATTN_W).astype(q.dtype)


def _ctx_attention(qc, kc, vc, sink):
    B, Lc = qc.shape[:2]
    qg = qc.reshape(B, Lc, N_KV_HEADS, Q_REP, HEAD_DIM)
    s = jnp.einsum('bqgrd,bkgd->bgrqk', qg, kc, preferred_element_type=jnp.float32) * ATTN_SCALE
    s_sink = jnp.broadcast_to(sink.astype(jnp.float32).reshape(1, N_KV_HEADS, Q_REP, 1, 1), s.shape[:-1] + (1,))
    p = jax.nn.softmax(jnp.concatenate([s, s_sink], axis=-1), axis=-1)[..., :Lc]
    o = jnp.einsum('bgrqk,bkgd->bqgrd', p, vc.astype(jnp.float32))
    return o.reshape(B, Lc, ATTN_W).astype(qc.dtype)


def _s5_discretize(a_re, a_im, log_dt, b_re, b_im):
    a_re = a_re.astype(jnp.float32)
    a_im = a_im.astype(jnp.float32)
    dt = jnp.exp(log_dt.astype(jnp.float32))[:, None]
    mag = jnp.exp(a_re * dt)
    ar = mag * jnp.cos(a_im * dt)
    ai = mag * jnp.sin(a_im * dt)
    den = a_re * a_re + a_im * a_im
    qr = ((ar - 1) * a_re + ai * a_im) / den
    qi = (ai * a_re - (ar - 1) * a_im) / den
    br = b_re.astype(jnp.float32)
    bi = b_im.astype(jnp.float32)
    bbr = qr[..., None] * br - qi[..., None] * bi
    bbi = qr[..., None] * bi + qi[..., None] * br
    return ar, ai, bbr, bbi


def _complex_affine_combine(e1, e2):
    a1r, a1i, b1r, b1i = e1
    a2r, a2i, b2r, b2i = e2
    return (a2r * a1r - a2i * a1i,
            a2r * a1i + a2i * a1r,
            a2r * b1r - a2i * b1i + b2r,
            a2r * b1i + a2i * b1r + b2i)


def _s5_scan(u, disc, s0, reverse):
    ar, ai, bbr, bbi = disc
    xr = jnp.einsum('blgp,gnp->blgn', u, bbr)
    xi = jnp.einsum('blgp,gnp->blgn', u, bbi)
    if s0 is not None:
        s0r, s0i = s0
        edge = -1 if reverse else 0
        xr = xr.at[:, edge].add(ar * s0r - ai * s0i)
        xi = xi.at[:, edge].add(ar * s0i + ai * s0r)
    a_r = jnp.broadcast_to(ar, xr.shape)
    a_i = jnp.broadcast_to(ai, xi.shape)
    _, _, sr, si = lax.associative_scan(_complex_affine_combine, (a_r, a_i, xr, xi), reverse=reverse, axis=1)
    return sr, si


def _s5_readout(s, c_re, c_im):
    sr, si = s
    return (jnp.einsum('blgn,gpn->blgp', sr, c_re.astype(jnp.float32))
            - jnp.einsum('blgn,gpn->blgp', si, c_im.astype(jnp.float32)))


def _s5_output(s_fwd, s_bwd, u, c_re, c_im, d, glu_w, glu_b):
    Bu, Lu = u.shape[:2]
    y = _s5_readout(s_fwd, c_re[0], c_im[0]) + _s5_readout(s_bwd, c_re[1], c_im[1])
    y = y.reshape(Bu, Lu, SSM_W) + d.astype(jnp.float32) * u.reshape(Bu, Lu, SSM_W)
    g = jax.nn.gelu(y)
    return g * jax.nn.sigmoid(g @ glu_w.astype(jnp.float32) + glu_b.astype(jnp.float32))


def _multiscale_pool(p, pool_w, pool_scale):
    Bp, Lp = p.shape[:2]
    pf = p.astype(jnp.float32)
    cs = jnp.pad(jnp.cumsum(pf, axis=1), ((0, 0), (1, 0), (0, 0)))
    t = jnp.arange(Lp)
    diffs = []
    for g, w in enumerate(POOL_WINDOWS):
        lo = jnp.clip(t - w // 2, 0, Lp)
        hi = jnp.clip(t + w // 2, 0, Lp)
        sl = slice(g * POOL_GROUP, (g + 1) * POOL_GROUP)
        cg = cs[..., sl]
        mean = (cg[:, hi] - cg[:, lo]) / (hi - lo).astype(jnp.float32)[None, :, None]
        diffs.append(mean - pf[..., sl])
    dlt = jnp.stack(diffs, axis=2)
    y = jnp.einsum('blgc,gcd->blgd', dlt, pool_w.astype(jnp.float32)).reshape(Bp, Lp, POOL_W)
    return (y * pool_scale.astype(jnp.float32)).astype(p.dtype)


def _merge_groups(attn, ssm, pool, out_norm_w):
    return jnp.concatenate([_rmsnorm(attn, out_norm_w[:ATTN_W]),
                            _rmsnorm(ssm, out_norm_w[ATTN_W:]),
                            pool], axis=-1)


def _moe(h, router_w, router_b, w_gu, b_gu, w_down, b_down):
    N, D = h.shape
    logits = (h @ router_w + router_b).astype(jnp.float32)
    top_v, top_i = lax.top_k(logits, TOP_K)
    gates = jax.nn.softmax(top_v, axis=-1)
    flat_e = top_i.reshape(-1)
    order = jnp.argsort(flat_e)
    sorted_e = flat_e[order]
    sorted_tok = order // TOP_K
    counts = jnp.bincount(flat_e, length=N_EXPERTS)
    start = jnp.cumsum(counts) - counts
    padded = (counts + MOE_BLOCK - 1) // MOE_BLOCK * MOE_BLOCK
    pend = jnp.cumsum(padded)
    pstart = pend - padded
    n_assign = N * TOP_K
    dest_sorted = pstart[sorted_e] + jnp.arange(n_assign) - start[sorted_e]
    n_blocks = -(-n_assign // MOE_BLOCK) + N_EXPERTS
    rows = n_blocks * MOE_BLOCK
    row_tok = jnp.full((rows,), N, jnp.int32).at[dest_sorted].set(sorted_tok.astype(jnp.int32))
    block_e = jnp.minimum(jnp.searchsorted(pend, jnp.arange(n_blocks) * MOE_BLOCK, side='right'), N_EXPERTS - 1)
    h_pad = jnp.concatenate([h, jnp.zeros((1, D), h.dtype)], axis=0)
    xb = h_pad[row_tok].reshape(n_blocks, MOE_BLOCK, D)

    def expert_block(args):
        xblk, e = args
        gu = xblk @ w_gu[e] + b_gu[e]
        gate, up = jnp.split(gu, 2, axis=-1)
        gate = jnp.minimum(gate, SWIGLU_LIMIT)
        up = jnp.clip(up, -SWIGLU_LIMIT, SWIGLU_LIMIT)
        act = (up + 1) * (gate * jax.nn.sigmoid(SWIGLU_ALPHA * gate))
        return act @ w_down[e] + b_down[e]

    yb = lax.map(expert_block, (xb, block_e)).reshape(rows, D)
    dest = jnp.zeros((n_assign,), dest_sorted.dtype).at[order].set(dest_sorted)
    y = yb[dest].reshape(N, TOP_K, D)
    return jnp.einsum('nkd,nk->nd', y, gates.astype(y.dtype))


def setup_inputs(seed: int = 0) -> dict:
    key = jax.random.key(seed)
    ks = iter(jax.random.split(key, 40))
    f32 = jnp.float32
    D = D_MODEL

    def nrm(shape, scale):
        return jax.random.normal(next(ks), shape, f32) * scale

    x = nrm((BATCH, SEQ, D), 1.0)
    c = nrm((BATCH, D), 1.0)
    ctx = nrm((BATCH, CTX_LEN, D), 1.0)
    c_ctx = nrm((D,), 1.0)
    w_mod = nrm((DEPTH, D, N_MOD * D), 0.5 * D ** -0.5)
    b_mod = nrm((DEPTH, N_MOD * D), 0.02)
    norm1_w = 1.0 + nrm((DEPTH, D), 0.02)
    norm2_w = 1.0 + nrm((DEPTH, D), 0.02)
    w_in = nrm((DEPTH, D, IN_W), D ** -0.5)
    q_norm_w = 1.0 + nrm((DEPTH, HEAD_DIM), 0.02)
    k_norm_w = 1.0 + nrm((DEPTH, HEAD_DIM), 0.02)
    attn_sink = nrm((DEPTH, N_HEADS), 0.5)
    n_idx = jnp.arange(SSM_STATE, dtype=f32)
    ssm_a_re = -0.5 + nrm((DEPTH, 2, SSM_GROUPS, SSM_STATE), 0.01)
    ssm_a_im = math.pi * n_idx + nrm((DEPTH, 2, SSM_GROUPS, SSM_STATE), 0.01)
    ssm_log_dt = jax.random.uniform(next(ks), (DEPTH, 2, SSM_GROUPS), f32, math.log(DT_MIN), math.log(DT_MAX))
    ssm_b_re = nrm((DEPTH, 2, SSM_GROUPS, SSM_STATE, SSM_GROUP), (2 * SSM_GROUP) ** -0.5)
    ssm_b_im = nrm((DEPTH, 2, SSM_GROUPS, SSM_STATE, SSM_GROUP), (2 * SSM_GROUP) ** -0.5)
    ssm_c_re = nrm((DEPTH, 2, SSM_GROUPS, SSM_GROUP, SSM_STATE), SSM_STATE ** -0.5)
    ssm_c_im = nrm((DEPTH, 2, SSM_GROUPS, SSM_GROUP, SSM_STATE), SSM_STATE ** -0.5)
    ssm_d = nrm((DEPTH, SSM_W), 1.0)
    glu_w = nrm((DEPTH, SSM_W, SSM_W), SSM_W ** -0.5)
    glu_b = nrm((DEPTH, SSM_W), 0.02)
    pool_w = nrm((DEPTH, len(POOL_WINDOWS), POOL_GROUP, POOL_GROUP), POOL_GROUP ** -0.5)
    pool_scale = 1.0 + nrm((DEPTH, POOL_W), 0.02)
    out_norm_w = 1.0 + nrm((DEPTH, ATTN_W + SSM_W), 0.02)
    w_out = nrm((DEPTH, MIX_W, D), MIX_W ** -0.5)
    router_w = nrm((DEPTH, D, N_EXPERTS), D ** -0.5)
    router_b = nrm((DEPTH, N_EXPERTS), 0.01)
    exp_w_gu = nrm((DEPTH, N_EXPERTS, D, 2 * D_FF_EXPERT), D ** -0.5)
    exp_b_gu = nrm((DEPTH, N_EXPERTS, 2 * D_FF_EXPERT), 0.02)
    exp_w_down = nrm((DEPTH, N_EXPERTS, D_FF_EXPERT, D), D_FF_EXPERT ** -0.5)
    exp_b_down = nrm((DEPTH, N_EXPERTS, D), 0.02)
    return {'x': x, 'c': c, 'ctx': ctx, 'c_ctx': c_ctx, 'w_mod': w_mod, 'b_mod': b_mod,
            'norm1_w': norm1_w, 'norm2_w': norm2_w, 'w_in': w_in, 'q_norm_w': q_norm_w,
            'k_norm_w': k_norm_w, 'attn_sink': attn_sink, 'ssm_a_re': ssm_a_re, 'ssm_a_im': ssm_a_im,
            'ssm_log_dt': ssm_log_dt, 'ssm_b_re': ssm_b_re, 'ssm_b_im': ssm_b_im, 'ssm_c_re': ssm_c_re,
            'ssm_c_im': ssm_c_im, 'ssm_d': ssm_d, 'glu_w': glu_w, 'glu_b': glu_b, 'pool_w': pool_w,
            'pool_scale': pool_scale, 'out_norm_w': out_norm_w, 'w_out': w_out, 'router_w': router_w,
            'router_b': router_b, 'exp_w_gu': exp_w_gu, 'exp_b_gu': exp_b_gu, 'exp_w_down': exp_w_down,
            'exp_b_down': exp_b_down}


def reference(x, c, ctx, c_ctx, w_mod, b_mod, norm1_w, norm2_w, w_in, q_norm_w, k_norm_w, attn_sink,
              ssm_a_re, ssm_a_im, ssm_log_dt, ssm_b_re, ssm_b_im, ssm_c_re, ssm_c_im, ssm_d, glu_w, glu_b,
              pool_w, pool_scale, out_norm_w, w_out, router_w, router_b, exp_w_gu, exp_b_gu, exp_w_down,
              exp_b_down):
    B, L, D = x.shape
    Lc = ctx.shape[1]
    cos, sin = _axial_rope_tables(L)
    silu_c = jax.nn.silu(c)
    silu_cc = jax.nn.silu(c_ctx)
    for l in range(DEPTH):
        last = l == DEPTH - 1
        mod = jnp.split((silu_c @ w_mod[l] + b_mod[l])[:, None, :], N_MOD, axis=-1)
        n_cm = 2 if last else N_MOD
        mod_c = jnp.split(silu_cc @ w_mod[l][:, :n_cm * D] + b_mod[l][:n_cm * D], n_cm)

        hx = _modulate(_rmsnorm(x, norm1_w[l]), mod[0], mod[1])
        hc = _modulate(_rmsnorm(ctx, norm1_w[l]), mod_c[0], mod_c[1])
        px = hx @ w_in[l]
        c_lo, c_hi = (Q_END, U_END) if last else (0, P_END)
        pc = hc @ w_in[l][:, c_lo:c_hi]

        q = _apply_rope(_rmsnorm(px[..., :Q_END].reshape(B, L, N_HEADS, HEAD_DIM), q_norm_w[l]), cos, sin)
        k = _apply_rope(_rmsnorm(px[..., Q_END:K_END].reshape(B, L, N_KV_HEADS, HEAD_DIM), k_norm_w[l]), cos, sin)
        v = px[..., K_END:V_END].reshape(B, L, N_KV_HEADS, HEAD_DIM)
        kc = _rmsnorm(_cols(pc, c_lo, Q_END, K_END).reshape(B, Lc, N_KV_HEADS, HEAD_DIM), k_norm_w[l])
        vc = _cols(pc, c_lo, K_END, V_END).reshape(B, Lc, N_KV_HEADS, HEAD_DIM)
        attn_x = _window_attention(q, k, v, kc, vc, attn_sink[l])

        disc_f = _s5_discretize(ssm_a_re[l, 0], ssm_a_im[l, 0], ssm_log_dt[l, 0], ssm_b_re[l, 0], ssm_b_im[l, 0])
        disc_b = _s5_discretize(ssm_a_re[l, 1], ssm_a_im[l, 1], ssm_log_dt[l, 1], ssm_b_re[l, 1], ssm_b_im[l, 1])
        u_x = px[..., V_END:U_END].reshape(B, L, SSM_GROUPS, SSM_GROUP).astype(jnp.float32)
        u_c = _cols(pc, c_lo, V_END, U_END).reshape(B, Lc, SSM_GROUPS, SSM_GROUP).astype(jnp.float32)
        sc_f = _s5_scan(u_c, disc_f, None, reverse=False)
        sc_b = _s5_scan(u_c, disc_b, None, reverse=True)
        sx_f = _s5_scan(u_x, disc_f, (sc_f[0][:, -1], sc_f[1][:, -1]), reverse=False)
        sx_b = _s5_scan(u_x, disc_b, (sc_b[0][:, 0], sc_b[1][:, 0]), reverse=True)
        ssm_x = _s5_output(sx_f, sx_b, u_x, ssm_c_re[l], ssm_c_im[l], ssm_d[l], glu_w[l], glu_b[l]).astype(x.dtype)

        pool_x = _multiscale_pool(px[..., U_END:P_END], pool_w[l], pool_scale[l])

        x = x + mod[2] * (_merge_groups(attn_x, ssm_x, pool_x, out_norm_w[l]) @ w_out[l])
        if not last:
            qc = _rmsnorm(_cols(pc, c_lo, 0, Q_END).reshape(B, Lc, N_HEADS, HEAD_DIM), q_norm_w[l])
            attn_c = _ctx_attention(qc, kc, vc, attn_sink[l])
            ssm_c = _s5_output(sc_f, sc_b, u_c, ssm_c_re[l], ssm_c_im[l], ssm_d[l], glu_w[l], glu_b[l]).astype(ctx.dtype)
            pool_c = _multiscale_pool(_cols(pc, c_lo, U_END, P_END), pool_w[l], pool_scale[l])
            ctx = ctx + mod_c[2] * (_merge_groups(attn_c, ssm_c, pool_c, out_norm_w[l]) @ w_out[l])

        h2x = _modulate(_rmsnorm(x, norm2_w[l]), mod[3], mod[4]).reshape(B * L, D)
        if last:
            y2 = _moe(h2x, router_w[l], router_b[l], exp_w_gu[l], exp_b_gu[l], exp_w_down[l], exp_b_down[l])
            x = x + mod[5] * y2.reshape(B, L, D)
        else:
            h2c = _modulate(_rmsnorm(ctx, norm2_w[l]), mod_c[3], mod_c[4]).reshape(B * Lc, D)
            y2 = _moe(jnp.concatenate([h2x, h2c], axis=0), router_w[l], router_b[l], exp_w_gu[l],
                      exp_b_gu[l], exp_w_down[l], exp_b_down[l])
            x = x + mod[5] * y2[:B * L].reshape(B, L, D)
            ctx = ctx + mod_c[5] * y2[B * L:].reshape(B, Lc, D)
    return x
```

```python
from contextlib import ExitStack
import math
import numpy as np
import ml_dtypes
import concourse.bass as bass
import concourse.mybir as mybir
from concourse.bass_utils import run_bass_kernel_spmd

F32 = mybir.dt.float32
BF16 = mybir.dt.bfloat16
AF = mybir.ActivationFunctionType
ALU = mybir.AluOpType
AX = mybir.AxisListType

D = 1024
L = 2048
LC = 256
T = L + LC
NT = T // 128
NXT = L // 128
DEPTH = 4
NE = 32
EPS = 1e-6
ATT_SCALE = 64 ** -0.5
KSW = 1.0 / 1.702
ENGS = ("pe", "act", "dve", "pool", "sp")
TSL = [(0, 512), (512, 512), (1024, 512), (1536, 512), (2048, 256)]
POOLW = (2, 4, 8, 16)


class _Probe:
    def __init__(self):
        self.outs = []

    def __getattr__(self, name):
        def f(*a, **kw):
            o = kw.get("out", None)
            if o is None and a:
                o = a[0]
            self.outs.append(o)
            if kw.get("accum_out", None) is not None:
                self.outs.append(kw["accum_out"])
            return self
        return f

    def small(self):
        for o in self.outs:
            try:
                n = int(np.prod(o.shape[1:]))
            except Exception:
                n = 1 << 20
            if n < 512:
                return True
        return False


class Prog:
    def __init__(self, nc):
        self.nc = nc
        self.q = {e: [] for e in ENGS}
        self.cnt = {e: 0 for e in ENGS}
        self.waited = {e: {} for e in ENGS}
        self.sems = {}
        self.dcnt = {}
        self.last_sig = {}
        self.epoch = 0
        self.stack = ExitStack()
        self.sb_off = 16512
        self.sb_n = 0

    def sb(self, shape, dtype, name=None):
        nbytes = int(np.prod(shape[1:])) * mybir.dt.size(dtype)
        nbytes = (nbytes + 31) // 32 * 32
        off = self.sb_off
        self.sb_off += nbytes
        assert self.sb_off <= 229376, ("SBUF overflow", self.sb_off, name)
        self.sb_n += 1
        return self.nc.alloc_sbuf_tensor_at(f"sb{self.sb_n}_{name or ''}", list(shape), dtype, offset=off)

    def mark(self):
        return self.sb_off

    def release(self, m):
        self.sb_off = m

    def sem(self, name):
        if name not in self.sems:
            self.sems[name] = self.stack.enter_context(self.nc.semaphore(name))
        return self.sems[name]

    def emit(self, eng, fn, sig=False):
        self.fence_last[eng] = False
        ent = [fn, None]
        self.q[eng].append(ent)
        if eng in ("act", "dve", "pool"):
            pr = _Probe()
            fn(pr)
            if pr.small():
                ev = self._sig(eng, ent)
                sm = self.sem(ev[3])
                self.q[eng].append([lambda e, sm=sm, v=ev[2]: e.wait_ge(sm, v), None, "wait"])
                return ev
        if sig:
            return self._sig(eng, ent)
        return None

    def _sig(self, eng, ent):
        self.cnt[eng] += 1
        nm = "p_%s_%d" % (eng, self.epoch)
        ent[1] = self.sem(nm)
        self.last_sig[eng] = ("e", eng, self.cnt[eng], nm)
        return self.last_sig[eng]

    def init_fences(self):
        self.fz = {e: self.sb([128, 64], F32, "fz_" + e) for e in ("act", "dve", "pool")}
        self.fence_last = {e: False for e in ENGS}

    def sig_last(self, eng):
        last = None
        for ent in reversed(self.q[eng]):
            if len(ent) == 3:
                continue
            last = ent
            break
        if last is None:
            return None
        if eng in self.fz:
            if self.fence_last[eng] and eng in self.last_sig:
                return self.last_sig[eng]
            fz = self.fz[eng]
            if eng == "act":
                ent = [lambda e: e.copy(out=fz[:, :], in_=fz[:, :]), None]
            else:
                ent = [lambda e: e.tensor_copy(out=fz[:, :], in_=fz[:, :]), None]
            self.q[eng].append(ent)
            self.fence_last[eng] = True
            return self._sig(eng, ent)
        if last[1] is None:
            return self._sig(eng, last)
        if eng in self.last_sig:
            return self.last_sig[eng]
        return None

    def wait(self, eng, ev):
        if ev is None:
            return
        if isinstance(ev, list):
            for x in ev:
                self.wait(eng, x)
            return
        kind, key, val = ev[0], ev[1], ev[2]
        if kind == "e" and key == eng:
            return
        semname = ev[3] if kind == "e" else key
        if self.waited[eng].get(semname, 0) >= val:
            return
        self.waited[eng][semname] = val
        s = self.sem(semname)
        self.q[eng].append([lambda e, s=s, val=val: e.wait_ge(s, val), None, "wait"])

    def dma(self, qeng, out, in_, semname, **kw):
        s = self.sem(semname)
        self.dcnt[semname] = self.dcnt.get(semname, 0) + 16
        self.q[qeng].append([lambda e, out=out, in_=in_, s=s, kw=kw: e.dma_start(out=out, in_=in_, **kw).then_inc(s, 16), None, "dma"])
        return ("d", semname, self.dcnt[semname])

    def dma_k(self, qeng, out, in_, semname, **kw):
        ev = None
        for k in range(out.shape[1]):
            ev = self.dma(qeng, out[:, k, :], in_[:, k, :], semname, **kw)
        return ev

    def new_epoch(self):
        self.barrier()
        self.epoch += 1
        self.cnt = {e: 0 for e in ENGS}
        self.fence_last = {e: False for e in ENGS}
        self.last_sig = {}

    def barrier(self, engs=ENGS):
        evs = [self.sig_last(e) for e in engs]
        for e in engs:
            self.wait(e, evs)

    def pe(self, fn, sig=False):
        return self.emit("pe", fn, sig)

    def act(self, fn, sig=False):
        return self.emit("act", fn, sig)

    def dve(self, fn, sig=False):
        return self.emit("dve", fn, sig)

    def pool(self, fn, sig=False):
        return self.emit("pool", fn, sig)

    def finish(self):
        nc = self.nc
        q = self.q

        def run(e, lst):
            for ent in lst:
                ins = ent[0](e)
                if ent[1] is not None:
                    ins.then_inc(ent[1], 1)

        with nc.Block() as block:
            @block.tensor
            def _(e):
                run(e, q["pe"])

            @block.scalar
            def _(e):
                run(e, q["act"])

            @block.vector
            def _(e):
                run(e, q["dve"])

            @block.gpsimd
            def _(e):
                run(e, q["pool"])

            @block.sync
            def _(e):
                run(e, q["sp"])
        self.stack.close()


class Ring:
    def __init__(self, banks):
        self.banks = banks
        self.free = [None] * len(banks)
        self.i = 0

    def next(self, P, eng="pe"):
        i = self.i
        self.i = (self.i + 1) % len(self.banks)
        P.wait(eng, self.free[i])
        return i, self.banks[i]

    def done(self, i, ev):
        self.free[i] = ev


def bcast_row(ap_row, n=128):
    return ap_row.partition_broadcast(n)[:, 0, :]


def build_program(n_layers=DEPTH, dbg=None, moe_experts=NE, stop=None, we=NE):
    dbg = dbg or {}
    nc = bass.Bass("TRN2", target_bir_lowering=False)
    P = Prog(nc)

    def din(name, shape, dt=F32):
        return nc.dram_tensor(name, list(shape), dt, kind="ExternalInput").ap()

    WD = n_layers
    x_in = din("x", [L, D])
    ctx_in = din("ctx", [LC, D])
    cc_in = din("cc", [2, D])
    w_mod = din("w_mod", [WD, D, 6 * D])
    b_mod = din("b_mod", [WD, 6 * D])
    norm1_w = din("norm1_w", [WD, D])
    norm2_w = din("norm2_w", [WD, D])
    w_in = din("w_in", [WD, D, 1280])
    q_norm_w = din("q_norm_w", [WD, 64])
    k_norm_w = din("k_norm_w", [WD, 64])
    attn_sink = din("attn_sink", [WD, 8])
    ssm_a_re = din("ssm_a_re", [WD, 2, 16, 64])
    ssm_a_im = din("ssm_a_im", [WD, 2, 16, 64])
    ssm_log_dt = din("ssm_log_dt", [WD, 2, 16])
    ssm_b_re = din("ssm_b_re", [WD, 2, 16, 64, 16])
    ssm_b_im = din("ssm_b_im", [WD, 2, 16, 64, 16])
    ssm_c_re = din("ssm_c_re", [WD, 2, 16, 16, 64])
    ssm_c_im = din("ssm_c_im", [WD, 2, 16, 16, 64])
    ssm_d = din("ssm_d", [WD, 256])
    glu_w = din("glu_w", [WD, 256, 256])
    glu_b = din("glu_b", [WD, 256])
    pool_w = din("pool_w", [WD, 4, 64, 64])
    pool_scale = din("pool_scale", [WD, 256])
    out_norm_w = din("out_norm_w", [WD, 768])
    w_out = din("w_out", [WD, D, D])
    router_w = din("router_w", [WD, D, NE])
    router_b = din("router_b", [WD, NE])
    exp_w_gu = din("exp_w_gu", [WD, we, D, 2 * D])
    exp_b_gu = din("exp_b_gu", [WD, we, 2 * D])
    exp_w_down = din("exp_w_down", [WD, we, D, D])
    exp_b_down = din("exp_b_down", [WD, we, D])
    c_ident = din("c_ident", [128, 128])
    c_rope = din("c_rope", [128, NXT, 128])
    c_mask = din("c_mask", [128, 2, 128])
    c_poolfix = din("c_poolfix", [128, 2, 2, 8])
    c_poolinv = din("c_poolinv", [128, 2])
    out = nc.dram_tensor("out", [L, D], F32, kind="ExternalOutput").ap()
    modscr = nc.dram_tensor("modscr", [DEPTH, 2, 6 * D], F32, kind="Internal").ap()
    dbg_out = {k: nc.dram_tensor("dbg_" + k, list(shp), F32, kind="ExternalOutput").ap() for k, shp in dbg.items()}

    psb = [nc.alloc_psum_tensor(f"ps{i}", [128, 512], F32) for i in range(8)]

    def psv(i, dt=F32):
        return psb[i][:, :] if dt == F32 else psb[i][:, :].bitcast(dt)

    P.init_fences()
    X = P.sb([128, NT, D], F32, "X")
    ident_f = P.sb([128, 128], F32, "identf")
    ident_b = P.sb([128, 128], BF16, "identb")
    ones_b = P.sb([128, 128], BF16, "onesb")
    masks = P.sb([128, 2, 128], BF16, "masks")
    poolfix = P.sb([128, 2, 2, 8], F32, "poolfix")
    poolinv = P.sb([128, 2], F32, "poolinv")
    epsc = P.sb([128, 1], F32, "epsc")
    halfpi = P.sb([128, 1], F32, "halfpi")
    base_mark = P.mark()

    def dump(name, src_ap, dst_ap=None):
        if name not in dbg_out:
            return
        P.barrier()
        ev = P.dma("pool", dbg_out[name] if dst_ap is None else dst_ap, src_ap, "dbgsem")
        for en in ENGS:
            P.wait(en, ev)
        P.barrier()

    def finish_prog():
        P.barrier()
        evs_o = [P.dma("sp", out[i * 128:(i + 1) * 128, :], X[:, i, :], "st_out") for i in range(NXT)]
        P.wait("sp", evs_o)
        P.finish()
        return nc

    m0 = P.mark()
    mstage = P.sb([128, 2, 128], F32, "mstage")
    evs = [P.dma("sp", ident_f[:, :], c_ident[:, :], "ld0"),
           P.dma("sp", mstage[:, :, :], c_mask[:, :, :], "ld0"),
           P.dma("sp", poolfix[:, :, :, :], c_poolfix[:, :, :, :], "ld0"),
           P.dma("sp", poolinv[:, :], c_poolinv[:, :], "ld0")]
    for i in range(NXT):
        evs.append(P.dma("sp", X[:, i, :], x_in[i * 128:(i + 1) * 128, :], "ld0"))
    for i in range(2):
        evs.append(P.dma("sp", X[:, NXT + i, :], ctx_in[i * 128:(i + 1) * 128, :], "ld0"))
    P.wait("dve", evs)
    for fe, ft in P.fz.items():
        P.emit(fe if fe != "act" else "dve", lambda e, ft=ft: e.memset(ft[:, :], 0.0))
    P.dve(lambda e: e.tensor_copy(out=ident_b[:, :], in_=ident_f[:, :]))
    P.dve(lambda e: e.tensor_copy(out=masks[:, :, :], in_=mstage[:, :, :]))
    P.dve(lambda e: e.memset(ones_b[:, :], 1.0))
    P.dve(lambda e: e.memset(epsc[:, :], EPS))
    P.dve(lambda e: e.memset(halfpi[:, :], math.pi / 2))
    P.barrier()
    P.release(m0)

    if stop == 'S0':
        return finish_prog()
    m0 = P.mark()
    ccs = P.sb([128, D], F32, "ccs")
    sil = P.sb([128, 8, 128], F32, "sil")
    wst = [P.sb([128, 8, 512], F32, f"wst{i}") for i in range(2)]
    brow = P.sb([2, 6 * D], F32, "brow")
    mrow = P.sb([2, 512], F32, "mrow")
    P.dve(lambda e: e.memset(ccs[:, :], 0.0))
    P.dve(lambda e: e.memset(sil[:, :, :], 0.0))
    evz = P.sig_last("dve")
    P.wait("sp", evz)
    ev = P.dma("sp", ccs[0:2, :], cc_in[:, :], "ld1")
    P.wait("act", ev)
    P.act(lambda e: e.activation(out=ccs[0:2, :], in_=ccs[0:2, :], func=AF.Silu))
    ev_a = P.sig_last("act")
    P.wait("pe", ev_a)
    for k in range(8):
        P.pe(lambda e, k=k: e.transpose(out=psv(k // 4)[:, (k % 4) * 128:(k % 4 + 1) * 128], in_=ccs[:, k * 128:(k + 1) * 128], identity=ident_f[:, :]))
    ev = P.sig_last("pe")
    P.wait("dve", ev)
    for hh in range(2):
        P.dve(lambda e, hh=hh: e.tensor_copy(out=sil[:, 4 * hh:4 * hh + 4, 0:2], in_=psv(hh).rearrange("p (k c) -> p k c", k=4)[:, :, 0:2]))
    ev_sil = P.sig_last("dve")
    P.wait("pe", ev_sil)
    if stop == 'S1a':
        return finish_prog()
    ring = Ring([2, 3])
    wfree = [None, None]
    mrow_free = None
    it = 0
    for l in range(n_layers):
        P.wait("sp", mrow_free)
        evb0 = [P.dma("sp", brow[r:r + 1, :], b_mod[l:l + 1, :], "ld1b") for r in range(2)]
        for ct in range(12):
            s = it % 2
            it += 1
            P.wait("sp", wfree[s])
            evw = P.dma_k("sp", wst[s][:, :, :], w_mod[l, :, ct * 512:(ct + 1) * 512].rearrange("(k p) n -> p k n", p=128), f"ld1w{s}")
            bi, b = ring.next(P)
            P.wait("pe", evw)
            for k in range(8):
                P.pe(lambda e, k=k, s=s, b=b: e.matmul(psv(b)[:, :], lhsT=sil[:, k, :], rhs=wst[s][:, k, :], start=(k == 0), stop=(k == 7)))
            evp = P.sig_last("pe")
            wfree[s] = evp
            P.wait("dve", [evp, mrow_free] + evb0)
            P.dve(lambda e, b=b, ct=ct: e.tensor_tensor(out=mrow[0:2, :], in0=psv(b)[0:2, :], in1=brow[0:2, ct * 512:(ct + 1) * 512], op=ALU.add))
            evd = P.sig_last("dve")
            ring.done(bi, evd)
            P.wait("sp", evd)
            e0 = P.dma("sp", modscr[l, 0:2, ct * 512:(ct + 1) * 512], mrow[0:2, :], "st1")
            P.wait("sp", e0)
            mrow_free = e0
    P.barrier()
    P.release(m0)

    if stop == 'S1':
        return finish_prog()
    def norm_mod_transpose(l, nw, shift_idx, scale_idx, hT, ntiles, router=None):
        m = P.mark()
        A = [P.sb([128, D], F32, "A0"), P.sb([128, D], F32, "A1")]
        S = [P.sb([128, D], F32, "S0"), P.sb([128, D], F32, "S1")]
        nwb = P.sb([128, D], F32, "nwb")
        junk = P.sb([128, D], BF16, "junk")
        hf = [P.sb([128, D], F32, f"hf{i}") for i in range(2)]
        st = P.sb([128, NT, 4], F32, "st")
        evs = [P.dma("sp", nwb[:, :], bcast_row(nw[l:l + 1, :]), "ldn")]
        for r in range(2):
            evs.append(P.dma("sp", A[r][:, :], bcast_row(modscr[l, r:r + 1, scale_idx * D:(scale_idx + 1) * D]), "ldn"))
            evs.append(P.dma("sp", S[r][:, :], bcast_row(modscr[l, r:r + 1, shift_idx * D:(shift_idx + 1) * D]), "ldn"))
        P.wait("dve", evs)
        for r in range(2):
            P.dve(lambda e, r=r: e.scalar_tensor_tensor(out=A[r][:, :], in0=A[r][:, :], scalar=1.0, in1=nwb[:, :], op0=ALU.add, op1=ALU.mult))
        if router is not None:
            rw32, rbb, LG, h32 = router
        ring_t = Ring([0, 2])
        hfree = [None, None]
        lg_ring = Ring([6, 7])
        for i in range(ntiles):
            r = 0 if i < NXT else 1
            s = i % 2
            P.act(lambda e, i=i: e.activation(out=junk[:, :], in_=X[:, i, :], func=AF.Square, accum_out=st[:, i, 0:1]))
            P.act(lambda e, i=i: e.activation(out=st[:, i, 1:2], in_=st[:, i, 0:1], func=AF.Sqrt, bias=epsc[:, 0:1], scale=1.0 / D))
            eva = P.sig_last("act")
            P.wait("dve", [eva, hfree[s]])
            P.dve(lambda e, i=i: e.reciprocal(out=st[:, i, 2:3], in_=st[:, i, 1:2]))
            P.dve(lambda e, i=i, r=r, s=s: e.scalar_tensor_tensor(out=hf[s][:, :], in0=X[:, i, :], scalar=st[:, i, 2:3], in1=A[r][:, :], op0=ALU.mult, op1=ALU.mult))
            P.dve(lambda e, r=r, s=s: e.tensor_tensor(out=hf[s][:, :], in0=hf[s][:, :], in1=S[r][:, :], op=ALU.add))
            evd = P.sig_last("dve")
            bi, b = ring_t.next(P)
            P.wait("pe", evd)
            for k in range(8):
                P.pe(lambda e, k=k, b=b, s=s: e.transpose(out=psv(b + k // 4)[:, (k % 4) * 128:(k % 4 + 1) * 128], in_=hf[s][:, k * 128:(k + 1) * 128], identity=ident_f[:, :]))
            evp = P.sig_last("pe")
            hfree[s] = evp
            P.wait("act", evp)
            for hh in range(2):
                P.act(lambda e, b=b, hh=hh, i=i: e.copy(out=hT[:, 4 * hh:4 * hh + 4, i * 128:(i + 1) * 128],
                                                       in_=psv(b + hh).rearrange("p (k t) -> p k t", k=4)))
            if router is not None:
                P.wait("act", h32_free[0])
                for hh in range(2):
                    P.act(lambda e, b=b, hh=hh: e.copy(out=h32[:, 4 * hh:4 * hh + 4, :], in_=psv(b + hh).rearrange("p (k t) -> p k t", k=4)))
            evc = P.sig_last("act")
            ring_t.done(bi, evc)
            if router is not None:
                li, lb = lg_ring.next(P)
                P.wait("pe", evc)
                for k in range(8):
                    P.pe(lambda e, k=k, lb=lb: e.matmul(psv(lb)[:, 0:NE], lhsT=h32[:, k, :], rhs=rw32[:, k, :], start=(k == 0), stop=(k == 7)))
                evl = P.sig_last("pe")
                h32_free[0] = evl
                P.wait("dve", evl)
                P.dve(lambda e, lb=lb, i=i: e.tensor_tensor(out=LG[:, i, :], in0=psv(lb)[:, 0:NE], in1=rbb[:, :], op=ALU.add))
                lg_ring.done(li, P.sig_last("dve"))
        P.barrier()
        P.release(m)

    h32_free = [None]
    pl_free = [None]
    pool_ev = [None]
    dve_step_ev = [None]

    R1 = (P.mark() + 31) // 32 * 32
    R2 = R1 + 36864
    R3 = R2 + 9216
    PW = 8 + L + 8 + 8 + LC + 8
    R4 = R3 + 2 * PW * 4
    R5 = R4 + 18432 + 18432 + 4704
    for l in range(n_layers):
        last = l == DEPTH - 1
        P.new_epoch()
        P.release(R1)
        hT = P.sb([128, 8, T], BF16, "hT")
        mergedT = hT
        uT = P.sb([128, 2, T], BF16, "uT")
        poolP = P.sb([128, 2, PW], F32, "poolP")
        qT = P.sb([128, 4, T], BF16, "qT")
        kT2 = P.sb([128, 2, 2, T], BF16, "kT2")
        Vaug = P.sb([128, NT, 2, 65], BF16, "Vaug")
        assert P.mark() <= R5, (P.mark(), R5)
        P.release(R4)
        norm_mod_transpose(l, norm1_w, 0, 1, hT, NT)
        if l == 0:
            dump("hT", hT[:, :, :].rearrange("p k t -> p (k t)"))
        if stop == 'L1':
            return finish_prog()
        P.release(R5)
        win = P.sb([128, 8, 768], BF16, "win")
        ropet = [P.sb([128, 128], F32, f"ropet{i}") for i in range(2)]
        NW = P.sb([128, 10, 64], F32, "NW")
        QK = [P.sb([128, 1024], BF16, f"QK{i}") for i in range(2)]
        tq = [P.sb([128, 640], F32, f"tq{i}") for i in range(2)]
        tr = P.sb([128, 640], F32, "tr")
        ss = P.sb([128, NT, 32], F32, "ss")
        evw = P.dma_k("pool", win[:, :, :], w_in[l, :, 0:768].rearrange("(k p) n -> p k n", p=128), "ldw_in")
        evr = []
        for h in range(10):
            src = q_norm_w if h < 8 else k_norm_w
            evr.append(P.dma("sp", NW[:, h, :], bcast_row(src[l:l + 1, :]), "ld2"))
        P.dve(lambda e: e.memset(Vaug[:, :, :, 64:65], 1.0))
        P.dve(lambda e: e.memset(poolP[:, :, :], 0.0))
        for qq in QK:
            P.dve(lambda e, qq=qq: e.memset(qq[:, :], 0.0))
        P.wait("pe", evw)
        P.wait("dve", evr)
        ringA = Ring([0, 1])
        ringB = Ring([2, 3])
        ringT = Ring([4, 5])
        qkfree = [None, None]
        ropefree = [None, None]
        for i in range(NT):
            s = i % 2
            if i < NXT:
                P.wait("sp", ropefree[s])
                evrope = P.dma("sp", ropet[s][:, :], c_rope[:, i, :], "ld2r%d" % s)
            ai, a = ringA.next(P)
            bi, b = ringB.next(P)
            for k in range(8):
                P.pe(lambda e, k=k, a=a, i=i: e.matmul(psv(a)[:, :], lhsT=hT[:, k, i * 128:(i + 1) * 128], rhs=win[:, k, 0:512], start=(k == 0), stop=(k == 7)))
            for k in range(8):
                P.pe(lambda e, k=k, b=b, i=i: e.matmul(psv(b)[:, 0:256], lhsT=hT[:, k, i * 128:(i + 1) * 128], rhs=win[:, k, 512:768], start=(k == 0), stop=(k == 7)))
            evp = P.sig_last("pe")
            P.wait("act", evp)
            P.act(lambda e, b=b, i=i: e.copy(out=Vaug[:, i, :, 0:64], in_=psv(b)[:, 128:256].rearrange("p (g d) -> p g d", g=2)))
            P.wait("dve", [evp, qkfree[s]])
            t = tq[s]
            P.dve(lambda e, a=a, t=t: e.tensor_copy(out=t[:, 0:512], in_=psv(a)[:, :]))
            P.dve(lambda e, b=b, t=t: e.tensor_copy(out=t[:, 512:640], in_=psv(b)[:, 0:128]))
            evcp = P.sig_last("dve")
            P.dve(lambda e, t=t: e.tensor_tensor(out=tr[:, :], in0=t[:, :], in1=t[:, :], op=ALU.mult))
            P.dve(lambda e, i=i: e.tensor_reduce(out=ss[:, i, 0:10], in_=tr[:, :].rearrange("p (h d) -> p h d", d=64), axis=AX.X, op=ALU.add))
            evd = P.sig_last("dve")
            P.wait("act", evd)
            P.act(lambda e, i=i: e.activation(out=ss[:, i, 10:20], in_=ss[:, i, 0:10], func=AF.Sqrt, bias=epsc[:, 0:1], scale=1.0 / 64))
            eva = P.sig_last("act")
            ringA.done(ai, evcp)
            ringB.done(bi, eva)
            P.wait("dve", eva)
            P.dve(lambda e, i=i: e.reciprocal(out=ss[:, i, 20:30], in_=ss[:, i, 10:20]))
            t3 = t[:, :].rearrange("p (h d) -> p h d", d=64)
            P.dve(lambda e, i=i, t3=t3: e.tensor_tensor(out=t3, in0=t3, in1=ss[:, i, 20:30].unsqueeze(2).broadcast_to([128, 10, 64]), op=ALU.mult))
            P.dve(lambda e, t3=t3: e.tensor_tensor(out=t3, in0=t3, in1=NW[:, :, :], op=ALU.mult))
            qk = QK[s]
            if i < NXT:
                P.wait("dve", evrope)
                rp = ropet[s]
                t5 = t[:, :].rearrange("p (h x f d) -> p h x f d", h=10, x=2, f=2)
                r5 = tr[:, :].rearrange("p (h x f d) -> p h x f d", h=10, x=2, f=2)
                sn = rp[:, 64:128].rearrange("p (x f d) -> p x f d", x=2, f=2)
                cs = rp[:, 0:64]
                for f in range(2):
                    P.dve(lambda e, f=f, t5=t5, r5=r5, sn=sn: e.tensor_tensor(out=r5[:, :, :, f, :], in0=t5[:, :, :, 1 - f, :],
                                                                             in1=sn[:, :, f, :].unsqueeze(1).broadcast_to([128, 10, 2, 16]), op=ALU.mult))
                P.dve(lambda e, t3=t3, cs=cs: e.tensor_tensor(out=t3, in0=t3, in1=cs.unsqueeze(1).broadcast_to([128, 10, 64]), op=ALU.mult))
                P.dve(lambda e, t=t, qk=qk: e.tensor_tensor(out=qk[:, 0:512], in0=t[:, 0:512], in1=tr[:, 0:512], op=ALU.add))
                for g in range(2):
                    for dup in range(2):
                        P.dve(lambda e, t=t, qk=qk, g=g, dup=dup: e.tensor_tensor(out=qk[:, 512 + g * 256 + dup * 192:512 + g * 256 + dup * 192 + 64],
                                                                               in0=t[:, 512 + g * 64:576 + g * 64], in1=tr[:, 512 + g * 64:576 + g * 64], op=ALU.add))
                ropefree[s] = P.sig_last("dve")
            else:
                P.dve(lambda e, t=t, qk=qk: e.tensor_copy(out=qk[:, 0:512], in_=t[:, 0:512]))
                for g in range(2):
                    for dup in range(2):
                        P.dve(lambda e, t=t, qk=qk, g=g, dup=dup: e.tensor_copy(out=qk[:, 512 + g * 256 + dup * 192:512 + g * 256 + dup * 192 + 64],
                                                                             in_=t[:, 512 + g * 64:576 + g * 64]))
            evq = P.sig_last("dve")
            ti, tb = ringT.next(P)
            P.wait("pe", evq)
            tp = psv(tb, BF16)
            for c in range(8):
                P.pe(lambda e, c=c, tp=tp, qk=qk: e.transpose(out=tp[:, c * 128:(c + 1) * 128], in_=qk[:, c * 128:(c + 1) * 128], identity=ident_b[:, :]))
            evt = P.sig_last("pe")
            qkfree[s] = evt
            P.wait("act", evt)
            P.act(lambda e, tp=tp, i=i: e.copy(out=qT[:, :, i * 128:(i + 1) * 128], in_=tp[:, 0:512].rearrange("p (c t) -> p c t", c=4)))
            P.act(lambda e, tp=tp, i=i: e.copy(out=kT2[:, :, :, i * 128:(i + 1) * 128], in_=tp[:, 512:1024].rearrange("p (g h t) -> p g h t", g=2, h=2)))
            ringT.done(ti, P.sig_last("act"))
        evpe = P.sig_last("pe")
        P.wait("pool", evpe)
        evw = P.dma_k("pool", win[:, :, 0:512], w_in[l, :, 768:1280].rearrange("(k p) n -> p k n", p=128), "ldw_in")
        P.wait("pe", evw)
        ringU = Ring([6, 7])
        for m_ in range(4):
            for (t0, tn) in TSL:
                ui, ub = ringU.next(P)
                for k in range(8):
                    P.pe(lambda e, k=k, ub=ub, m_=m_, t0=t0, tn=tn: e.matmul(psv(ub)[:, 0:tn], lhsT=win[:, k, m_ * 128:(m_ + 1) * 128],
                                                                          rhs=hT[:, k, t0:t0 + tn], start=(k == 0), stop=(k == 7)))
                evp = P.sig_last("pe")
                P.wait("act", evp)
                if m_ < 2:
                    P.act(lambda e, ub=ub, m_=m_, t0=t0, tn=tn: e.copy(out=uT[:, m_, t0:t0 + tn], in_=psv(ub)[:, 0:tn]))
                else:
                    po = 8 + t0 if t0 < L else 8 + L + 8 + 8
                    P.act(lambda e, ub=ub, m_=m_, po=po, tn=tn: e.copy(out=poolP[:, m_ - 2, po:po + tn], in_=psv(ub)[:, 0:tn]))
                ringU.done(ui, P.sig_last("act"))
        P.barrier()
        if l == 0:
            dump("qT", qT[:, :, :].rearrange("p k t -> p (k t)"))
            dump("kT2", kT2[:, :, :, :].rearrange("p g h t -> p (g h t)"))
            dump("uT", uT[:, :, :].rearrange("p k t -> p (k t)"))
            dump("poolP", poolP[:, :, :].rearrange("p k t -> p (k t)"))

        if stop == 'L2':
            return finish_prog()
        P.release(R5)
        esink = P.sb([128, 8], F32, "esink")
        ONW = P.sb([128, 512], F32, "ONW")
        PT = [P.sb([128, 512], BF16, f"PT{i}") for i in range(6)]
        Otm = P.sb([128, 512], F32, "Otm")
        Ob = P.sb([128, 512], BF16, "Ob")
        junk3 = P.sb([128, 512], BF16, "junk3")
        den = P.sb([128, 8], F32, "den")
        st3 = P.sb([128, NT, 4], F32, "st3")
        ev1 = P.dma("sp", esink[:, :], bcast_row(attn_sink[l:l + 1, :]), "ld3")
        ev2 = P.dma("sp", ONW[:, :], bcast_row(out_norm_w[l:l + 1, 0:512]), "ld3")
        P.wait("act", ev1)
        P.act(lambda e: e.activation(out=esink[:, :], in_=esink[:, :], func=AF.Exp))
        ev_es = P.sig_last("act")
        P.wait("dve", [ev_es, ev2])
        ringS = Ring([0, 1, 2])
        ringO = Ring([3, 4])
        ringT = Ring([5, 6])
        ptfree = [None] * 6
        pti = 0
        o_free = None
        ob_free = None
        qtiles = list(range(NXT)) + ([] if last else [NXT, NXT + 1])
        for n in qtiles:
            if n < NXT:
                kbs = ([(n - 1, 0)] if n > 0 else []) + [(n, None)] + ([(n + 1, 1)] if n < NXT - 1 else []) + [(NXT, None), (NXT + 1, None)]
            else:
                kbs = [(NXT, None), (NXT + 1, None)]
            for g in range(2):
                pts = []
                for (kb, mk) in kbs:
                    si, sbk = ringS.next(P)
                    for hp in range(2):
                        P.pe(lambda e, hp=hp, sbk=sbk, g=g, kb=kb, n=n: e.matmul(psv(sbk)[:, hp * 256:(hp + 1) * 256],
                                                                                lhsT=kT2[:, g, hp, kb * 128:(kb + 1) * 128],
                                                                                rhs=qT[:, 2 * g:2 * g + 2, n * 128:(n + 1) * 128],
                                                                                start=True, stop=True))
                    evp = P.sig_last("pe")
                    pslot = pti % 6
                    pti += 1
                    P.wait("act", [evp, ptfree[pslot]])
                    pt = PT[pslot]
                    P.act(lambda e, pt=pt, sbk=sbk: e.activation(out=pt[:, :], in_=psv(sbk)[:, :], func=AF.Exp, scale=ATT_SCALE))
                    eva = P.sig_last("act")
                    ringS.done(si, eva)
                    if mk is not None:
                        P.wait("dve", eva)
                        P.dve(lambda e, pt=pt, mk=mk: e.tensor_tensor(out=pt[:, :].rearrange("p (s q) -> p s q", s=4), in0=pt[:, :].rearrange("p (s q) -> p s q", s=4),
                                                                      in1=masks[:, mk, :].unsqueeze(1).broadcast_to([128, 4, 128]), op=ALU.mult))
                        eva = P.sig_last("dve")
                    pts.append((pslot, pt, kb, eva))
                oi, ob = ringO.next(P)
                ops = psv(ob)[:, 0:260].rearrange("p (s d) -> p s d", d=65)
                for s_ in range(4):
                    for j, (pslot, pt, kb, eva) in enumerate(pts):
                        P.wait("pe", eva)
                        P.pe(lambda e, s_=s_, pt=pt, kb=kb, g=g, j=j, ops=ops, npt=len(pts): e.matmul(ops[:, s_, :], lhsT=pt[:, s_ * 128:(s_ + 1) * 128],
                                                                                                     rhs=Vaug[:, kb, g, :], start=(j == 0), stop=(j == npt - 1)))
                evo = P.sig_last("pe")
                for (pslot, pt, kb, eva) in pts:
                    ptfree[pslot] = evo
                P.wait("dve", [evo, o_free])
                es_v = esink[:, :].rearrange("p (g pl hp) -> p g hp pl", g=2, pl=2)[:, g]
                P.dve(lambda e, ops=ops, es_v=es_v: e.tensor_tensor(out=den[:, 0:4].rearrange("p (a b) -> p a b", a=2), in0=ops[:, :, 64].rearrange("p (a b) -> p a b", a=2), in1=es_v, op=ALU.add))
                P.dve(lambda e: e.reciprocal(out=den[:, 4:8], in_=den[:, 0:4]))
                o_v = Otm[:, :].rearrange("p (g pl hp d) -> p g hp pl d", g=2, pl=2, hp=2)[:, g]
                for hp in range(2):
                    P.dve(lambda e, hp=hp, ops=ops, o_v=o_v: e.tensor_tensor(out=o_v[:, hp], in0=ops[:, 2 * hp:2 * hp + 2, 0:64],
                                                                          in1=den[:, 4 + 2 * hp:6 + 2 * hp].unsqueeze(2).broadcast_to([128, 2, 64]), op=ALU.mult))
                ringO.done(oi, P.sig_last("dve"))
            evd = P.sig_last("dve")
            P.wait("act", evd)
            P.act(lambda e, n=n: e.activation(out=junk3[:, :], in_=Otm[:, :], func=AF.Square, accum_out=st3[:, n, 0:1]))
            P.act(lambda e, n=n: e.activation(out=st3[:, n, 1:2], in_=st3[:, n, 0:1], func=AF.Sqrt, bias=epsc[:, 0:1], scale=1.0 / 512))
            eva = P.sig_last("act")
            P.wait("dve", [eva, ob_free])
            P.dve(lambda e, n=n: e.reciprocal(out=st3[:, n, 2:3], in_=st3[:, n, 1:2]))
            P.dve(lambda e, n=n: e.scalar_tensor_tensor(out=Ob[:, :], in0=Otm[:, :], scalar=st3[:, n, 2:3], in1=ONW[:, :], op0=ALU.mult, op1=ALU.mult))
            evd = P.sig_last("dve")
            o_free = evd
            ti, tb = ringT.next(P)
            P.wait("pe", evd)
            tp = psv(tb, BF16)
            for c in range(4):
                P.pe(lambda e, c=c, tp=tp: e.transpose(out=tp[:, c * 128:(c + 1) * 128], in_=Ob[:, c * 128:(c + 1) * 128], identity=ident_b[:, :]))
            evt = P.sig_last("pe")
            ob_free = evt
            P.wait("act", evt)
            P.act(lambda e, tp=tp, n=n: e.copy(out=mergedT[:, 0:4, n * 128:(n + 1) * 128], in_=tp[:, 0:512].rearrange("p (c t) -> p c t", c=4)))
            ringT.done(ti, P.sig_last("act"))
        P.barrier()

        if l == 0:
            dump("mT3", mergedT[:, :, :].rearrange("p k t -> p (k t)"))
        if stop == 'L3':
            return finish_prog()
        P.release(R5)
        pa = P.sb([128, PW], F32, "pa")
        pb_ = P.sb([128, PW], F32, "pb")
        dlt = P.sb([128, 2, T], BF16, "dlt")
        dfx = P.sb([128, 16], F32, "dfx")
        pwst = P.sb([128, 2, 128], F32, "pwst")
        pwb = P.sb([128, 2, 128], BF16, "pwb")
        psc = P.sb([128, 2], F32, "psc")
        P.dve(lambda e: e.memset(pwst[:, :, :], 0.0))
        evz = P.sig_last("dve")
        P.wait("sp", evz)
        evs = []
        for ct in range(2):
            for hh in range(2):
                evs.append(P.dma("sp", pwst[hh * 64:(hh + 1) * 64, ct, hh * 64:(hh + 1) * 64], pool_w[l, 2 * ct + hh, :, :], "ld4"))
            evs.append(P.dma("sp", psc[:, ct:ct + 1], pool_scale[l, ct * 128:(ct + 1) * 128].rearrange("(p o) -> p o", o=1), "ld4"))
        P.wait("dve", evs)
        P.wait("act", evs)
        P.dve(lambda e: e.tensor_copy(out=pwb[:, :, :], in_=pwst[:, :, :]))
        segs = [(8, L, 0), (8 + L + 8 + 8, LC, L)]
        for ct in range(2):
            for hh in range(2):
                w = POOLW[2 * ct + hh]
                pr = slice(hh * 64, (hh + 1) * 64)
                for (po, ln, tok0) in segs:
                    lo = po - 8
                    n_all = ln + 16
                    src = poolP[pr, ct, lo:lo + n_all]
                    bufs = [pa[pr, 0:n_all], pb_[pr, 0:n_all]]
                    cur = src
                    sh = 1
                    bi = 0
                    while sh < w:
                        dst = bufs[bi]
                        P.dve(lambda e, dst=dst, cur=cur, sh=sh, n_all=n_all: e.tensor_tensor(out=dst[:, sh:n_all], in0=cur[:, sh:n_all], in1=cur[:, 0:n_all - sh], op=ALU.add))
                        cur = dst
                        bi ^= 1
                        sh *= 2
                    o0 = 8 + w // 2 - 1
                    P.dve(lambda e, cur=cur, o0=o0, ln=ln, w=w, ct=ct, pr=pr, po=po, tok0=tok0: e.scalar_tensor_tensor(
                        out=dlt[pr, ct, tok0:tok0 + ln], in0=cur[:, o0:o0 + ln], scalar=1.0 / w, in1=poolP[pr, ct, po:po + ln], op0=ALU.mult, op1=ALU.subtract))
                    hw = w // 2
                    for side in range(2):
                        tb0 = 0 if side == 0 else ln - hw
                        P.dve(lambda e, cur=cur, o0=o0, tb0=tb0, hw=hw, pr=pr, ct=ct, side=side: e.tensor_tensor(
                            out=dfx[pr, 0:hw], in0=cur[:, o0 + tb0:o0 + tb0 + hw], in1=poolfix[pr, ct, side, 0:hw], op=ALU.mult))
                        P.dve(lambda e, tb0=tb0, hw=hw, pr=pr, ct=ct, po=po, tok0=tok0: e.tensor_tensor(
                            out=dlt[pr, ct, tok0 + tb0:tok0 + tb0 + hw], in0=dfx[pr, 0:hw], in1=poolP[pr, ct, po + tb0:po + tb0 + hw], op=ALU.subtract))
        evd = P.sig_last("dve")
        P.wait("pe", evd)
        ringP = Ring([0, 1])
        for ct in range(2):
            for (t0, tn) in TSL:
                pi, pbk = ringP.next(P)
                P.pe(lambda e, ct=ct, t0=t0, tn=tn, pbk=pbk: e.matmul(psv(pbk)[:, 0:tn], lhsT=pwb[:, ct, :], rhs=dlt[:, ct, t0:t0 + tn], start=True, stop=True))
                evp = P.sig_last("pe")
                P.wait("act", evp)
                P.act(lambda e, ct=ct, t0=t0, tn=tn, pbk=pbk: e.activation(out=mergedT[:, 6 + ct, t0:t0 + tn], in_=psv(pbk)[:, 0:tn], func=AF.Copy, scale=psc[:, ct:ct + 1]))
                ringP.done(pi, P.sig_last("act"))
        P.barrier()

        if l == 0:
            dump("mT4", mergedT[:, :, :].rearrange("p k t -> p (k t)"))
        if stop == 'L4':
            return finish_prog()
        P.release(R3)
        prm = P.sb([128, 12, 2, 8], F32, "prm")
        upw = P.sb([128, 2, 8, 3, 12], F32, "upw")
        rcol = P.sb([128, 2, 8], F32, "rcol")
        TP = P.sb([128, 2, 16, 16], F32, "TP")
        tt1 = P.sb([128, 16, 8], F32, "tt1")
        tt2 = P.sb([128, 16, 8], F32, "tt2")
        Cs = P.sb([128, 2, 2, 8, 16], F32, "Cs")
        bbz = P.sb([128, 2, 128], BF16, "bbz")
        BzT = P.sb([128, 2, 2, 128], BF16, "BzT")
        Cz = P.sb([128, 2, 2, 128], BF16, "Cz")
        pl = [P.sb([128, T], F32, f"pl{i}") for i in range(6)]
        sbf = mergedT[:, 4:6, :]
        bst = pl[5][:, 0:512].rearrange("p (d r j q) -> p d r j q", d=2, r=2, j=8)
        yT = P.sb([128, 2, T], F32, "yT")
        dcol = P.sb([128, 8], F32, "dcol")
        gst = P.sb([128, 2, 256], F32, "gst")
        gwb = P.sb([128, 2, 256], BF16, "gwb")
        bbr = P.sb([128, 2, 8, 16], F32, "bbr")
        bbi = P.sb([128, 2, 8, 16], F32, "bbi")
        btmp = pl[4][:, 0:256].rearrange("p (d j q) -> p d j q", d=2, j=8)
        evs = []
        for gl in range(2):
            pr = slice(gl * 64, (gl + 1) * 64)
            for d in range(2):
                evs.append(P.dma("sp", prm[pr, 0, d, :], ssm_a_re[l, d].rearrange("(j gl) n -> gl n j", gl=2)[gl], "ld5", allow_slow_non_contiguous=True))
                evs.append(P.dma("sp", prm[pr, 1, d, :], ssm_a_im[l, d].rearrange("(j gl) n -> gl n j", gl=2)[gl], "ld5", allow_slow_non_contiguous=True))
                evs.append(P.dma("sp", prm[pr, 2, d, :], ssm_log_dt[l, d:d + 1, :].rearrange("o (j gl) -> gl o j", gl=2)[gl].partition_broadcast(64)[:, 0, :],
                                 "ld5", allow_slow_non_contiguous=True))
                for ri, src in enumerate((ssm_b_re, ssm_b_im)):
                    evs.append(P.dma("sp", bst[pr, d, ri, :, :], src[l, d].rearrange("(j gl) n p -> gl n j p", gl=2)[gl], "ld5"))
                for ri, src in enumerate((ssm_c_re, ssm_c_im)):
                    for jj in range(8):
                        evs.append(P.dma("sp", Cs[pr, d, ri, jj, :], src[l, d, 2 * jj + gl].rearrange("p n -> n p"), "ld5c_%d" % l, allow_slow_non_contiguous=True))
        for m_ in range(2):
            evs.append(P.dma("sp", dcol[:, m_:m_ + 1], ssm_d[l, m_ * 128:(m_ + 1) * 128].rearrange("(p o) -> p o", o=1), "ld5"))
            evs.append(P.dma("sp", dcol[:, 2 + m_:3 + m_], glu_b[l, m_ * 128:(m_ + 1) * 128].rearrange("(p o) -> p o", o=1), "ld5"))
            evs.append(P.dma("sp", dcol[:, 4 + m_:5 + m_], out_norm_w[l, 512 + m_ * 128:512 + (m_ + 1) * 128].rearrange("(p o) -> p o", o=1), "ld5"))
        evs.append(P.dma_k("sp", gst[:, :, :], glu_w[l].rearrange("(k p) n -> p k n", p=128), "ld5"))
        P.wait("act", evs)
        P.wait("dve", evs)
        if stop == 'L5a':
            return finish_prog()
        f2 = lambda s_: prm[:, s_, :, :].rearrange("p d j -> p (d j)")
        a_re, a_im, ldt = f2(0), f2(1), f2(2)
        dt_, mag, th, cc_, sn_, ar, ai, t1, t2 = f2(3), f2(4), f2(5), f2(6), f2(7), f2(8), f2(9), f2(10), f2(11)
        P.act(lambda e: e.activation(out=dt_, in_=ldt, func=AF.Exp))
        eva = P.sig_last("act")
        P.wait("dve", eva)
        P.dve(lambda e: e.tensor_scalar(out=Cs[:, :, 1, :, :], in0=Cs[:, :, 1, :, :], scalar1=-1.0, scalar2=None, op0=ALU.mult))
        P.dve(lambda e: e.tensor_tensor(out=mag, in0=a_re, in1=dt_, op=ALU.mult))
        P.dve(lambda e: e.tensor_tensor(out=th, in0=a_im, in1=dt_, op=ALU.mult))
        evd = P.sig_last("dve")
        P.wait("act", evd)
        P.act(lambda e: e.activation(out=mag, in_=mag, func=AF.Exp))
        P.act(lambda e: e.activation(out=sn_, in_=th, func=AF.Sin, scale=1.0 / 16))
        P.act(lambda e: e.activation(out=cc_, in_=th, func=AF.Sin, scale=1.0 / 16, bias=halfpi[:, 0:1]))
        eva = P.sig_last("act")
        P.wait("dve", eva)
        for _ in range(4):
            P.dve(lambda e: e.tensor_tensor(out=t1, in0=cc_, in1=sn_, op=ALU.mult))
            P.dve(lambda e: e.tensor_tensor(out=cc_, in0=cc_, in1=cc_, op=ALU.mult))
            P.dve(lambda e: e.tensor_tensor(out=t2, in0=sn_, in1=sn_, op=ALU.mult))
            P.dve(lambda e: e.tensor_tensor(out=cc_, in0=cc_, in1=t2, op=ALU.subtract))
            P.dve(lambda e: e.tensor_scalar(out=sn_, in0=t1, scalar1=2.0, scalar2=None, op0=ALU.mult))
        P.dve(lambda e: e.tensor_tensor(out=t1, in0=cc_, in1=cc_, op=ALU.mult))
        P.dve(lambda e: e.tensor_tensor(out=t2, in0=sn_, in1=sn_, op=ALU.mult))
        P.dve(lambda e: e.tensor_tensor(out=t1, in0=t1, in1=t2, op=ALU.add))
        evd = P.sig_last("dve")
        P.wait("act", evd)
        P.act(lambda e: e.activation(out=ar, in_=t1, func=AF.Sqrt))
        eva = P.sig_last("act")
        P.wait("dve", eva)
        P.dve(lambda e: e.reciprocal(out=t2, in_=ar))
        P.dve(lambda e: e.tensor_tensor(out=ar, in0=t2, in1=t2, op=ALU.mult))
        P.dve(lambda e: e.tensor_tensor(out=ar, in0=ar, in1=t1, op=ALU.mult))
        P.dve(lambda e: e.tensor_scalar(out=ar, in0=ar, scalar1=-0.5, scalar2=1.5, op0=ALU.mult, op1=ALU.add))
        P.dve(lambda e: e.tensor_tensor(out=t2, in0=t2, in1=ar, op=ALU.mult))
        P.dve(lambda e: e.tensor_tensor(out=cc_, in0=cc_, in1=t2, op=ALU.mult))
        P.dve(lambda e: e.tensor_tensor(out=sn_, in0=sn_, in1=t2, op=ALU.mult))
        P.dve(lambda e: e.tensor_tensor(out=ar, in0=mag, in1=cc_, op=ALU.mult))
        P.dve(lambda e: e.tensor_tensor(out=ai, in0=mag, in1=sn_, op=ALU.mult))
        upv = lambda c, k: upw[:, :, :, c, k].rearrange("p d j -> p (d j)")
        P.dve(lambda e: e.tensor_copy(out=rcol[:, :, :].rearrange("p d j -> p (d j)"), in_=mag))
        P.dve(lambda e: e.tensor_copy(out=upv(0, 0), in_=cc_))
        P.dve(lambda e: e.tensor_copy(out=upv(1, 0), in_=sn_))
        for k in range(1, 12):
            P.dve(lambda e, k=k: e.tensor_tensor(out=t1, in0=upv(0, k - 1), in1=upv(0, k - 1), op=ALU.mult))
            P.dve(lambda e, k=k: e.tensor_tensor(out=t2, in0=upv(1, k - 1), in1=upv(1, k - 1), op=ALU.mult))
            P.dve(lambda e, k=k: e.tensor_tensor(out=upv(0, k), in0=t1, in1=t2, op=ALU.subtract))
            P.dve(lambda e, k=k: e.tensor_tensor(out=t1, in0=upv(0, k - 1), in1=upv(1, k - 1), op=ALU.mult))
            P.dve(lambda e, k=k: e.tensor_scalar(out=upv(1, k), in0=t1, scalar1=2.0, scalar2=None, op0=ALU.mult))
        P.dve(lambda e: e.tensor_scalar(out=upw[:, :, :, 2, :], in0=upw[:, :, :, 1, :], scalar1=-1.0, scalar2=None, op0=ALU.mult))
        P.dve(lambda e: e.memset(TP[:, 0, :, 0:1], 1.0))
        P.dve(lambda e: e.memset(TP[:, 1, :, 0:1], 0.0))
        for k in range(4):
            m = 1 << k
            ucb = upv(0, k).unsqueeze(2).broadcast_to([128, 16, m])
            usb = upv(1, k).unsqueeze(2).broadcast_to([128, 16, m])
            c_old, s_old = TP[:, 0, :, 0:m], TP[:, 1, :, 0:m]
            P.dve(lambda e, m=m, ucb=ucb, c_old=c_old: e.tensor_tensor(out=tt1[:, :, 0:m], in0=c_old, in1=ucb, op=ALU.mult))
            P.dve(lambda e, m=m, usb=usb, s_old=s_old: e.tensor_tensor(out=tt2[:, :, 0:m], in0=s_old, in1=usb, op=ALU.mult))
            P.dve(lambda e, m=m: e.tensor_tensor(out=TP[:, 0, :, m:2 * m], in0=tt1[:, :, 0:m], in1=tt2[:, :, 0:m], op=ALU.subtract))
            P.dve(lambda e, m=m, ucb=ucb, s_old=s_old: e.tensor_tensor(out=tt1[:, :, 0:m], in0=s_old, in1=ucb, op=ALU.mult))
            P.dve(lambda e, m=m, usb=usb, c_old=c_old: e.tensor_tensor(out=tt2[:, :, 0:m], in0=c_old, in1=usb, op=ALU.mult))
            P.dve(lambda e, m=m: e.tensor_tensor(out=TP[:, 1, :, m:2 * m], in0=tt1[:, :, 0:m], in1=tt2[:, :, 0:m], op=ALU.add))
        P.dve(lambda e: e.tensor_tensor(out=t1, in0=a_re, in1=a_re, op=ALU.mult))
        P.dve(lambda e: e.tensor_tensor(out=t2, in0=a_im, in1=a_im, op=ALU.mult))
        P.dve(lambda e: e.tensor_tensor(out=t1, in0=t1, in1=t2, op=ALU.add))
        P.dve(lambda e: e.reciprocal(out=dt_, in_=t1))
        P.dve(lambda e: e.tensor_scalar(out=cc_, in0=ar, scalar1=-1.0, scalar2=None, op0=ALU.add))
        P.dve(lambda e: e.tensor_tensor(out=t1, in0=cc_, in1=a_re, op=ALU.mult))
        P.dve(lambda e: e.tensor_tensor(out=t2, in0=ai, in1=a_im, op=ALU.mult))
        P.dve(lambda e: e.tensor_tensor(out=t1, in0=t1, in1=t2, op=ALU.add))
        P.dve(lambda e: e.tensor_tensor(out=mag, in0=t1, in1=dt_, op=ALU.mult))
        P.dve(lambda e: e.tensor_tensor(out=t1, in0=ai, in1=a_re, op=ALU.mult))
        P.dve(lambda e: e.tensor_tensor(out=t2, in0=cc_, in1=a_im, op=ALU.mult))
        P.dve(lambda e: e.tensor_tensor(out=t1, in0=t1, in1=t2, op=ALU.subtract))
        P.dve(lambda e: e.tensor_tensor(out=th, in0=t1, in1=dt_, op=ALU.mult))
        P.dve(lambda e: e.tensor_copy(out=gwb[:, :, :], in_=gst[:, :, :]))
        qr3 = prm[:, 4, :, :]
        qi3 = prm[:, 5, :, :]
        bq = lambda q3: q3.unsqueeze(3).broadcast_to([128, 2, 8, 16])
        P.dve(lambda e: e.tensor_tensor(out=bbr[:, :, :, :], in0=bst[:, :, 0, :, :], in1=bq(qr3), op=ALU.mult))
        P.dve(lambda e: e.tensor_tensor(out=btmp[:, :, :, :], in0=bst[:, :, 1, :, :], in1=bq(qi3), op=ALU.mult))
        P.dve(lambda e: e.tensor_tensor(out=bbr[:, :, :, :], in0=bbr[:, :, :, :], in1=btmp[:, :, :, :], op=ALU.subtract))
        P.dve(lambda e: e.tensor_tensor(out=bbi[:, :, :, :], in0=bst[:, :, 1, :, :], in1=bq(qr3), op=ALU.mult))
        P.dve(lambda e: e.tensor_tensor(out=btmp[:, :, :, :], in0=bst[:, :, 0, :, :], in1=bq(qi3), op=ALU.mult))
        P.dve(lambda e: e.tensor_tensor(out=bbi[:, :, :, :], in0=bbi[:, :, :, :], in1=btmp[:, :, :, :], op=ALU.add))
        if stop == 'L5b':
            return finish_prog()
        ringX = Ring([0, 1, 2, 3])
        ringY = Ring([4, 5])
        ringZ = Ring([6, 7])
        bz_free = None
        sbf_free = None
        y_started = [False, False]
        for j in range(8):
            mt = j // 4
            P.wait("dve", bz_free)
            for d in range(2):
                P.dve(lambda e: e.memset(bbz[:, :, :], 0.0))
                for ri, bb in enumerate((bbr, bbi)):
                    for gl in range(2):
                        c0 = 16 * ((2 * j + gl) % 8)
                        P.dve(lambda e, gl=gl, c0=c0, bb=bb, d=d, ri=ri, j=j: e.tensor_copy(out=bbz[gl * 64:(gl + 1) * 64, ri, c0:c0 + 16], in_=bb[gl * 64:(gl + 1) * 64, d, j, :]))
                evd = P.sig_last("dve")
                zi, zb = ringZ.next(P)
                P.wait("pe", evd)
                for ri in range(2):
                    P.pe(lambda e, ri=ri, zb=zb: e.transpose(out=psv(zb, BF16)[:, ri * 128:(ri + 1) * 128], in_=bbz[:, ri, :], identity=ident_b[:, :]))
                evp = P.sig_last("pe")
                P.wait("dve", evp)
                P.dve(lambda e, zb=zb, d=d: e.tensor_copy(out=BzT[:, d, :, :], in_=psv(zb, BF16)[:, 0:256].rearrange("p (r s) -> p r s", r=2)))
                ringZ.done(zi, P.sig_last("dve"))
                P.dve(lambda e, d=d: e.memset(Cz[:, d, :, :], 0.0))
                for ri in range(2):
                    for gl in range(2):
                        c0 = 16 * ((2 * j + gl) % 8)
                        P.dve(lambda e, gl=gl, c0=c0, d=d, ri=ri, j=j: e.tensor_copy(out=Cz[gl * 64:(gl + 1) * 64, d, ri, c0:c0 + 16], in_=Cs[gl * 64:(gl + 1) * 64, d, ri, j, :]))
            ev_mats = P.sig_last("dve")
            P.wait("pe", ev_mats)
            for d in range(2):
                for (t0, tn) in TSL:
                    c0 = (t0 + LC if t0 < L else 0) if d == 0 else t0
                    for ri in range(2):
                        xi, xb = ringX.next(P)
                        P.pe(lambda e, ri=ri, xb=xb, d=d, t0=t0, tn=tn, mt=mt: e.matmul(psv(xb)[:, 0:tn], lhsT=BzT[:, d, ri, :], rhs=uT[:, mt, t0:t0 + tn], start=True, stop=True))
                        evp = P.sig_last("pe")
                        P.wait("act", [evp] + (pl_free[0] or []))
                        P.act(lambda e, ri=ri, xb=xb, c0=c0, tn=tn: e.copy(out=pl[ri][:, c0:c0 + tn], in_=psv(xb)[:, 0:tn]))
                        ringX.done(xi, P.sig_last("act"))
                eva = P.sig_last("act")
                P.wait("dve", [eva, sbf_free])
                V = (lambda ap: ap) if d == 0 else (lambda ap: ap[:, ::-1])
                Tc, Ts, P5, P6 = pl[2], pl[3], pl[4], pl[5]
                xr, xi = V(pl[0][:, :]), V(pl[1][:, :])
                cmb = d * 8 + j
                P.dve(lambda e, cmb=cmb: e.tensor_copy(out=Tc[:, 0:16], in_=TP[:, 0, cmb, :]))
                P.dve(lambda e, cmb=cmb: e.tensor_copy(out=Ts[:, 0:16], in_=TP[:, 1, cmb, :]))
                for k in range(4, 12):
                    m = 1 << k
                    n = min(m, T - m)
                    uc = upw[:, d, j, 0, k:k + 1]
                    us = upw[:, d, j, 1, k:k + 1]
                    nus = upw[:, d, j, 2, k:k + 1]
                    P.dve(lambda e, m=m, n=n, uc=uc: e.tensor_scalar(out=Tc[:, m:m + n], in0=Tc[:, 0:n], scalar1=uc, scalar2=None, op0=ALU.mult))
                    P.dve(lambda e, m=m, n=n, nus=nus: e.scalar_tensor_tensor(out=Tc[:, m:m + n], in0=Ts[:, 0:n], scalar=nus, in1=Tc[:, m:m + n], op0=ALU.mult, op1=ALU.add))
                    P.dve(lambda e, m=m, n=n, uc=uc: e.tensor_scalar(out=Ts[:, m:m + n], in0=Ts[:, 0:n], scalar1=uc, scalar2=None, op0=ALU.mult))
                    P.dve(lambda e, m=m, n=n, us=us: e.scalar_tensor_tensor(out=Ts[:, m:m + n], in0=Tc[:, 0:n], scalar=us, in1=Ts[:, m:m + n], op0=ALU.mult, op1=ALU.add))
                P.dve(lambda e, xr=xr: e.tensor_tensor(out=P5[:, :], in0=Tc[:, :], in1=xr, op=ALU.mult))
                P.dve(lambda e, xi=xi: e.tensor_tensor(out=P6[:, :], in0=Ts[:, :], in1=xi, op=ALU.mult))
                P.dve(lambda e: e.tensor_tensor(out=P5[:, :], in0=P5[:, :], in1=P6[:, :], op=ALU.add))
                P.dve(lambda e, xr=xr: e.tensor_tensor(out=P6[:, :], in0=Ts[:, :], in1=xr, op=ALU.mult))
                P.dve(lambda e, xi=xi: e.tensor_tensor(out=xi, in0=Tc[:, :], in1=xi, op=ALU.mult))
                P.dve(lambda e, xi=xi: e.tensor_tensor(out=xi, in0=xi, in1=P6[:, :], op=ALU.subtract))
                rb = rcol[:, d, j:j + 1].broadcast_to([128, T])
                zr, zi = pl[0], P6
                P.dve(lambda e, rb=rb: e.tensor_tensor_scan(out=zr[:, :], data0=rb, data1=P5[:, :], initial=0.0, op0=ALU.mult, op1=ALU.add))
                P.dve(lambda e, rb=rb, xi=xi: e.tensor_tensor_scan(out=zi[:, :], data0=rb, data1=xi, initial=0.0, op0=ALU.mult, op1=ALU.add))
                tmp = pl[1]
                P.dve(lambda e: e.tensor_tensor(out=P5[:, :], in0=Tc[:, :], in1=zr[:, :], op=ALU.mult))
                P.dve(lambda e: e.tensor_tensor(out=tmp[:, :], in0=Ts[:, :], in1=zi[:, :], op=ALU.mult))
                so_re, so_im = V(sbf[:, 0, :]), V(sbf[:, 1, :])
                P.dve(lambda e, so_re=so_re: e.tensor_tensor(out=so_re, in0=P5[:, :], in1=tmp[:, :], op=ALU.subtract))
                P.dve(lambda e: e.tensor_tensor(out=P5[:, :], in0=Ts[:, :], in1=zr[:, :], op=ALU.mult))
                P.dve(lambda e: e.tensor_tensor(out=tmp[:, :], in0=Tc[:, :], in1=zi[:, :], op=ALU.mult))
                P.dve(lambda e, so_im=so_im: e.tensor_tensor(out=so_im, in0=P5[:, :], in1=tmp[:, :], op=ALU.add))
                pl_free[0] = [P.sig_last("dve")]
                P.wait("pe", pl_free[0])
                for (t0, tn) in TSL:
                    c0 = (t0 + LC if t0 < L else 0) if d == 0 else t0
                    yi, yb = ringY.next(P)
                    for ri in range(2):
                        P.pe(lambda e, d=d, ri=ri, yb=yb, c0=c0, tn=tn: e.matmul(psv(yb)[:, 0:tn], lhsT=Cz[:, d, ri, :], rhs=sbf[:, ri, c0:c0 + tn], start=(ri == 0), stop=(ri == 1)))
                    evp = P.sig_last("pe")
                    P.wait("dve", evp)
                    if not y_started[mt]:
                        P.dve(lambda e, yb=yb, t0=t0, tn=tn, mt=mt: e.tensor_copy(out=yT[:, mt, t0:t0 + tn], in_=psv(yb)[:, 0:tn]))
                    else:
                        P.dve(lambda e, yb=yb, t0=t0, tn=tn, mt=mt: e.tensor_tensor(out=yT[:, mt, t0:t0 + tn], in0=yT[:, mt, t0:t0 + tn], in1=psv(yb)[:, 0:tn], op=ALU.add))
                    ringY.done(yi, P.sig_last("dve"))
                y_started[mt] = True
                sbf_free = P.sig_last("pe")
            bz_free = P.sig_last("pe")
        P.barrier()
        if l == 0:
            dump("Tc", pl[2][:, :])
            dump("Ts", pl[3][:, :])
            dump("zr", pl[0][:, :])
        if stop == 'L5c':
            return finish_prog()
        if l == 0:
            dump("yT", yT[:, :, :].rearrange("p k t -> p (k t)"))
        g32 = pl[0]
        tA = pl[1]
        gb = pl[2][:, :].bitcast(BF16).rearrange("p (k t) -> p k t", k=2)
        sq = pl[3][:, :].bitcast(BF16).rearrange("p (k t) -> p k t", k=2)
        for m_ in range(2):
            y_ = yT[:, m_, :]
            P.dve(lambda e, m_=m_, y_=y_: e.scalar_tensor_tensor(out=y_, in0=uT[:, m_, :], scalar=dcol[:, m_:m_ + 1], in1=y_, op0=ALU.mult, op1=ALU.add))
            P.dve(lambda e, y_=y_: e.tensor_tensor(out=tA[:, :], in0=y_, in1=y_, op=ALU.mult))
            P.dve(lambda e: e.tensor_scalar(out=tA[:, :], in0=tA[:, :], scalar1=0.044715, scalar2=1.0, op0=ALU.mult, op1=ALU.add))
            P.dve(lambda e, y_=y_: e.tensor_tensor(out=tA[:, :], in0=tA[:, :], in1=y_, op=ALU.mult))
            evd = P.sig_last("dve")
            P.wait("act", evd)
            P.act(lambda e: e.activation(out=tA[:, :], in_=tA[:, :], func=AF.Sigmoid, scale=1.5957691216057308))
            eva = P.sig_last("act")
            P.wait("dve", eva)
            P.dve(lambda e, y_=y_: e.tensor_tensor(out=y_, in0=y_, in1=tA[:, :], op=ALU.mult))
            P.dve(lambda e, y_=y_, m_=m_: e.tensor_copy(out=gb[:, m_, :], in_=y_))
        evd = P.sig_last("dve")
        P.wait("pe", evd)
        P.wait("act", evd)
        ringG = Ring([0, 1])
        for m_ in range(2):
            for (t0, tn) in TSL:
                gi, gbk = ringG.next(P)
                for kt in range(2):
                    P.pe(lambda e, kt=kt, m_=m_, gbk=gbk, t0=t0, tn=tn: e.matmul(psv(gbk)[:, 0:tn], lhsT=gwb[:, kt, m_ * 128:(m_ + 1) * 128], rhs=gb[:, kt, t0:t0 + tn], start=(kt == 0), stop=(kt == 1)))
                evp = P.sig_last("pe")
                P.wait("act", evp)
                P.act(lambda e, m_=m_, gbk=gbk, t0=t0, tn=tn: e.activation(out=tA[:, t0:t0 + tn], in_=psv(gbk)[:, 0:tn], func=AF.Sigmoid, bias=dcol[:, 2 + m_:3 + m_]))
                eva = P.sig_last("act")
                ringG.done(gi, eva)
                P.wait("dve", eva)
                P.dve(lambda e, m_=m_, t0=t0, tn=tn: e.tensor_tensor(out=yT[:, m_, t0:t0 + tn], in0=yT[:, m_, t0:t0 + tn], in1=tA[:, t0:t0 + tn], op=ALU.mult))
                P.dve(lambda e, m_=m_, t0=t0, tn=tn: e.tensor_tensor(out=sq[:, m_, t0:t0 + tn], in0=yT[:, m_, t0:t0 + tn], in1=yT[:, m_, t0:t0 + tn], op=ALU.mult))
                evd = P.sig_last("dve")
                P.wait("act", evd)
        evd = P.sig_last("dve")
        P.wait("pe", evd)
        if l == 0:
            dump("ssm", yT[:, :, :].rearrange("p k t -> p (k t)"))
        ringG = Ring([2, 3])
        for (t0, tn) in TSL:
            gi, gbk = ringG.next(P)
            for kt in range(2):
                P.pe(lambda e, kt=kt, gbk=gbk, t0=t0, tn=tn: e.matmul(psv(gbk)[:, 0:tn], lhsT=ones_b[:, :], rhs=sq[:, kt, t0:t0 + tn], start=(kt == 0), stop=(kt == 1)))
            evp = P.sig_last("pe")
            P.wait("act", evp)
            P.act(lambda e, gbk=gbk, t0=t0, tn=tn: e.activation(out=g32[:, t0:t0 + tn], in_=psv(gbk)[:, 0:tn], func=AF.Sqrt, bias=epsc[:, 0:1], scale=1.0 / 256))
            eva = P.sig_last("act")
            ringG.done(gi, eva)
            P.wait("dve", eva)
            P.dve(lambda e, t0=t0, tn=tn: e.reciprocal(out=g32[:, t0:t0 + tn], in_=g32[:, t0:t0 + tn]))
            for m_ in range(2):
                P.dve(lambda e, m_=m_, t0=t0, tn=tn: e.scalar_tensor_tensor(out=mergedT[:, 4 + m_, t0:t0 + tn], in0=yT[:, m_, t0:t0 + tn], scalar=dcol[:, 4 + m_:5 + m_],
                                                                           in1=g32[:, t0:t0 + tn], op0=ALU.mult, op1=ALU.mult))
        P.barrier()
        if l == 0:
            dump("mergedT", mergedT[:, :, :].rearrange("p k t -> p (k t)"))

        if stop == 'L5':
            return finish_prog()
        P.release(R2)
        wob = P.sb([128, 8, D], BF16, "wob")
        g1 = [P.sb([128, D], F32, f"g1_{r}") for r in range(2)]
        tmp6 = [P.sb([128, 512], F32, f"tmp6_{i}") for i in range(2)]
        evw = P.dma_k("pool", wob[:, :, :], w_out[l].rearrange("(k p) n -> p k n", p=128), "ldw_out")
        evg = [P.dma("sp", g1[r][:, :], bcast_row(modscr[l, r:r + 1, 2 * D:3 * D]), "ld6") for r in range(2)]
        P.wait("pe", evw)
        P.wait("dve", evg)
        ring6 = Ring([0, 1, 2, 3])
        tfree = [None, None]
        it = 0
        for i in range(NXT if last else NT):
            r = 0 if i < NXT else 1
            for hf_ in range(2):
                oi, obk = ring6.next(P)
                for c in range(8):
                    P.pe(lambda e, c=c, obk=obk, i=i, hf_=hf_: e.matmul(psv(obk)[:, :], lhsT=mergedT[:, c, i * 128:(i + 1) * 128], rhs=wob[:, c, hf_ * 512:(hf_ + 1) * 512], start=(c == 0), stop=(c == 7)))
                evp = P.sig_last("pe")
                s = it % 2
                it += 1
                P.wait("dve", [evp, tfree[s]])
                P.dve(lambda e, obk=obk, r=r, hf_=hf_, s=s: e.tensor_tensor(out=tmp6[s][:, :], in0=psv(obk)[:, :], in1=g1[r][:, hf_ * 512:(hf_ + 1) * 512], op=ALU.mult))
                evd = P.sig_last("dve")
                ring6.done(oi, evd)
                P.wait("pool", evd)
                P.pool(lambda e, i=i, hf_=hf_, s=s: e.tensor_tensor(out=X[:, i, hf_ * 512:(hf_ + 1) * 512], in0=X[:, i, hf_ * 512:(hf_ + 1) * 512], in1=tmp6[s][:, :], op=ALU.add))
                tfree[s] = P.sig_last("pool")
        P.barrier()
        if l == 0:
            dump("xmix0", X[:, :, :].rearrange("p i d -> p (i d)"))
        if moe_experts == 0:
            continue

        P.new_epoch()
        ntm = NXT if last else NT
        P.release(R1)
        h2T = P.sb([128, 8, T], BF16, "h2T")
        Gs = P.sb([128, NT, NE], F32, "Gs")
        g2 = [P.sb([128, D], F32, f"g2_{r}") for r in range(2)]
        GTu = P.sb([128, NT, 128], BF16, "GTu")
        OFF_M = P.mark()
        LG = P.sb([128, NT, NE], F32, "LG")
        rwst = P.sb([128, 8, NE], F32, "rwst")
        rbb = P.sb([128, NE], F32, "rbb")
        h32 = P.sb([128, 8, 128], F32, "h32")
        m8 = P.sb([128, 8], F32, "m8")
        msk = P.sb([128, NE], F32, "msk")
        ex = P.sb([128, NE], F32, "ex")
        gu_ = P.sb([128, 128], F32, "gu_")
        st7 = P.sb([128, 4], F32, "st7")
        evs = [P.dma_k("sp", rwst[:, :, :], router_w[l].rearrange("(k p) n -> p k n", p=128), "ld7"),
               P.dma("sp", rbb[:, :], bcast_row(router_b[l:l + 1, :]), "ld7")]
        evs += [P.dma("sp", g2[r][:, :], bcast_row(modscr[l, r:r + 1, 5 * D:6 * D]), "ld7") for r in range(2)]
        P.wait("pe", evs)
        P.wait("dve", evs)
        h32_free[0] = None
        norm_mod_transpose(l, norm2_w, 3, 4, h2T, ntm, router=(rwst, rbb, LG, h32))
        ringG = Ring([0, 1])
        P.dve(lambda e: e.memset(gu_[:, :], 0.0))
        for i in range(ntm):
            P.dve(lambda e, i=i: e.max(out=m8[:, :], in_=LG[:, i, :]))
            P.dve(lambda e, i=i: e.tensor_scalar(out=msk[:, :], in0=LG[:, i, :], scalar1=m8[:, 3:4], scalar2=None, op0=ALU.is_ge))
            P.dve(lambda e: e.tensor_scalar(out=st7[:, 0:1], in0=m8[:, 0:1], scalar1=-1.0, scalar2=None, op0=ALU.mult))
            evd = P.sig_last("dve")
            P.wait("act", evd)
            P.act(lambda e, i=i: e.activation(out=ex[:, :], in_=LG[:, i, :], func=AF.Exp, bias=st7[:, 0:1]))
            eva = P.sig_last("act")
            P.wait("dve", eva)
            P.dve(lambda e: e.tensor_tensor(out=ex[:, :], in0=ex[:, :], in1=msk[:, :], op=ALU.mult))
            P.dve(lambda e: e.tensor_reduce(out=st7[:, 1:2], in_=ex[:, :], axis=AX.X, op=ALU.add))
            P.dve(lambda e: e.reciprocal(out=st7[:, 2:3], in_=st7[:, 1:2]))
            P.dve(lambda e: e.tensor_scalar(out=gu_[:, 0:NE], in0=ex[:, :], scalar1=st7[:, 2:3], scalar2=None, op0=ALU.mult))
            P.dve(lambda e, i=i: e.tensor_scalar(out=Gs[:, i, :], in0=gu_[:, 0:NE], scalar1=KSW, scalar2=None, op0=ALU.mult))
            evd = P.sig_last("dve")
            gi, gbk = ringG.next(P)
            P.wait("pe", evd)
            P.pe(lambda e, gbk=gbk: e.transpose(out=psv(gbk)[:, 0:128], in_=gu_[:, :], identity=ident_f[:, :]))
            evp = P.sig_last("pe")
            P.wait("act", evp)
            P.act(lambda e, gbk=gbk, i=i: e.copy(out=GTu[:, i, :], in_=psv(gbk)[:, 0:128]))
            eva = P.sig_last("act")
            ringG.done(gi, eva)
            P.wait("dve", eva)
        P.barrier()
        if l == 0:
            dump("gates0", Gs[:, :, :].rearrange("p i e -> p (i e)"))

        P.release(OFF_M)
        actT = P.sb([128, 8, T], BF16, "actT")
        wgu = [P.sb([128, 8, 256], BF16, f"wgu{i}") for i in range(2)]
        wdn = P.sb([128, 8, D], BF16, "wdn")
        bguT = P.sb([128, 16, NE], F32, "bguT")
        gcb = [P.sb([128, 512], F32, f"gc{i}") for i in range(2)]
        gsb = [P.sb([128, 512], F32, f"gs{i}") for i in range(2)]
        u1b = [P.sb([128, 512], F32, f"u1{i}") for i in range(2)]
        dtm = [P.sb([128, 512], F32, f"dt{i}") for i in range(2)]
        wd_u8 = wdn[:, :, :].rearrange("p k n -> p (k n)")
        bgall = wd_u8[:, 0:4096].bitcast(F32)
        bdn = wd_u8[:, 4096:6144].bitcast(F32)
        bdnb = wd_u8[:, 6144:7168]
        ringGU = Ring([0, 1, 2, 3])
        ringD = Ring([4, 5, 6, 7])
        wgu_free = [None, None]
        tmp_free = [None, None]
        dtm_free = [None, None]
        tsl_m = TSL if not last else TSL[:4]
        gq = 0
        blk = 0
        dblk = [0]

        def down_accum(lhs_fn, rhs_fn, nk, gate_fn, i_tiles):
            for i in i_tiles:
                r = 0 if i < NXT else 1
                for hf_ in range(2):
                    di, dbk = ringD.next(P)
                    for c in range(nk):
                        P.pe(lambda e, c=c, dbk=dbk, i=i, hf_=hf_: e.matmul(psv(dbk)[:, :], lhsT=lhs_fn(c, i), rhs=rhs_fn(c, hf_), start=(c == 0), stop=(c == nk - 1)))
                    evp = P.sig_last("pe")
                    s = dblk[0] % 2
                    dblk[0] += 1
                    P.wait("dve", [evp, dtm_free[s]])
                    gsc = gate_fn(i)
                    if gsc is None:
                        P.dve(lambda e, dbk=dbk, r=r, hf_=hf_, s=s: e.tensor_tensor(out=dtm[s][:, :], in0=psv(dbk)[:, :], in1=g2[r][:, hf_ * 512:(hf_ + 1) * 512], op=ALU.mult))
                    else:
                        P.dve(lambda e, dbk=dbk, r=r, hf_=hf_, s=s, gsc=gsc: e.scalar_tensor_tensor(out=dtm[s][:, :], in0=psv(dbk)[:, :], scalar=gsc, in1=g2[r][:, hf_ * 512:(hf_ + 1) * 512], op0=ALU.mult, op1=ALU.mult))
                    evd = P.sig_last("dve")
                    ringD.done(di, evd)
                    P.wait("pool", evd)
                    P.pool(lambda e, i=i, hf_=hf_, s=s: e.tensor_tensor(out=X[:, i, hf_ * 512:(hf_ + 1) * 512], in0=X[:, i, hf_ * 512:(hf_ + 1) * 512], in1=dtm[s][:, :], op=ALU.add))
                    dtm_free[s] = P.sig_last("pool")

        P.dve(lambda e: e.memset(wd_u8[:, 0:7168], 0.0))
        evz = P.sig_last("dve")
        P.wait("sp", evz)
        nwe = exp_b_gu.shape[1]
        evb = [P.dma("sp", bgall[0:nwe, :], exp_b_gu[l, :, :], "ld8b"), P.dma("sp", bdn[0:nwe, :], exp_b_down[l, :, :], "ld8b")]
        P.wait("pe", evb)
        P.wait("dve", evb)
        for c in range(16):
            P.pe(lambda e, c=c: e.transpose(out=psv(c // 4)[:, (c % 4) * 128:(c % 4 + 1) * 128], in_=bgall[:, c * 128:(c + 1) * 128], identity=ident_f[:, :]))
        evp = P.sig_last("pe")
        P.wait("dve", evp)
        for c4 in range(4):
            P.dve(lambda e, c4=c4: e.tensor_copy(out=bguT[:, 4 * c4:4 * c4 + 4, :], in_=psv(c4).rearrange("p (c x) -> p c x", c=4)[:, :, 0:NE]))
        P.dve(lambda e: e.tensor_copy(out=bdnb, in_=bdn))
        evd = P.sig_last("dve")
        P.wait("pe", evd)
        down_accum(lambda c, i: GTu[:, i, :], lambda c, hf_: bdnb[:, hf_ * 512:(hf_ + 1) * 512], 1, lambda i: None, range(ntm))
        wdn_free = P.sig_last("pe")
        act_free = None

        for ex_i in range(moe_experts):
            evwd = None
            for grp in range(8):
                ws = gq % 2
                gq += 1
                P.wait("pool", wgu_free[ws])
                evg1 = P.dma_k("pool", wgu[ws][:, :, 0:128], exp_w_gu[l, ex_i, :, grp * 128:(grp + 1) * 128].rearrange("(k p) n -> p k n", p=128), "ld8u%d_%d" % (ws, l))
                evg2 = P.dma_k("pool", wgu[ws][:, :, 128:256], exp_w_gu[l, ex_i, :, D + grp * 128:D + (grp + 1) * 128].rearrange("(k p) n -> p k n", p=128), "ld8u%d_%d" % (ws, l))
                if grp == 2:
                    P.wait("pool", wdn_free)
                    evwd = P.dma_k("pool", wdn[:, :, :], exp_w_down[l, ex_i].rearrange("(k p) n -> p k n", p=128), "ld8d")
                P.wait("pe", [evg1, evg2])
                ch = grp
                for (t0, tn) in tsl_m:
                    gi, gbk = ringGU.next(P)
                    ui, ubk = ringGU.next(P)
                    for k in range(8):
                        P.pe(lambda e, k=k, gbk=gbk, ws=ws, t0=t0, tn=tn: e.matmul(psv(gbk)[:, 0:tn], lhsT=wgu[ws][:, k, 0:128], rhs=h2T[:, k, t0:t0 + tn], start=(k == 0), stop=(k == 7)))
                    for k in range(8):
                        P.pe(lambda e, k=k, ubk=ubk, ws=ws, t0=t0, tn=tn: e.matmul(psv(ubk)[:, 0:tn], lhsT=wgu[ws][:, k, 128:256], rhs=h2T[:, k, t0:t0 + tn], start=(k == 0), stop=(k == 7)))
                    evp = P.sig_last("pe")
                    s = blk % 2
                    blk += 1
                    P.wait("dve", [evp, tmp_free[s], act_free])
                    P.wait("act", [evp, tmp_free[s]])
                    P.dve(lambda e, gbk=gbk, s=s, ch=ch, tn=tn, ex_i=ex_i: e.tensor_scalar(out=gcb[s][:, 0:tn], in0=psv(gbk)[:, 0:tn], scalar1=bguT[:, ch, ex_i:ex_i + 1], scalar2=7.0, op0=ALU.add, op1=ALU.min))
                    evd1 = P.sig_last("dve")
                    P.act(lambda e, ubk=ubk, s=s, ch=ch, tn=tn, ex_i=ex_i: e.activation(out=u1b[s][:, 0:tn], in_=psv(ubk)[:, 0:tn], func=AF.Identity, bias=bguT[:, 8 + ch, ex_i:ex_i + 1]))
                    P.wait("act", evd1)
                    P.act(lambda e, s=s, tn=tn: e.activation(out=gsb[s][:, 0:tn], in_=gcb[s][:, 0:tn], func=AF.Silu, scale=1.702))
                    eva = P.sig_last("act")
                    ringGU.done(gi, evd1)
                    ringGU.done(ui, eva)
                    P.wait("dve", eva)
                    P.dve(lambda e, s=s, tn=tn: e.tensor_scalar(out=u1b[s][:, 0:tn], in0=u1b[s][:, 0:tn], scalar1=-7.0, scalar2=7.0, op0=ALU.max, op1=ALU.min))
                    P.dve(lambda e, s=s, ch=ch, t0=t0, tn=tn: e.scalar_tensor_tensor(out=actT[:, ch, t0:t0 + tn], in0=u1b[s][:, 0:tn], scalar=1.0, in1=gsb[s][:, 0:tn], op0=ALU.add, op1=ALU.mult))
                    tmp_free[s] = P.sig_last("dve")
                wgu_free[ws] = P.sig_last("pe")
            act_written = P.sig_last("dve")
            P.wait("pe", [act_written, evwd])
            down_accum(lambda c, i: actT[:, c, i * 128:(i + 1) * 128], lambda c, hf_: wdn[:, c, hf_ * 512:(hf_ + 1) * 512], 8,
                       lambda i, ex_i=ex_i: Gs[:, i, ex_i:ex_i + 1], range(ntm))
            wdn_free = P.sig_last("pe")
            act_free = wdn_free
        P.barrier()
        if l == 0:
            dump("x_l0", X[:, :, :].rearrange("p i d -> p (i d)"))

    return finish_prog()


def _consts():
    ident = np.eye(128, dtype=np.float32)
    t = np.arange(L)
    row = (t // 64).astype(np.float32)
    col = (t % 64).astype(np.float32)
    inv = (10000.0 ** (-np.arange(0, 32, 2, dtype=np.float32) / 32)).astype(np.float32)
    ang = np.concatenate([row[:, None] * inv, row[:, None] * inv, col[:, None] * inv, col[:, None] * inv], axis=-1).astype(np.float32)
    cos = np.cos(ang).astype(np.float32)
    sin = np.sin(ang).astype(np.float32)
    sgn = np.concatenate([-np.ones(16), np.ones(16), -np.ones(16), np.ones(16)]).astype(np.float32)
    rope = np.concatenate([cos, sin * sgn[None, :]], axis=-1).reshape(NXT, 128, 128).transpose(1, 0, 2)
    j = np.arange(128)[:, None]
    i = np.arange(128)[None, :]
    mask = np.stack([(i <= j), (j <= i)], axis=1).astype(np.float32)
    fix = np.ones((128, 2, 2, 8), np.float32)
    pinv = np.zeros((128, 2), np.float32)
    for p in range(128):
        for ct in range(2):
            w = POOLW[2 * ct + p // 64]
            pinv[p, ct] = 1.0 / w
            for tt in range(w // 2):
                fix[p, ct, 0, tt] = 1.0 / (tt + w // 2)
                fix[p, ct, 1, tt] = 1.0 / (w - tt)
    return {"c_ident": ident, "c_rope": np.ascontiguousarray(rope, dtype=np.float32), "c_mask": np.ascontiguousarray(mask),
            "c_poolfix": fix, "c_poolinv": pinv}


WEIGHT_KEYS = ["w_mod", "b_mod", "norm1_w", "norm2_w", "w_in", "q_norm_w", "k_norm_w", "attn_sink", "ssm_a_re", "ssm_a_im",
               "ssm_log_dt", "ssm_b_re", "ssm_b_im", "ssm_c_re", "ssm_c_im", "ssm_d", "glu_w", "glu_b", "pool_w", "pool_scale",
               "out_norm_w", "w_out", "router_w", "router_b", "exp_w_gu", "exp_b_gu", "exp_w_down", "exp_b_down"]


def make_in_map(inputs, b, consts, wd=DEPTH, we=NE):
    m = {k: np.ascontiguousarray(np.asarray(inputs[k][:wd, :we] if k.startswith('exp_') else inputs[k][:wd], dtype=np.float32)) for k in WEIGHT_KEYS}
    m["x"] = np.ascontiguousarray(np.asarray(inputs["x"][b], dtype=np.float32))
    m["ctx"] = np.ascontiguousarray(np.asarray(inputs["ctx"][b], dtype=np.float32))
    m["cc"] = np.ascontiguousarray(np.stack([np.asarray(inputs["c"][b]), np.asarray(inputs["c_ctx"])]).astype(np.float32))
    m.update(consts)
    return m


def kernel(**inputs):
    nc = build_program()
    consts = _consts()
    in_maps = [make_in_map(inputs, b, consts) for b in range(8)]
    res = run_bass_kernel_spmd(nc, in_maps, core_ids=list(range(8)))
    return np.stack([np.asarray(r["out"], dtype=np.float32) for r in res.results], axis=0)
```

```python
from contextlib import ExitStack
import math
import numpy as np
import ml_dtypes
import concourse.bass as bass
import concourse.mybir as mybir
from concourse.bass_utils import run_bass_kernel_spmd

F32 = mybir.dt.float32
BF16 = mybir.dt.bfloat16
AF = mybir.ActivationFunctionType
ALU = mybir.AluOpType
AX = mybir.AxisListType

D = 1024
L = 2048
LC = 256
T = L + LC
NT = T // 128
NXT = L // 128
DEPTH = 4
NE = 32
EPS = 1e-6
ATT_SCALE = 64 ** -0.5
KSW = 1.0 / 1.702
ENGS = ("pe", "act", "dve", "pool", "sp")
TSL = [(0, 512), (512, 512), (1024, 512), (1536, 512), (2048, 256)]
POOLW = (2, 4, 8, 16)


class _Probe:
    def __init__(self):
        self.outs = []

    def __getattr__(self, name):
        def f(*a, **kw):
            o = kw.get("out", None)
            if o is None and a:
                o = a[0]
            self.outs.append(o)
            if kw.get("accum_out", None) is not None:
                self.outs.append(kw["accum_out"])
            return self
        return f

    def small(self):
        for o in self.outs:
            try:
                n = int(np.prod(o.shape[1:]))
            except Exception:
                n = 1 << 20
            if n < 128:
                return True
        return False


class Prog:
    def __init__(self, nc):
        self.nc = nc
        self.q = {e: [] for e in ENGS}
        self.cnt = {e: 0 for e in ENGS}
        self.waited = {e: {} for e in ENGS}
        self.sems = {}
        self.dcnt = {}
        self.last_sig = {}
        self.epoch = 0
        self.stack = ExitStack()
        self.sb_off = 16512
        self.sb_n = 0

    def sb(self, shape, dtype, name=None):
        nbytes = int(np.prod(shape[1:])) * mybir.dt.size(dtype)
        nbytes = (nbytes + 31) // 32 * 32
        off = self.sb_off
        self.sb_off += nbytes
        assert self.sb_off <= 229376, ("SBUF overflow", self.sb_off, name)
        self.sb_n += 1
        return self.nc.alloc_sbuf_tensor_at(f"sb{self.sb_n}_{name or ''}", list(shape), dtype, offset=off)

    def mark(self):
        return self.sb_off

    def release(self, m):
        self.sb_off = m

    def sem(self, name):
        if name not in self.sems:
            self.sems[name] = self.stack.enter_context(self.nc.semaphore(name))
        return self.sems[name]

    def emit(self, eng, fn, sig=False):
        self.fence_last[eng] = False
        ent = [fn, None]
        self.q[eng].append(ent)
        if eng in ("act", "dve", "pool"):
            pr = _Probe()
            fn(pr)
            if pr.small():
                ev = self._sig(eng, ent)
                sm = self.sem(ev[3])
                self.q[eng].append([lambda e, sm=sm, v=ev[2]: e.wait_ge(sm, v), None, "wait"])
                return ev
        if sig:
            return self._sig(eng, ent)
        return None

    def _sig(self, eng, ent):
        self.cnt[eng] += 1
        nm = "p_%s_%d" % (eng, self.epoch)
        ent[1] = self.sem(nm)
        self.last_sig[eng] = ("e", eng, self.cnt[eng], nm)
        return self.last_sig[eng]

    def init_fences(self):
        self.fz = {e: self.sb([128, 64], F32, "fz_" + e) for e in ("act", "dve", "pool")}
        self.fence_last = {e: False for e in ENGS}

    def sig_last(self, eng):
        last = None
        for ent in reversed(self.q[eng]):
            if len(ent) == 3:
                continue
            last = ent
            break
        if last is None:
            return None
        if eng in self.fz:
            if self.fence_last[eng] and eng in self.last_sig:
                return self.last_sig[eng]
            fz = self.fz[eng]
            if eng == "act":
                ent = [lambda e: e.copy(out=fz[:, :], in_=fz[:, :]), None]
            else:
                ent = [lambda e: e.tensor_copy(out=fz[:, :], in_=fz[:, :]), None]
            self.q[eng].append(ent)
            self.fence_last[eng] = True
            return self._sig(eng, ent)
        if last[1] is None:
            return self._sig(eng, last)
        if eng in self.last_sig:
            return self.last_sig[eng]
        return None

    def wait(self, eng, ev):
        if ev is None:
            return
        if isinstance(ev, list):
            for x in ev:
                self.wait(eng, x)
            return
        kind, key, val = ev[0], ev[1], ev[2]
        if kind == "e" and key == eng:
            return
        semname = ev[3] if kind == "e" else key
        if self.waited[eng].get(semname, 0) >= val:
            return
        self.waited[eng][semname] = val
        s = self.sem(semname)
        self.q[eng].append([lambda e, s=s, val=val: e.wait_ge(s, val), None, "wait"])

    def dma(self, qeng, out, in_, semname, **kw):
        s = self.sem(semname)
        self.dcnt[semname] = self.dcnt.get(semname, 0) + 16
        self.q[qeng].append([lambda e, out=out, in_=in_, s=s, kw=kw: e.dma_start(out=out, in_=in_, **kw).then_inc(s, 16), None, "dma"])
        return ("d", semname, self.dcnt[semname])

    def dma_k(self, qeng, out, in_, semname, **kw):
        ev = None
        for k in range(out.shape[1]):
            ev = self.dma(qeng, out[:, k, :], in_[:, k, :], semname, **kw)
        return ev

    def new_epoch(self):
        self.barrier()
        self.epoch += 1
        self.cnt = {e: 0 for e in ENGS}
        self.fence_last = {e: False for e in ENGS}
        self.last_sig = {}

    def barrier(self, engs=ENGS):
        evs = [self.sig_last(e) for e in engs]
        for e in engs:
            self.wait(e, evs)

    def pe(self, fn, sig=False):
        return self.emit("pe", fn, sig)

    def act(self, fn, sig=False):
        return self.emit("act", fn, sig)

    def dve(self, fn, sig=False):
        return self.emit("dve", fn, sig)

    def pool(self, fn, sig=False):
        return self.emit("pool", fn, sig)

    def finish(self):
        nc = self.nc
        q = self.q

        def run(e, lst):
            for ent in lst:
                ins = ent[0](e)
                if ent[1] is not None:
                    ins.then_inc(ent[1], 1)

        with nc.Block() as block:
            @block.tensor
            def _(e):
                run(e, q["pe"])

            @block.scalar
            def _(e):
                run(e, q["act"])

            @block.vector
            def _(e):
                run(e, q["dve"])

            @block.gpsimd
            def _(e):
                run(e, q["pool"])

            @block.sync
            def _(e):
                run(e, q["sp"])
        self.stack.close()


class Ring:
    def __init__(self, banks):
        self.banks = banks
        self.free = [None] * len(banks)
        self.i = 0

    def next(self, P, eng="pe"):
        i = self.i
        self.i = (self.i + 1) % len(self.banks)
        P.wait(eng, self.free[i])
        return i, self.banks[i]

    def done(self, i, ev):
        self.free[i] = ev


def bcast_row(ap_row, n=128):
    return ap_row.partition_broadcast(n)[:, 0, :]


def build_program(n_layers=DEPTH, dbg=None, moe_experts=NE, stop=None, we=NE):
    dbg = dbg or {}
    nc = bass.Bass("TRN2", target_bir_lowering=False)
    P = Prog(nc)

    def din(name, shape, dt=F32):
        return nc.dram_tensor(name, list(shape), dt, kind="ExternalInput").ap()

    WD = n_layers
    x_in = din("x", [L, D])
    ctx_in = din("ctx", [LC, D])
    cc_in = din("cc", [2, D])
    w_mod = din("w_mod", [WD, D, 6 * D])
    b_mod = din("b_mod", [WD, 6 * D])
    norm1_w = din("norm1_w", [WD, D])
    norm2_w = din("norm2_w", [WD, D])
    w_in = din("w_in", [WD, D, 1280])
    q_norm_w = din("q_norm_w", [WD, 64])
    k_norm_w = din("k_norm_w", [WD, 64])
    attn_sink = din("attn_sink", [WD, 8])
    ssm_a_re = din("ssm_a_re", [WD, 2, 16, 64])
    ssm_a_im = din("ssm_a_im", [WD, 2, 16, 64])
    ssm_log_dt = din("ssm_log_dt", [WD, 2, 16])
    ssm_b_re = din("ssm_b_re", [WD, 2, 16, 64, 16])
    ssm_b_im = din("ssm_b_im", [WD, 2, 16, 64, 16])
    ssm_c_re = din("ssm_c_re", [WD, 2, 16, 16, 64])
    ssm_c_im = din("ssm_c_im", [WD, 2, 16, 16, 64])
    ssm_d = din("ssm_d", [WD, 256])
    glu_w = din("glu_w", [WD, 256, 256])
    glu_b = din("glu_b", [WD, 256])
    pool_w = din("pool_w", [WD, 4, 64, 64])
    pool_scale = din("pool_scale", [WD, 256])
    out_norm_w = din("out_norm_w", [WD, 768])
    w_out = din("w_out", [WD, D, D])
    router_w = din("router_w", [WD, D, NE])
    router_b = din("router_b", [WD, NE])
    exp_w_gu = din("exp_w_gu", [WD, we, D, 2 * D])
    exp_b_gu = din("exp_b_gu", [WD, we, 2 * D])
    exp_w_down = din("exp_w_down", [WD, we, D, D])
    exp_b_down = din("exp_b_down", [WD, we, D])
    c_ident = din("c_ident", [128, 128])
    c_rope = din("c_rope", [128, NXT, 128])
    c_mask = din("c_mask", [128, 2, 128])
    c_poolfix = din("c_poolfix", [128, 2, 2, 8])
    c_poolinv = din("c_poolinv", [128, 2])
    out = nc.dram_tensor("out", [L, D], F32, kind="ExternalOutput").ap()
    modscr = nc.dram_tensor("modscr", [DEPTH, 2, 6 * D], F32, kind="Internal").ap()
    dbg_out = {k: nc.dram_tensor("dbg_" + k, list(shp), F32, kind="ExternalOutput").ap() for k, shp in dbg.items()}

    psb = [nc.alloc_psum_tensor(f"ps{i}", [128, 512], F32) for i in range(8)]

    def psv(i, dt=F32):
        return psb[i][:, :] if dt == F32 else psb[i][:, :].bitcast(dt)

    P.init_fences()
    X = P.sb([128, NT, D], F32, "X")
    ident_f = P.sb([128, 128], F32, "identf")
    ident_b = P.sb([128, 128], BF16, "identb")
    ones_b = P.sb([128, 128], BF16, "onesb")
    masks = P.sb([128, 2, 128], BF16, "masks")
    poolfix = P.sb([128, 2, 2, 8], F32, "poolfix")
    poolinv = P.sb([128, 2], F32, "poolinv")
    epsc = P.sb([128, 1], F32, "epsc")
    halfpi = P.sb([128, 1], F32, "halfpi")
    base_mark = P.mark()

    def dump(name, src_ap, dst_ap=None):
        if name not in dbg_out:
            return
        P.barrier()
        ev = P.dma("pool", dbg_out[name] if dst_ap is None else dst_ap, src_ap, "dbgsem")
        for en in ENGS:
            P.wait(en, ev)
        P.barrier()

    def finish_prog():
        P.barrier()
        evs_o = [P.dma("sp", out[i * 128:(i + 1) * 128, :], X[:, i, :], "st_out") for i in range(NXT)]
        P.wait("sp", evs_o)
        P.finish()
        return nc

    m0 = P.mark()
    mstage = P.sb([128, 2, 128], F32, "mstage")
    evs = [P.dma("sp", ident_f[:, :], c_ident[:, :], "ld0"),
           P.dma("sp", mstage[:, :, :], c_mask[:, :, :], "ld0"),
           P.dma("sp", poolfix[:, :, :, :], c_poolfix[:, :, :, :], "ld0"),
           P.dma("sp", poolinv[:, :], c_poolinv[:, :], "ld0")]
    for i in range(NXT):
        evs.append(P.dma("sp", X[:, i, :], x_in[i * 128:(i + 1) * 128, :], "ld0"))
    for i in range(2):
        evs.append(P.dma("sp", X[:, NXT + i, :], ctx_in[i * 128:(i + 1) * 128, :], "ld0"))
    P.wait("dve", evs)
    for fe, ft in P.fz.items():
        P.emit(fe if fe != "act" else "dve", lambda e, ft=ft: e.memset(ft[:, :], 0.0))
    P.dve(lambda e: e.tensor_copy(out=ident_b[:, :], in_=ident_f[:, :]))
    P.dve(lambda e: e.tensor_copy(out=masks[:, :, :], in_=mstage[:, :, :]))
    P.dve(lambda e: e.memset(ones_b[:, :], 1.0))
    P.dve(lambda e: e.memset(epsc[:, :], EPS))
    P.dve(lambda e: e.memset(halfpi[:, :], math.pi / 2))
    P.barrier()
    P.release(m0)

    if stop == 'S0':
        return finish_prog()
    m0 = P.mark()
    ccs = P.sb([128, D], F32, "ccs")
    sil = P.sb([128, 8, 128], F32, "sil")
    wst = [P.sb([128, 8, 512], F32, f"wst{i}") for i in range(2)]
    brow = P.sb([2, 6 * D], F32, "brow")
    mrow = P.sb([2, 512], F32, "mrow")
    P.dve(lambda e: e.memset(ccs[:, :], 0.0))
    P.dve(lambda e: e.memset(sil[:, :, :], 0.0))
    evz = P.sig_last("dve")
    P.wait("sp", evz)
    ev = P.dma("sp", ccs[0:2, :], cc_in[:, :], "ld1")
    P.wait("act", ev)
    P.act(lambda e: e.activation(out=ccs[0:2, :], in_=ccs[0:2, :], func=AF.Silu))
    ev_a = P.sig_last("act")
    P.wait("pe", ev_a)
    for k in range(8):
        P.pe(lambda e, k=k: e.transpose(out=psv(k // 4)[:, (k % 4) * 128:(k % 4 + 1) * 128], in_=ccs[:, k * 128:(k + 1) * 128], identity=ident_f[:, :]))
    ev = P.sig_last("pe")
    P.wait("dve", ev)
    for hh in range(2):
        P.dve(lambda e, hh=hh: e.tensor_copy(out=sil[:, 4 * hh:4 * hh + 4, 0:2], in_=psv(hh).rearrange("p (k c) -> p k c", k=4)[:, :, 0:2]))
    ev_sil = P.sig_last("dve")
    P.wait("pe", ev_sil)
    if stop == 'S1a':
        return finish_prog()
    ring = Ring([2, 3])
    wfree = [None, None]
    mrow_free = None
    it = 0
    for l in range(n_layers):
        P.wait("sp", mrow_free)
        evb0 = [P.dma("sp", brow[r:r + 1, :], b_mod[l:l + 1, :], "ld1b") for r in range(2)]
        for ct in range(12):
            s = it % 2
            it += 1
            P.wait("sp", wfree[s])
            evw = P.dma_k("sp", wst[s][:, :, :], w_mod[l, :, ct * 512:(ct + 1) * 512].rearrange("(k p) n -> p k n", p=128), f"ld1w{s}")
            bi, b = ring.next(P)
            P.wait("pe", evw)
            for k in range(8):
                P.pe(lambda e, k=k, s=s, b=b: e.matmul(psv(b)[:, :], lhsT=sil[:, k, :], rhs=wst[s][:, k, :], start=(k == 0), stop=(k == 7)))
            evp = P.sig_last("pe")
            wfree[s] = evp
            P.wait("dve", [evp, mrow_free] + evb0)
            P.dve(lambda e, b=b, ct=ct: e.tensor_tensor(out=mrow[0:2, :], in0=psv(b)[0:2, :], in1=brow[0:2, ct * 512:(ct + 1) * 512], op=ALU.add))
            evd = P.sig_last("dve")
            ring.done(bi, evd)
            P.wait("sp", evd)
            e0 = P.dma("sp", modscr[l, 0:2, ct * 512:(ct + 1) * 512], mrow[0:2, :], "st1")
            P.wait("sp", e0)
            mrow_free = e0
    P.barrier()
    P.release(m0)

    if stop == 'S1':
        return finish_prog()
    def norm_mod_transpose(l, nw, shift_idx, scale_idx, hT, ntiles, router=None):
        m = P.mark()
        A = [P.sb([128, D], F32, "A0"), P.sb([128, D], F32, "A1")]
        S = [P.sb([128, D], F32, "S0"), P.sb([128, D], F32, "S1")]
        nwb = P.sb([128, D], F32, "nwb")
        junk = P.sb([128, D], BF16, "junk")
        hf = [P.sb([128, D], F32, f"hf{i}") for i in range(2)]
        st = P.sb([128, NT, 4], F32, "st")
        evs = [P.dma("sp", nwb[:, :], bcast_row(nw[l:l + 1, :]), "ldn")]
        for r in range(2):
            evs.append(P.dma("sp", A[r][:, :], bcast_row(modscr[l, r:r + 1, scale_idx * D:(scale_idx + 1) * D]), "ldn"))
            evs.append(P.dma("sp", S[r][:, :], bcast_row(modscr[l, r:r + 1, shift_idx * D:(shift_idx + 1) * D]), "ldn"))
        P.wait("dve", evs)
        for r in range(2):
            P.dve(lambda e, r=r: e.scalar_tensor_tensor(out=A[r][:, :], in0=A[r][:, :], scalar=1.0, in1=nwb[:, :], op0=ALU.add, op1=ALU.mult))
        if router is not None:
            rw32, rbb, LG, h32 = router
        ring_t = Ring([0, 2])
        hfree = [None, None]
        lg_ring = Ring([6, 7])
        for i in range(ntiles):
            r = 0 if i < NXT else 1
            s = i % 2
            P.act(lambda e, i=i: e.activation(out=junk[:, :], in_=X[:, i, :], func=AF.Square, accum_out=st[:, i, 0:1]))
            P.act(lambda e, i=i: e.activation(out=st[:, i, 1:2], in_=st[:, i, 0:1], func=AF.Sqrt, bias=epsc[:, 0:1], scale=1.0 / D))
            eva = P.sig_last("act")
            P.wait("dve", [eva, hfree[s]])
            P.dve(lambda e, i=i: e.reciprocal(out=st[:, i, 2:3], in_=st[:, i, 1:2]))
            P.dve(lambda e, i=i, r=r, s=s: e.scalar_tensor_tensor(out=hf[s][:, :], in0=X[:, i, :], scalar=st[:, i, 2:3], in1=A[r][:, :], op0=ALU.mult, op1=ALU.mult))
            P.dve(lambda e, r=r, s=s: e.tensor_tensor(out=hf[s][:, :], in0=hf[s][:, :], in1=S[r][:, :], op=ALU.add))
            evd = P.sig_last("dve")
            bi, b = ring_t.next(P)
            P.wait("pe", evd)
            for k in range(8):
                P.pe(lambda e, k=k, b=b, s=s: e.transpose(out=psv(b + k // 4)[:, (k % 4) * 128:(k % 4 + 1) * 128], in_=hf[s][:, k * 128:(k + 1) * 128], identity=ident_f[:, :]))
            evp = P.sig_last("pe")
            hfree[s] = evp
            P.wait("act", evp)
            for hh in range(2):
                P.act(lambda e, b=b, hh=hh, i=i: e.copy(out=hT[:, 4 * hh:4 * hh + 4, i * 128:(i + 1) * 128],
                                                       in_=psv(b + hh).rearrange("p (k t) -> p k t", k=4)))
            if router is not None:
                P.wait("act", h32_free[0])
                for hh in range(2):
                    P.act(lambda e, b=b, hh=hh: e.copy(out=h32[:, 4 * hh:4 * hh + 4, :], in_=psv(b + hh).rearrange("p (k t) -> p k t", k=4)))
            evc = P.sig_last("act")
            ring_t.done(bi, evc)
            if router is not None:
                li, lb = lg_ring.next(P)
                P.wait("pe", evc)
                for k in range(8):
                    P.pe(lambda e, k=k, lb=lb: e.matmul(psv(lb)[:, 0:NE], lhsT=h32[:, k, :], rhs=rw32[:, k, :], start=(k == 0), stop=(k == 7)))
                evl = P.sig_last("pe")
                h32_free[0] = evl
                P.wait("dve", evl)
                P.dve(lambda e, lb=lb, i=i: e.tensor_tensor(out=LG[:, i, :], in0=psv(lb)[:, 0:NE], in1=rbb[:, :], op=ALU.add))
                lg_ring.done(li, P.sig_last("dve"))
        P.barrier()
        P.release(m)

    h32_free = [None]
    pl_free = [None]
    pool_ev = [None]
    dve_step_ev = [None]

    R1 = (P.mark() + 31) // 32 * 32
    R2 = R1 + 36864
    R3 = R2 + 9216
    PW = 8 + L + 8 + 8 + LC + 8
    R4 = R3 + 2 * PW * 4
    R5 = R4 + 18432 + 18432 + 4704
    for l in range(n_layers):
        last = l == DEPTH - 1
        P.new_epoch()
        P.release(R1)
        hT = P.sb([128, 8, T], BF16, "hT")
        mergedT = hT
        uT = P.sb([128, 2, T], BF16, "uT")
        poolP = P.sb([128, 2, PW], F32, "poolP")
        qT = P.sb([128, 4, T], BF16, "qT")
        kT2 = P.sb([128, 2, 2, T], BF16, "kT2")
        Vaug = P.sb([128, NT, 2, 65], BF16, "Vaug")
        assert P.mark() <= R5, (P.mark(), R5)
        P.release(R4)
        norm_mod_transpose(l, norm1_w, 0, 1, hT, NT)
        if l == 0:
            dump("hT", hT[:, :, :].rearrange("p k t -> p (k t)"))
        if stop == 'L1':
            return finish_prog()
        P.release(R5)
        win = P.sb([128, 8, 768], BF16, "win")
        ropet = [P.sb([128, 128], F32, f"ropet{i}") for i in range(2)]
        NW = P.sb([128, 10, 64], F32, "NW")
        QK = [P.sb([128, 1024], BF16, f"QK{i}") for i in range(2)]
        tq = [P.sb([128, 640], F32, f"tq{i}") for i in range(2)]
        tr = P.sb([128, 640], F32, "tr")
        ss = P.sb([128, NT, 32], F32, "ss")
        evw = P.dma_k("pool", win[:, :, :], w_in[l, :, 0:768].rearrange("(k p) n -> p k n", p=128), "ldw_in")
        evr = []
        for h in range(10):
            src = q_norm_w if h < 8 else k_norm_w
            evr.append(P.dma("sp", NW[:, h, :], bcast_row(src[l:l + 1, :]), "ld2"))
        P.dve(lambda e: e.memset(Vaug[:, :, :, 64:65], 1.0))
        P.dve(lambda e: e.memset(poolP[:, :, :], 0.0))
        for qq in QK:
            P.dve(lambda e, qq=qq: e.memset(qq[:, :], 0.0))
        P.wait("pe", evw)
        P.wait("dve", evr)
        ringA = Ring([0, 1])
        ringB = Ring([2, 3])
        ringT = Ring([4, 5])
        qkfree = [None, None]
        ropefree = [None, None]
        for i in range(NT):
            s = i % 2
            if i < NXT:
                P.wait("sp", ropefree[s])
                evrope = P.dma("sp", ropet[s][:, :], c_rope[:, i, :], "ld2r%d" % s)
            ai, a = ringA.next(P)
            bi, b = ringB.next(P)
            for k in range(8):
                P.pe(lambda e, k=k, a=a, i=i: e.matmul(psv(a)[:, :], lhsT=hT[:, k, i * 128:(i + 1) * 128], rhs=win[:, k, 0:512], start=(k == 0), stop=(k == 7)))
            for k in range(8):
                P.pe(lambda e, k=k, b=b, i=i: e.matmul(psv(b)[:, 0:256], lhsT=hT[:, k, i * 128:(i + 1) * 128], rhs=win[:, k, 512:768], start=(k == 0), stop=(k == 7)))
            evp = P.sig_last("pe")
            P.wait("act", evp)
            P.act(lambda e, b=b, i=i: e.copy(out=Vaug[:, i, :, 0:64], in_=psv(b)[:, 128:256].rearrange("p (g d) -> p g d", g=2)))
            P.wait("dve", [evp, qkfree[s]])
            t = tq[s]
            P.dve(lambda e, a=a, t=t: e.tensor_copy(out=t[:, 0:512], in_=psv(a)[:, :]))
            P.dve(lambda e, b=b, t=t: e.tensor_copy(out=t[:, 512:640], in_=psv(b)[:, 0:128]))
            evcp = P.sig_last("dve")
            P.dve(lambda e, t=t: e.tensor_tensor(out=tr[:, :], in0=t[:, :], in1=t[:, :], op=ALU.mult))
            P.dve(lambda e, i=i: e.tensor_reduce(out=ss[:, i, 0:10], in_=tr[:, :].rearrange("p (h d) -> p h d", d=64), axis=AX.X, op=ALU.add))
            evd = P.sig_last("dve")
            P.wait("act", evd)
            P.act(lambda e, i=i: e.activation(out=ss[:, i, 10:20], in_=ss[:, i, 0:10], func=AF.Sqrt, bias=epsc[:, 0:1], scale=1.0 / 64))
            eva = P.sig_last("act")
            ringA.done(ai, evcp)
            ringB.done(bi, eva)
            P.wait("dve", eva)
            P.dve(lambda e, i=i: e.reciprocal(out=ss[:, i, 20:30], in_=ss[:, i, 10:20]))
            t3 = t[:, :].rearrange("p (h d) -> p h d", d=64)
            P.dve(lambda e, i=i, t3=t3: e.tensor_tensor(out=t3, in0=t3, in1=ss[:, i, 20:30].unsqueeze(2).broadcast_to([128, 10, 64]), op=ALU.mult))
            P.dve(lambda e, t3=t3: e.tensor_tensor(out=t3, in0=t3, in1=NW[:, :, :], op=ALU.mult))
            qk = QK[s]
            if i < NXT:
                P.wait("dve", evrope)
                rp = ropet[s]
                t5 = t[:, :].rearrange("p (h x f d) -> p h x f d", h=10, x=2, f=2)
                r5 = tr[:, :].rearrange("p (h x f d) -> p h x f d", h=10, x=2, f=2)
                sn = rp[:, 64:128].rearrange("p (x f d) -> p x f d", x=2, f=2)
                cs = rp[:, 0:64]
                for f in range(2):
                    P.dve(lambda e, f=f, t5=t5, r5=r5, sn=sn: e.tensor_tensor(out=r5[:, :, :, f, :], in0=t5[:, :, :, 1 - f, :],
                                                                             in1=sn[:, :, f, :].unsqueeze(1).broadcast_to([128, 10, 2, 16]), op=ALU.mult))
                P.dve(lambda e, t3=t3, cs=cs: e.tensor_tensor(out=t3, in0=t3, in1=cs.unsqueeze(1).broadcast_to([128, 10, 64]), op=ALU.mult))
                P.dve(lambda e, t=t, qk=qk: e.tensor_tensor(out=qk[:, 0:512], in0=t[:, 0:512], in1=tr[:, 0:512], op=ALU.add))
                for g in range(2):
                    for dup in range(2):
                        P.dve(lambda e, t=t, qk=qk, g=g, dup=dup: e.tensor_tensor(out=qk[:, 512 + g * 256 + dup * 192:512 + g * 256 + dup * 192 + 64],
                                                                               in0=t[:, 512 + g * 64:576 + g * 64], in1=tr[:, 512 + g * 64:576 + g * 64], op=ALU.add))
                ropefree[s] = P.sig_last("dve")
            else:
                P.dve(lambda e, t=t, qk=qk: e.tensor_copy(out=qk[:, 0:512], in_=t[:, 0:512]))
                for g in range(2):
                    for dup in range(2):
                        P.dve(lambda e, t=t, qk=qk, g=g, dup=dup: e.tensor_copy(out=qk[:, 512 + g * 256 + dup * 192:512 + g * 256 + dup * 192 + 64],
                                                                             in_=t[:, 512 + g * 64:576 + g * 64]))
            evq = P.sig_last("dve")
            ti, tb = ringT.next(P)
            P.wait("pe", evq)
            tp = psv(tb, BF16)
            for c in range(8):
                P.pe(lambda e, c=c, tp=tp, qk=qk: e.transpose(out=tp[:, c * 128:(c + 1) * 128], in_=qk[:, c * 128:(c + 1) * 128], identity=ident_b[:, :]))
            evt = P.sig_last("pe")
            qkfree[s] = evt
            P.wait("act", evt)
            P.act(lambda e, tp=tp, i=i: e.copy(out=qT[:, :, i * 128:(i + 1) * 128], in_=tp[:, 0:512].rearrange("p (c t) -> p c t", c=4)))
            P.act(lambda e, tp=tp, i=i: e.copy(out=kT2[:, :, :, i * 128:(i + 1) * 128], in_=tp[:, 512:1024].rearrange("p (g h t) -> p g h t", g=2, h=2)))
            ringT.done(ti, P.sig_last("act"))
        evpe = P.sig_last("pe")
        P.wait("pool", evpe)
        evw = P.dma_k("pool", win[:, :, 0:512], w_in[l, :, 768:1280].rearrange("(k p) n -> p k n", p=128), "ldw_in")
        P.wait("pe", evw)
        ringU = Ring([6, 7])
        for m_ in range(4):
            for (t0, tn) in TSL:
                ui, ub = ringU.next(P)
                for k in range(8):
                    P.pe(lambda e, k=k, ub=ub, m_=m_, t0=t0, tn=tn: e.matmul(psv(ub)[:, 0:tn], lhsT=win[:, k, m_ * 128:(m_ + 1) * 128],
                                                                          rhs=hT[:, k, t0:t0 + tn], start=(k == 0), stop=(k == 7)))
                evp = P.sig_last("pe")
                P.wait("act", evp)
                if m_ < 2:
                    P.act(lambda e, ub=ub, m_=m_, t0=t0, tn=tn: e.copy(out=uT[:, m_, t0:t0 + tn], in_=psv(ub)[:, 0:tn]))
                else:
                    po = 8 + t0 if t0 < L else 8 + L + 8 + 8
                    P.act(lambda e, ub=ub, m_=m_, po=po, tn=tn: e.copy(out=poolP[:, m_ - 2, po:po + tn], in_=psv(ub)[:, 0:tn]))
                ringU.done(ui, P.sig_last("act"))
        P.barrier()
        if l == 0:
            dump("qT", qT[:, :, :].rearrange("p k t -> p (k t)"))
            dump("kT2", kT2[:, :, :, :].rearrange("p g h t -> p (g h t)"))
            dump("uT", uT[:, :, :].rearrange("p k t -> p (k t)"))
            dump("poolP", poolP[:, :, :].rearrange("p k t -> p (k t)"))

        if stop == 'L2':
            return finish_prog()
        P.release(R5)
        esink = P.sb([128, 8], F32, "esink")
        ONW = P.sb([128, 512], F32, "ONW")
        PT = [P.sb([128, 512], BF16, f"PT{i}") for i in range(6)]
        Otm = P.sb([128, 512], F32, "Otm")
        Ob = P.sb([128, 512], BF16, "Ob")
        junk3 = P.sb([128, 512], BF16, "junk3")
        den = P.sb([128, 8], F32, "den")
        st3 = P.sb([128, NT, 4], F32, "st3")
        ev1 = P.dma("sp", esink[:, :], bcast_row(attn_sink[l:l + 1, :]), "ld3")
        ev2 = P.dma("sp", ONW[:, :], bcast_row(out_norm_w[l:l + 1, 0:512]), "ld3")
        P.wait("act", ev1)
        P.act(lambda e: e.activation(out=esink[:, :], in_=esink[:, :], func=AF.Exp))
        ev_es = P.sig_last("act")
        P.wait("dve", [ev_es, ev2])
        ringS = Ring([0, 1, 2])
        ringO = Ring([3, 4])
        ringT = Ring([5, 6])
        ptfree = [None] * 6
        pti = 0
        o_free = None
        ob_free = None
        qtiles = list(range(NXT)) + ([] if last else [NXT, NXT + 1])
        for n in qtiles:
            if n < NXT:
                kbs = ([(n - 1, 0)] if n > 0 else []) + [(n, None)] + ([(n + 1, 1)] if n < NXT - 1 else []) + [(NXT, None), (NXT + 1, None)]
            else:
                kbs = [(NXT, None), (NXT + 1, None)]
            for g in range(2):
                pts = []
                for (kb, mk) in kbs:
                    si, sbk = ringS.next(P)
                    for hp in range(2):
                        P.pe(lambda e, hp=hp, sbk=sbk, g=g, kb=kb, n=n: e.matmul(psv(sbk)[:, hp * 256:(hp + 1) * 256],
                                                                                lhsT=kT2[:, g, hp, kb * 128:(kb + 1) * 128],
                                                                                rhs=qT[:, 2 * g:2 * g + 2, n * 128:(n + 1) * 128],
                                                                                start=True, stop=True))
                    evp = P.sig_last("pe")
                    pslot = pti % 6
                    pti += 1
                    P.wait("act", [evp, ptfree[pslot]])
                    pt = PT[pslot]
                    P.act(lambda e, pt=pt, sbk=sbk: e.activation(out=pt[:, :], in_=psv(sbk)[:, :], func=AF.Exp, scale=ATT_SCALE))
                    eva = P.sig_last("act")
                    ringS.done(si, eva)
                    if mk is not None:
                        P.wait("dve", eva)
                        P.dve(lambda e, pt=pt, mk=mk: e.tensor_tensor(out=pt[:, :].rearrange("p (s q) -> p s q", s=4), in0=pt[:, :].rearrange("p (s q) -> p s q", s=4),
                                                                      in1=masks[:, mk, :].unsqueeze(1).broadcast_to([128, 4, 128]), op=ALU.mult))
                        eva = P.sig_last("dve")
                    pts.append((pslot, pt, kb, eva))
                oi, ob = ringO.next(P)
                ops = psv(ob)[:, 0:260].rearrange("p (s d) -> p s d", d=65)
                for s_ in range(4):
                    for j, (pslot, pt, kb, eva) in enumerate(pts):
                        P.wait("pe", eva)
                        P.pe(lambda e, s_=s_, pt=pt, kb=kb, g=g, j=j, ops=ops, npt=len(pts): e.matmul(ops[:, s_, :], lhsT=pt[:, s_ * 128:(s_ + 1) * 128],
                                                                                                     rhs=Vaug[:, kb, g, :], start=(j == 0), stop=(j == npt - 1)))
                evo = P.sig_last("pe")
                for (pslot, pt, kb, eva) in pts:
                    ptfree[pslot] = evo
                P.wait("dve", [evo, o_free])
                es_v = esink[:, :].rearrange("p (g pl hp) -> p g hp pl", g=2, pl=2)[:, g]
                P.dve(lambda e, ops=ops, es_v=es_v: e.tensor_tensor(out=den[:, 0:4].rearrange("p (a b) -> p a b", a=2), in0=ops[:, :, 64].rearrange("p (a b) -> p a b", a=2), in1=es_v, op=ALU.add))
                P.dve(lambda e: e.reciprocal(out=den[:, 4:8], in_=den[:, 0:4]))
                o_v = Otm[:, :].rearrange("p (g pl hp d) -> p g hp pl d", g=2, pl=2, hp=2)[:, g]
                for hp in range(2):
                    P.dve(lambda e, hp=hp, ops=ops, o_v=o_v: e.tensor_tensor(out=o_v[:, hp], in0=ops[:, 2 * hp:2 * hp + 2, 0:64],
                                                                          in1=den[:, 4 + 2 * hp:6 + 2 * hp].unsqueeze(2).broadcast_to([128, 2, 64]), op=ALU.mult))
                ringO.done(oi, P.sig_last("dve"))
            evd = P.sig_last("dve")
            P.wait("act", evd)
            P.act(lambda e, n=n: e.activation(out=junk3[:, :], in_=Otm[:, :], func=AF.Square, accum_out=st3[:, n, 0:1]))
            P.act(lambda e, n=n: e.activation(out=st3[:, n, 1:2], in_=st3[:, n, 0:1], func=AF.Sqrt, bias=epsc[:, 0:1], scale=1.0 / 512))
            eva = P.sig_last("act")
            P.wait("dve", [eva, ob_free])
            P.dve(lambda e, n=n: e.reciprocal(out=st3[:, n, 2:3], in_=st3[:, n, 1:2]))
            P.dve(lambda e, n=n: e.scalar_tensor_tensor(out=Ob[:, :], in0=Otm[:, :], scalar=st3[:, n, 2:3], in1=ONW[:, :], op0=ALU.mult, op1=ALU.mult))
            evd = P.sig_last("dve")
            o_free = evd
            ti, tb = ringT.next(P)
            P.wait("pe", evd)
            tp = psv(tb, BF16)
            for c in range(4):
                P.pe(lambda e, c=c, tp=tp: e.transpose(out=tp[:, c * 128:(c + 1) * 128], in_=Ob[:, c * 128:(c + 1) * 128], identity=ident_b[:, :]))
            evt = P.sig_last("pe")
            ob_free = evt
            P.wait("act", evt)
            P.act(lambda e, tp=tp, n=n: e.copy(out=mergedT[:, 0:4, n * 128:(n + 1) * 128], in_=tp[:, 0:512].rearrange("p (c t) -> p c t", c=4)))
            ringT.done(ti, P.sig_last("act"))
        P.barrier()

        if l == 0:
            dump("mT3", mergedT[:, :, :].rearrange("p k t -> p (k t)"))
        if stop == 'L3':
            return finish_prog()
        P.release(R5)
        pa = P.sb([128, PW], F32, "pa")
        pb_ = P.sb([128, PW], F32, "pb")
        dlt = P.sb([128, 2, T], BF16, "dlt")
        dfx = P.sb([128, 16], F32, "dfx")
        pwst = P.sb([128, 2, 128], F32, "pwst")
        pwb = P.sb([128, 2, 128], BF16, "pwb")
        psc = P.sb([128, 2], F32, "psc")
        P.dve(lambda e: e.memset(pwst[:, :, :], 0.0))
        evz = P.sig_last("dve")
        P.wait("sp", evz)
        evs = []
        for ct in range(2):
            for hh in range(2):
                evs.append(P.dma("sp", pwst[hh * 64:(hh + 1) * 64, ct, hh * 64:(hh + 1) * 64], pool_w[l, 2 * ct + hh, :, :], "ld4"))
            evs.append(P.dma("sp", psc[:, ct:ct + 1], pool_scale[l, ct * 128:(ct + 1) * 128].rearrange("(p o) -> p o", o=1), "ld4"))
        P.wait("dve", evs)
        P.wait("act", evs)
        P.dve(lambda e: e.tensor_copy(out=pwb[:, :, :], in_=pwst[:, :, :]))
        segs = [(8, L, 0), (8 + L + 8 + 8, LC, L)]
        for ct in range(2):
            for hh in range(2):
                w = POOLW[2 * ct + hh]
                pr = slice(hh * 64, (hh + 1) * 64)
                for (po, ln, tok0) in segs:
                    lo = po - 8
                    n_all = ln + 16
                    src = poolP[pr, ct, lo:lo + n_all]
                    bufs = [pa[pr, 0:n_all], pb_[pr, 0:n_all]]
                    cur = src
                    sh = 1
                    bi = 0
                    while sh < w:
                        dst = bufs[bi]
                        P.dve(lambda e, dst=dst, cur=cur, sh=sh, n_all=n_all: e.tensor_tensor(out=dst[:, sh:n_all], in0=cur[:, sh:n_all], in1=cur[:, 0:n_all - sh], op=ALU.add))
                        cur = dst
                        bi ^= 1
                        sh *= 2
                    o0 = 8 + w // 2 - 1
                    P.dve(lambda e, cur=cur, o0=o0, ln=ln, w=w, ct=ct, pr=pr, po=po, tok0=tok0: e.scalar_tensor_tensor(
                        out=dlt[pr, ct, tok0:tok0 + ln], in0=cur[:, o0:o0 + ln], scalar=1.0 / w, in1=poolP[pr, ct, po:po + ln], op0=ALU.mult, op1=ALU.subtract))
                    hw = w // 2
                    for side in range(2):
                        tb0 = 0 if side == 0 else ln - hw
                        P.dve(lambda e, cur=cur, o0=o0, tb0=tb0, hw=hw, pr=pr, ct=ct, side=side: e.tensor_tensor(
                            out=dfx[pr, 0:hw], in0=cur[:, o0 + tb0:o0 + tb0 + hw], in1=poolfix[pr, ct, side, 0:hw], op=ALU.mult))
                        P.dve(lambda e, tb0=tb0, hw=hw, pr=pr, ct=ct, po=po, tok0=tok0: e.tensor_tensor(
                            out=dlt[pr, ct, tok0 + tb0:tok0 + tb0 + hw], in0=dfx[pr, 0:hw], in1=poolP[pr, ct, po + tb0:po + tb0 + hw], op=ALU.subtract))
        evd = P.sig_last("dve")
        P.wait("pe", evd)
        ringP = Ring([0, 1])
        for ct in range(2):
            for (t0, tn) in TSL:
                pi, pbk = ringP.next(P)
                P.pe(lambda e, ct=ct, t0=t0, tn=tn, pbk=pbk: e.matmul(psv(pbk)[:, 0:tn], lhsT=pwb[:, ct, :], rhs=dlt[:, ct, t0:t0 + tn], start=True, stop=True))
                evp = P.sig_last("pe")
                P.wait("act", evp)
                P.act(lambda e, ct=ct, t0=t0, tn=tn, pbk=pbk: e.activation(out=mergedT[:, 6 + ct, t0:t0 + tn], in_=psv(pbk)[:, 0:tn], func=AF.Copy, scale=psc[:, ct:ct + 1]))
                ringP.done(pi, P.sig_last("act"))
        P.barrier()

        if l == 0:
            dump("mT4", mergedT[:, :, :].rearrange("p k t -> p (k t)"))
        if stop == 'L4':
            return finish_prog()
        P.release(R3)
        prm = P.sb([128, 12, 2, 8], F32, "prm")
        upw = P.sb([128, 2, 8, 3, 12], F32, "upw")
        rcol = P.sb([128, 2, 8], F32, "rcol")
        TP = P.sb([128, 2, 16, 16], F32, "TP")
        tt1 = P.sb([128, 16, 8], F32, "tt1")
        tt2 = P.sb([128, 16, 8], F32, "tt2")
        Cs = P.sb([128, 2, 2, 8, 16], F32, "Cs")
        bbz = P.sb([128, 2, 128], BF16, "bbz")
        BzT = P.sb([128, 2, 2, 128], BF16, "BzT")
        Cz = P.sb([128, 2, 2, 128], BF16, "Cz")
        pl = [P.sb([128, T], F32, f"pl{i}") for i in range(6)]
        sbf = mergedT[:, 4:6, :]
        bst = pl[5][:, 0:512].rearrange("p (d r j q) -> p d r j q", d=2, r=2, j=8)
        yT = P.sb([128, 2, T], F32, "yT")
        dcol = P.sb([128, 8], F32, "dcol")
        gst = P.sb([128, 2, 256], F32, "gst")
        gwb = P.sb([128, 2, 256], BF16, "gwb")
        bbr = P.sb([128, 2, 8, 16], F32, "bbr")
        bbi = P.sb([128, 2, 8, 16], F32, "bbi")
        btmp = pl[4][:, 0:256].rearrange("p (d j q) -> p d j q", d=2, j=8)
        evs = []
        for gl in range(2):
            pr = slice(gl * 64, (gl + 1) * 64)
            for d in range(2):
                evs.append(P.dma("sp", prm[pr, 0, d, :], ssm_a_re[l, d].rearrange("(j gl) n -> gl n j", gl=2)[gl], "ld5", allow_slow_non_contiguous=True))
                evs.append(P.dma("sp", prm[pr, 1, d, :], ssm_a_im[l, d].rearrange("(j gl) n -> gl n j", gl=2)[gl], "ld5", allow_slow_non_contiguous=True))
                evs.append(P.dma("sp", prm[pr, 2, d, :], ssm_log_dt[l, d:d + 1, :].rearrange("o (j gl) -> gl o j", gl=2)[gl].partition_broadcast(64)[:, 0, :],
                                 "ld5", allow_slow_non_contiguous=True))
                for ri, src in enumerate((ssm_b_re, ssm_b_im)):
                    evs.append(P.dma("sp", bst[pr, d, ri, :, :], src[l, d].rearrange("(j gl) n p -> gl n j p", gl=2)[gl], "ld5"))
                for ri, src in enumerate((ssm_c_re, ssm_c_im)):
                    for jj in range(8):
                        evs.append(P.dma("sp", Cs[pr, d, ri, jj, :], src[l, d, 2 * jj + gl].rearrange("p n -> n p"), "ld5c_%d" % l, allow_slow_non_contiguous=True))
        for m_ in range(2):
            evs.append(P.dma("sp", dcol[:, m_:m_ + 1], ssm_d[l, m_ * 128:(m_ + 1) * 128].rearrange("(p o) -> p o", o=1), "ld5"))
            evs.append(P.dma("sp", dcol[:, 2 + m_:3 + m_], glu_b[l, m_ * 128:(m_ + 1) * 128].rearrange("(p o) -> p o", o=1), "ld5"))
            evs.append(P.dma("sp", dcol[:, 4 + m_:5 + m_], out_norm_w[l, 512 + m_ * 128:512 + (m_ + 1) * 128].rearrange("(p o) -> p o", o=1), "ld5"))
        evs.append(P.dma_k("sp", gst[:, :, :], glu_w[l].rearrange("(k p) n -> p k n", p=128), "ld5"))
        P.wait("act", evs)
        P.wait("dve", evs)
        if stop == 'L5a':
            return finish_prog()
        f2 = lambda s_: prm[:, s_, :, :].rearrange("p d j -> p (d j)")
        a_re, a_im, ldt = f2(0), f2(1), f2(2)
        dt_, mag, th, cc_, sn_, ar, ai, t1, t2 = f2(3), f2(4), f2(5), f2(6), f2(7), f2(8), f2(9), f2(10), f2(11)
        P.act(lambda e: e.activation(out=dt_, in_=ldt, func=AF.Exp))
        eva = P.sig_last("act")
        P.wait("dve", eva)
        P.dve(lambda e: e.tensor_scalar(out=Cs[:, :, 1, :, :], in0=Cs[:, :, 1, :, :], scalar1=-1.0, scalar2=None, op0=ALU.mult))
        P.dve(lambda e: e.tensor_tensor(out=mag, in0=a_re, in1=dt_, op=ALU.mult))
        P.dve(lambda e: e.tensor_tensor(out=th, in0=a_im, in1=dt_, op=ALU.mult))
        evd = P.sig_last("dve")
        P.wait("act", evd)
        P.act(lambda e: e.activation(out=mag, in_=mag, func=AF.Exp))
        P.act(lambda e: e.activation(out=sn_, in_=th, func=AF.Sin, scale=1.0 / 16))
        P.act(lambda e: e.activation(out=cc_, in_=th, func=AF.Sin, scale=1.0 / 16, bias=halfpi[:, 0:1]))
        eva = P.sig_last("act")
        P.wait("dve", eva)
        for _ in range(4):
            P.dve(lambda e: e.tensor_tensor(out=t1, in0=cc_, in1=sn_, op=ALU.mult))
            P.dve(lambda e: e.tensor_tensor(out=cc_, in0=cc_, in1=cc_, op=ALU.mult))
            P.dve(lambda e: e.tensor_tensor(out=t2, in0=sn_, in1=sn_, op=ALU.mult))
            P.dve(lambda e: e.tensor_tensor(out=cc_, in0=cc_, in1=t2, op=ALU.subtract))
            P.dve(lambda e: e.tensor_scalar(out=sn_, in0=t1, scalar1=2.0, scalar2=None, op0=ALU.mult))
        P.dve(lambda e: e.tensor_tensor(out=t1, in0=cc_, in1=cc_, op=ALU.mult))
        P.dve(lambda e: e.tensor_tensor(out=t2, in0=sn_, in1=sn_, op=ALU.mult))
        P.dve(lambda e: e.tensor_tensor(out=t1, in0=t1, in1=t2, op=ALU.add))
        evd = P.sig_last("dve")
        P.wait("act", evd)
        P.act(lambda e: e.activation(out=ar, in_=t1, func=AF.Sqrt))
        eva = P.sig_last("act")
        P.wait("dve", eva)
        P.dve(lambda e: e.reciprocal(out=t2, in_=ar))
        P.dve(lambda e: e.tensor_tensor(out=ar, in0=t2, in1=t2, op=ALU.mult))
        P.dve(lambda e: e.tensor_tensor(out=ar, in0=ar, in1=t1, op=ALU.mult))
        P.dve(lambda e: e.tensor_scalar(out=ar, in0=ar, scalar1=-0.5, scalar2=1.5, op0=ALU.mult, op1=ALU.add))
        P.dve(lambda e: e.tensor_tensor(out=t2, in0=t2, in1=ar, op=ALU.mult))
        P.dve(lambda e: e.tensor_tensor(out=cc_, in0=cc_, in1=t2, op=ALU.mult))
        P.dve(lambda e: e.tensor_tensor(out=sn_, in0=sn_, in1=t2, op=ALU.mult))
        P.dve(lambda e: e.tensor_tensor(out=ar, in0=mag, in1=cc_, op=ALU.mult))
        P.dve(lambda e: e.tensor_tensor(out=ai, in0=mag, in1=sn_, op=ALU.mult))
        upv = lambda c, k: upw[:, :, :, c, k].rearrange("p d j -> p (d j)")
        P.dve(lambda e: e.tensor_copy(out=rcol[:, :, :].rearrange("p d j -> p (d j)"), in_=mag))
        P.dve(lambda e: e.tensor_copy(out=upv(0, 0), in_=cc_))
        P.dve(lambda e: e.tensor_copy(out=upv(1, 0), in_=sn_))
        for k in range(1, 12):
            P.dve(lambda e, k=k: e.tensor_tensor(out=t1, in0=upv(0, k - 1), in1=upv(0, k - 1), op=ALU.mult))
            P.dve(lambda e, k=k: e.tensor_tensor(out=t2, in0=upv(1, k - 1), in1=upv(1, k - 1), op=ALU.mult))
            P.dve(lambda e, k=k: e.tensor_tensor(out=upv(0, k), in0=t1, in1=t2, op=ALU.subtract))
            P.dve(lambda e, k=k: e.tensor_tensor(out=t1, in0=upv(0, k - 1), in1=upv(1, k - 1), op=ALU.mult))
            P.dve(lambda e, k=k: e.tensor_scalar(out=upv(1, k), in0=t1, scalar1=2.0, scalar2=None, op0=ALU.mult))
        P.dve(lambda e: e.tensor_scalar(out=upw[:, :, :, 2, :], in0=upw[:, :, :, 1, :], scalar1=-1.0, scalar2=None, op0=ALU.mult))
        P.dve(lambda e: e.memset(TP[:, 0, :, 0:1], 1.0))
        P.dve(lambda e: e.memset(TP[:, 1, :, 0:1], 0.0))
        for k in range(4):
            m = 1 << k
            ucb = upv(0, k).unsqueeze(2).broadcast_to([128, 16, m])
            usb = upv(1, k).unsqueeze(2).broadcast_to([128, 16, m])
            c_old, s_old = TP[:, 0, :, 0:m], TP[:, 1, :, 0:m]
            P.dve(lambda e, m=m, ucb=ucb, c_old=c_old: e.tensor_tensor(out=tt1[:, :, 0:m], in0=c_old, in1=ucb, op=ALU.mult))
            P.dve(lambda e, m=m, usb=usb, s_old=s_old: e.tensor_tensor(out=tt2[:, :, 0:m], in0=s_old, in1=usb, op=ALU.mult))
            P.dve(lambda e, m=m: e.tensor_tensor(out=TP[:, 0, :, m:2 * m], in0=tt1[:, :, 0:m], in1=tt2[:, :, 0:m], op=ALU.subtract))
            P.dve(lambda e, m=m, ucb=ucb, s_old=s_old: e.tensor_tensor(out=tt1[:, :, 0:m], in0=s_old, in1=ucb, op=ALU.mult))
            P.dve(lambda e, m=m, usb=usb, c_old=c_old: e.tensor_tensor(out=tt2[:, :, 0:m], in0=c_old, in1=usb, op=ALU.mult))
            P.dve(lambda e, m=m: e.tensor_tensor(out=TP[:, 1, :, m:2 * m], in0=tt1[:, :, 0:m], in1=tt2[:, :, 0:m], op=ALU.add))
        P.dve(lambda e: e.tensor_tensor(out=t1, in0=a_re, in1=a_re, op=ALU.mult))
        P.dve(lambda e: e.tensor_tensor(out=t2, in0=a_im, in1=a_im, op=ALU.mult))
        P.dve(lambda e: e.tensor_tensor(out=t1, in0=t1, in1=t2, op=ALU.add))
        P.dve(lambda e: e.reciprocal(out=dt_, in_=t1))
        P.dve(lambda e: e.tensor_scalar(out=cc_, in0=ar, scalar1=-1.0, scalar2=None, op0=ALU.add))
        P.dve(lambda e: e.tensor_tensor(out=t1, in0=cc_, in1=a_re, op=ALU.mult))
        P.dve(lambda e: e.tensor_tensor(out=t2, in0=ai, in1=a_im, op=ALU.mult))
        P.dve(lambda e: e.tensor_tensor(out=t1, in0=t1, in1=t2, op=ALU.add))
        P.dve(lambda e: e.tensor_tensor(out=mag, in0=t1, in1=dt_, op=ALU.mult))
        P.dve(lambda e: e.tensor_tensor(out=t1, in0=ai, in1=a_re, op=ALU.mult))
        P.dve(lambda e: e.tensor_tensor(out=t2, in0=cc_, in1=a_im, op=ALU.mult))
        P.dve(lambda e: e.tensor_tensor(out=t1, in0=t1, in1=t2, op=ALU.subtract))
        P.dve(lambda e: e.tensor_tensor(out=th, in0=t1, in1=dt_, op=ALU.mult))
        P.dve(lambda e: e.tensor_copy(out=gwb[:, :, :], in_=gst[:, :, :]))
        qr3 = prm[:, 4, :, :]
        qi3 = prm[:, 5, :, :]
        bq = lambda q3: q3.unsqueeze(3).broadcast_to([128, 2, 8, 16])
        P.dve(lambda e: e.tensor_tensor(out=bbr[:, :, :, :], in0=bst[:, :, 0, :, :], in1=bq(qr3), op=ALU.mult))
        P.dve(lambda e: e.tensor_tensor(out=btmp[:, :, :, :], in0=bst[:, :, 1, :, :], in1=bq(qi3), op=ALU.mult))
        P.dve(lambda e: e.tensor_tensor(out=bbr[:, :, :, :], in0=bbr[:, :, :, :], in1=btmp[:, :, :, :], op=ALU.subtract))
        P.dve(lambda e: e.tensor_tensor(out=bbi[:, :, :, :], in0=bst[:, :, 1, :, :], in1=bq(qr3), op=ALU.mult))
        P.dve(lambda e: e.tensor_tensor(out=btmp[:, :, :, :], in0=bst[:, :, 0, :, :], in1=bq(qi3), op=ALU.mult))
        P.dve(lambda e: e.tensor_tensor(out=bbi[:, :, :, :], in0=bbi[:, :, :, :], in1=btmp[:, :, :, :], op=ALU.add))
        if stop == 'L5b':
            return finish_prog()
        ringX = Ring([0, 1, 2, 3])
        ringY = Ring([4, 5])
        ringZ = Ring([6, 7])
        bz_free = None
        sbf_free = None
        y_started = [False, False]
        for j in range(8):
            mt = j // 4
            P.wait("dve", bz_free)
            for d in range(2):
                P.dve(lambda e: e.memset(bbz[:, :, :], 0.0))
                for ri, bb in enumerate((bbr, bbi)):
                    for gl in range(2):
                        c0 = 16 * ((2 * j + gl) % 8)
                        P.dve(lambda e, gl=gl, c0=c0, bb=bb, d=d, ri=ri, j=j: e.tensor_copy(out=bbz[gl * 64:(gl + 1) * 64, ri, c0:c0 + 16], in_=bb[gl * 64:(gl + 1) * 64, d, j, :]))
                evd = P.sig_last("dve")
                zi, zb = ringZ.next(P)
                P.wait("pe", evd)
                for ri in range(2):
                    P.pe(lambda e, ri=ri, zb=zb: e.transpose(out=psv(zb, BF16)[:, ri * 128:(ri + 1) * 128], in_=bbz[:, ri, :], identity=ident_b[:, :]))
                evp = P.sig_last("pe")
                P.wait("dve", evp)
                P.dve(lambda e, zb=zb, d=d: e.tensor_copy(out=BzT[:, d, :, :], in_=psv(zb, BF16)[:, 0:256].rearrange("p (r s) -> p r s", r=2)))
                ringZ.done(zi, P.sig_last("dve"))
                P.dve(lambda e, d=d: e.memset(Cz[:, d, :, :], 0.0))
                for ri in range(2):
                    for gl in range(2):
                        c0 = 16 * ((2 * j + gl) % 8)
                        P.dve(lambda e, gl=gl, c0=c0, d=d, ri=ri, j=j: e.tensor_copy(out=Cz[gl * 64:(gl + 1) * 64, d, ri, c0:c0 + 16], in_=Cs[gl * 64:(gl + 1) * 64, d, ri, j, :]))
            ev_mats = P.sig_last("dve")
            P.wait("pe", ev_mats)
            for d in range(2):
                for (t0, tn) in TSL:
                    c0 = (t0 + LC if t0 < L else 0) if d == 0 else t0
                    for ri in range(2):
                        xi, xb = ringX.next(P)
                        P.pe(lambda e, ri=ri, xb=xb, d=d, t0=t0, tn=tn, mt=mt: e.matmul(psv(xb)[:, 0:tn], lhsT=BzT[:, d, ri, :], rhs=uT[:, mt, t0:t0 + tn], start=True, stop=True))
                        evp = P.sig_last("pe")
                        P.wait("act", [evp] + (pl_free[0] or []))
                        P.act(lambda e, ri=ri, xb=xb, c0=c0, tn=tn: e.copy(out=pl[ri][:, c0:c0 + tn], in_=psv(xb)[:, 0:tn]))
                        ringX.done(xi, P.sig_last("act"))
                eva = P.sig_last("act")
                P.wait("dve", [eva, sbf_free])
                V = (lambda ap: ap) if d == 0 else (lambda ap: ap[:, ::-1])
                Tc, Ts, P5, P6 = pl[2], pl[3], pl[4], pl[5]
                xr, xi = V(pl[0][:, :]), V(pl[1][:, :])
                cmb = d * 8 + j
                P.dve(lambda e, cmb=cmb: e.tensor_copy(out=Tc[:, 0:16], in_=TP[:, 0, cmb, :]))
                P.dve(lambda e, cmb=cmb: e.tensor_copy(out=Ts[:, 0:16], in_=TP[:, 1, cmb, :]))
                for k in range(4, 12):
                    m = 1 << k
                    n = min(m, T - m)
                    uc = upw[:, d, j, 0, k:k + 1]
                    us = upw[:, d, j, 1, k:k + 1]
                    nus = upw[:, d, j, 2, k:k + 1]
                    P.dve(lambda e, m=m, n=n, uc=uc: e.tensor_scalar(out=Tc[:, m:m + n], in0=Tc[:, 0:n], scalar1=uc, scalar2=None, op0=ALU.mult))
                    P.dve(lambda e, m=m, n=n, nus=nus: e.scalar_tensor_tensor(out=Tc[:, m:m + n], in0=Ts[:, 0:n], scalar=nus, in1=Tc[:, m:m + n], op0=ALU.mult, op1=ALU.add))
                    P.dve(lambda e, m=m, n=n, uc=uc: e.tensor_scalar(out=Ts[:, m:m + n], in0=Ts[:, 0:n], scalar1=uc, scalar2=None, op0=ALU.mult))
                    P.dve(lambda e, m=m, n=n, us=us: e.scalar_tensor_tensor(out=Ts[:, m:m + n], in0=Tc[:, 0:n], scalar=us, in1=Ts[:, m:m + n], op0=ALU.mult, op1=ALU.add))
                P.dve(lambda e, xr=xr: e.tensor_tensor(out=P5[:, :], in0=Tc[:, :], in1=xr, op=ALU.mult))
                P.dve(lambda e, xi=xi: e.tensor_tensor(out=P6[:, :], in0=Ts[:, :], in1=xi, op=ALU.mult))
                P.dve(lambda e: e.tensor_tensor(out=P5[:, :], in0=P5[:, :], in1=P6[:, :], op=ALU.add))
                P.dve(lambda e, xr=xr: e.tensor_tensor(out=P6[:, :], in0=Ts[:, :], in1=xr, op=ALU.mult))
                P.dve(lambda e, xi=xi: e.tensor_tensor(out=xi, in0=Tc[:, :], in1=xi, op=ALU.mult))
                P.dve(lambda e, xi=xi: e.tensor_tensor(out=xi, in0=xi, in1=P6[:, :], op=ALU.subtract))
                rb = rcol[:, d, j:j + 1].broadcast_to([128, T])
                zr, zi = pl[0], P6
                P.dve(lambda e, rb=rb: e.tensor_tensor_scan(out=zr[:, :], data0=rb, data1=P5[:, :], initial=0.0, op0=ALU.mult, op1=ALU.add))
                P.dve(lambda e, rb=rb, xi=xi: e.tensor_tensor_scan(out=zi[:, :], data0=rb, data1=xi, initial=0.0, op0=ALU.mult, op1=ALU.add))
                tmp = pl[1]
                P.dve(lambda e: e.tensor_tensor(out=P5[:, :], in0=Tc[:, :], in1=zr[:, :], op=ALU.mult))
                P.dve(lambda e: e.tensor_tensor(out=tmp[:, :], in0=Ts[:, :], in1=zi[:, :], op=ALU.mult))
                so_re, so_im = V(sbf[:, 0, :]), V(sbf[:, 1, :])
                P.dve(lambda e, so_re=so_re: e.tensor_tensor(out=so_re, in0=P5[:, :], in1=tmp[:, :], op=ALU.subtract))
                P.dve(lambda e: e.tensor_tensor(out=P5[:, :], in0=Ts[:, :], in1=zr[:, :], op=ALU.mult))
                P.dve(lambda e: e.tensor_tensor(out=tmp[:, :], in0=Tc[:, :], in1=zi[:, :], op=ALU.mult))
                P.dve(lambda e, so_im=so_im: e.tensor_tensor(out=so_im, in0=P5[:, :], in1=tmp[:, :], op=ALU.add))
                pl_free[0] = [P.sig_last("dve")]
                P.wait("pe", pl_free[0])
                for (t0, tn) in TSL:
                    c0 = (t0 + LC if t0 < L else 0) if d == 0 else t0
                    yi, yb = ringY.next(P)
                    for ri in range(2):
                        P.pe(lambda e, d=d, ri=ri, yb=yb, c0=c0, tn=tn: e.matmul(psv(yb)[:, 0:tn], lhsT=Cz[:, d, ri, :], rhs=sbf[:, ri, c0:c0 + tn], start=(ri == 0), stop=(ri == 1)))
                    evp = P.sig_last("pe")
                    P.wait("dve", evp)
                    if not y_started[mt]:
                        P.dve(lambda e, yb=yb, t0=t0, tn=tn, mt=mt: e.tensor_copy(out=yT[:, mt, t0:t0 + tn], in_=psv(yb)[:, 0:tn]))
                    else:
                        P.dve(lambda e, yb=yb, t0=t0, tn=tn, mt=mt: e.tensor_tensor(out=yT[:, mt, t0:t0 + tn], in0=yT[:, mt, t0:t0 + tn], in1=psv(yb)[:, 0:tn], op=ALU.add))
                    ringY.done(yi, P.sig_last("dve"))
                y_started[mt] = True
                sbf_free = P.sig_last("pe")
            bz_free = P.sig_last("pe")
        P.barrier()
        if l == 0:
            dump("Tc", pl[2][:, :])
            dump("Ts", pl[3][:, :])
            dump("zr", pl[0][:, :])
        if stop == 'L5c':
            return finish_prog()
        if l == 0:
            dump("yT", yT[:, :, :].rearrange("p k t -> p (k t)"))
        g32 = pl[0]
        tA = pl[1]
        gb = pl[2][:, :].bitcast(BF16).rearrange("p (k t) -> p k t", k=2)
        sq = pl[3][:, :].bitcast(BF16).rearrange("p (k t) -> p k t", k=2)
        for m_ in range(2):
            y_ = yT[:, m_, :]
            P.dve(lambda e, m_=m_, y_=y_: e.scalar_tensor_tensor(out=y_, in0=uT[:, m_, :], scalar=dcol[:, m_:m_ + 1], in1=y_, op0=ALU.mult, op1=ALU.add))
            P.dve(lambda e, y_=y_: e.tensor_tensor(out=tA[:, :], in0=y_, in1=y_, op=ALU.mult))
            P.dve(lambda e: e.tensor_scalar(out=tA[:, :], in0=tA[:, :], scalar1=0.044715, scalar2=1.0, op0=ALU.mult, op1=ALU.add))
            P.dve(lambda e, y_=y_: e.tensor_tensor(out=tA[:, :], in0=tA[:, :], in1=y_, op=ALU.mult))
            evd = P.sig_last("dve")
            P.wait("act", evd)
            P.act(lambda e: e.activation(out=tA[:, :], in_=tA[:, :], func=AF.Sigmoid, scale=1.5957691216057308))
            eva = P.sig_last("act")
            P.wait("dve", eva)
            P.dve(lambda e, y_=y_: e.tensor_tensor(out=y_, in0=y_, in1=tA[:, :], op=ALU.mult))
            P.dve(lambda e, y_=y_, m_=m_: e.tensor_copy(out=gb[:, m_, :], in_=y_))
        evd = P.sig_last("dve")
        P.wait("pe", evd)
        P.wait("act", evd)
        ringG = Ring([0, 1])
        for m_ in range(2):
            for (t0, tn) in TSL:
                gi, gbk = ringG.next(P)
                for kt in range(2):
                    P.pe(lambda e, kt=kt, m_=m_, gbk=gbk, t0=t0, tn=tn: e.matmul(psv(gbk)[:, 0:tn], lhsT=gwb[:, kt, m_ * 128:(m_ + 1) * 128], rhs=gb[:, kt, t0:t0 + tn], start=(kt == 0), stop=(kt == 1)))
                evp = P.sig_last("pe")
                P.wait("act", evp)
                P.act(lambda e, m_=m_, gbk=gbk, t0=t0, tn=tn: e.activation(out=tA[:, t0:t0 + tn], in_=psv(gbk)[:, 0:tn], func=AF.Sigmoid, bias=dcol[:, 2 + m_:3 + m_]))
                eva = P.sig_last("act")
                ringG.done(gi, eva)
                P.wait("dve", eva)
                P.dve(lambda e, m_=m_, t0=t0, tn=tn: e.tensor_tensor(out=yT[:, m_, t0:t0 + tn], in0=yT[:, m_, t0:t0 + tn], in1=tA[:, t0:t0 + tn], op=ALU.mult))
                P.dve(lambda e, m_=m_, t0=t0, tn=tn: e.tensor_tensor(out=sq[:, m_, t0:t0 + tn], in0=yT[:, m_, t0:t0 + tn], in1=yT[:, m_, t0:t0 + tn], op=ALU.mult))
                evd = P.sig_last("dve")
                P.wait("act", evd)
        evd = P.sig_last("dve")
        P.wait("pe", evd)
        if l == 0:
            dump("ssm", yT[:, :, :].rearrange("p k t -> p (k t)"))
        ringG = Ring([2, 3])
        for (t0, tn) in TSL:
            gi, gbk = ringG.next(P)
            for kt in range(2):
                P.pe(lambda e, kt=kt, gbk=gbk, t0=t0, tn=tn: e.matmul(psv(gbk)[:, 0:tn], lhsT=ones_b[:, :], rhs=sq[:, kt, t0:t0 + tn], start=(kt == 0), stop=(kt == 1)))
            evp = P.sig_last("pe")
            P.wait("act", evp)
            P.act(lambda e, gbk=gbk, t0=t0, tn=tn: e.activation(out=g32[:, t0:t0 + tn], in_=psv(gbk)[:, 0:tn], func=AF.Sqrt, bias=epsc[:, 0:1], scale=1.0 / 256))
            eva = P.sig_last("act")
            ringG.done(gi, eva)
            P.wait("dve", eva)
            P.dve(lambda e, t0=t0, tn=tn: e.reciprocal(out=g32[:, t0:t0 + tn], in_=g32[:, t0:t0 + tn]))
            for m_ in range(2):
                P.dve(lambda e, m_=m_, t0=t0, tn=tn: e.scalar_tensor_tensor(out=mergedT[:, 4 + m_, t0:t0 + tn], in0=yT[:, m_, t0:t0 + tn], scalar=dcol[:, 4 + m_:5 + m_],
                                                                           in1=g32[:, t0:t0 + tn], op0=ALU.mult, op1=ALU.mult))
        P.barrier()
        if l == 0:
            dump("mergedT", mergedT[:, :, :].rearrange("p k t -> p (k t)"))

        if stop == 'L5':
            return finish_prog()
        P.release(R2)
        wob = P.sb([128, 8, D], BF16, "wob")
        g1 = [P.sb([128, D], F32, f"g1_{r}") for r in range(2)]
        tmp6 = [P.sb([128, 512], F32, f"tmp6_{i}") for i in range(2)]
        evw = P.dma_k("pool", wob[:, :, :], w_out[l].rearrange("(k p) n -> p k n", p=128), "ldw_out")
        evg = [P.dma("sp", g1[r][:, :], bcast_row(modscr[l, r:r + 1, 2 * D:3 * D]), "ld6") for r in range(2)]
        P.wait("pe", evw)
        P.wait("dve", evg)
        ring6 = Ring([0, 1, 2, 3])
        tfree = [None, None]
        it = 0
        for i in range(NXT if last else NT):
            r = 0 if i < NXT else 1
            for hf_ in range(2):
                oi, obk = ring6.next(P)
                for c in range(8):
                    P.pe(lambda e, c=c, obk=obk, i=i, hf_=hf_: e.matmul(psv(obk)[:, :], lhsT=mergedT[:, c, i * 128:(i + 1) * 128], rhs=wob[:, c, hf_ * 512:(hf_ + 1) * 512], start=(c == 0), stop=(c == 7)))
                evp = P.sig_last("pe")
                s = it % 2
                it += 1
                P.wait("dve", [evp, tfree[s]])
                P.dve(lambda e, obk=obk, r=r, hf_=hf_, s=s: e.tensor_tensor(out=tmp6[s][:, :], in0=psv(obk)[:, :], in1=g1[r][:, hf_ * 512:(hf_ + 1) * 512], op=ALU.mult))
                evd = P.sig_last("dve")
                ring6.done(oi, evd)
                P.wait("pool", evd)
                P.pool(lambda e, i=i, hf_=hf_, s=s: e.tensor_tensor(out=X[:, i, hf_ * 512:(hf_ + 1) * 512], in0=X[:, i, hf_ * 512:(hf_ + 1) * 512], in1=tmp6[s][:, :], op=ALU.add))
                tfree[s] = P.sig_last("pool")
        P.barrier()
        if l == 0:
            dump("xmix0", X[:, :, :].rearrange("p i d -> p (i d)"))
        if moe_experts == 0:
            continue

        P.new_epoch()
        ntm = NXT if last else NT
        P.release(R1)
        h2T = P.sb([128, 8, T], BF16, "h2T")
        Gs = P.sb([128, NT, NE], F32, "Gs")
        g2 = [P.sb([128, D], F32, f"g2_{r}") for r in range(2)]
        GTu = P.sb([128, NT, 128], BF16, "GTu")
        OFF_M = P.mark()
        LG = P.sb([128, NT, NE], F32, "LG")
        rwst = P.sb([128, 8, NE], F32, "rwst")
        rbb = P.sb([128, NE], F32, "rbb")
        h32 = P.sb([128, 8, 128], F32, "h32")
        m8 = P.sb([128, 8], F32, "m8")
        msk = P.sb([128, NE], F32, "msk")
        ex = P.sb([128, NE], F32, "ex")
        gu_ = P.sb([128, 128], F32, "gu_")
        st7 = P.sb([128, 4], F32, "st7")
        evs = [P.dma_k("sp", rwst[:, :, :], router_w[l].rearrange("(k p) n -> p k n", p=128), "ld7"),
               P.dma("sp", rbb[:, :], bcast_row(router_b[l:l + 1, :]), "ld7")]
        evs += [P.dma("sp", g2[r][:, :], bcast_row(modscr[l, r:r + 1, 5 * D:6 * D]), "ld7") for r in range(2)]
        P.wait("pe", evs)
        P.wait("dve", evs)
        h32_free[0] = None
        norm_mod_transpose(l, norm2_w, 3, 4, h2T, ntm, router=(rwst, rbb, LG, h32))
        ringG = Ring([0, 1])
        P.dve(lambda e: e.memset(gu_[:, :], 0.0))
        for i in range(ntm):
            P.dve(lambda e, i=i: e.max(out=m8[:, :], in_=LG[:, i, :]))
            P.dve(lambda e, i=i: e.tensor_scalar(out=msk[:, :], in0=LG[:, i, :], scalar1=m8[:, 3:4], scalar2=None, op0=ALU.is_ge))
            P.dve(lambda e: e.tensor_scalar(out=st7[:, 0:1], in0=m8[:, 0:1], scalar1=-1.0, scalar2=None, op0=ALU.mult))
            evd = P.sig_last("dve")
            P.wait("act", evd)
            P.act(lambda e, i=i: e.activation(out=ex[:, :], in_=LG[:, i, :], func=AF.Exp, bias=st7[:, 0:1]))
            eva = P.sig_last("act")
            P.wait("dve", eva)
            P.dve(lambda e: e.tensor_tensor(out=ex[:, :], in0=ex[:, :], in1=msk[:, :], op=ALU.mult))
            P.dve(lambda e: e.tensor_reduce(out=st7[:, 1:2], in_=ex[:, :], axis=AX.X, op=ALU.add))
            P.dve(lambda e: e.reciprocal(out=st7[:, 2:3], in_=st7[:, 1:2]))
            P.dve(lambda e: e.tensor_scalar(out=gu_[:, 0:NE], in0=ex[:, :], scalar1=st7[:, 2:3], scalar2=None, op0=ALU.mult))
            P.dve(lambda e, i=i: e.tensor_scalar(out=Gs[:, i, :], in0=gu_[:, 0:NE], scalar1=KSW, scalar2=None, op0=ALU.mult))
            evd = P.sig_last("dve")
            gi, gbk = ringG.next(P)
            P.wait("pe", evd)
            P.pe(lambda e, gbk=gbk: e.transpose(out=psv(gbk)[:, 0:128], in_=gu_[:, :], identity=ident_f[:, :]))
            evp = P.sig_last("pe")
            P.wait("act", evp)
            P.act(lambda e, gbk=gbk, i=i: e.copy(out=GTu[:, i, :], in_=psv(gbk)[:, 0:128]))
            eva = P.sig_last("act")
            ringG.done(gi, eva)
            P.wait("dve", eva)
        P.barrier()
        if l == 0:
            dump("gates0", Gs[:, :, :].rearrange("p i e -> p (i e)"))

        P.release(OFF_M)
        actT = P.sb([128, 8, T], BF16, "actT")
        wgu = [P.sb([128, 8, 256], BF16, f"wgu{i}") for i in range(2)]
        wdn = P.sb([128, 8, D], BF16, "wdn")
        bguT = P.sb([128, 16, NE], F32, "bguT")
        gcb = [P.sb([128, 512], F32, f"gc{i}") for i in range(2)]
        gsb = [P.sb([128, 512], F32, f"gs{i}") for i in range(2)]
        u1b = [P.sb([128, 512], F32, f"u1{i}") for i in range(2)]
        dtm = [P.sb([128, 512], F32, f"dt{i}") for i in range(2)]
        wd_u8 = wdn[:, :, :].rearrange("p k n -> p (k n)")
        bgall = wd_u8[:, 0:4096].bitcast(F32)
        bdn = wd_u8[:, 4096:6144].bitcast(F32)
        bdnb = wd_u8[:, 6144:7168]
        ringGU = Ring([0, 1, 2, 3])
        ringD = Ring([4, 5, 6, 7])
        wgu_free = [None, None]
        tmp_free = [None, None]
        dtm_free = [None, None]
        tsl_m = TSL if not last else TSL[:4]
        gq = 0
        blk = 0
        dblk = [0]

        def down_accum(lhs_fn, rhs_fn, nk, gate_fn, i_tiles):
            for i in i_tiles:
                r = 0 if i < NXT else 1
                for hf_ in range(2):
                    di, dbk = ringD.next(P)
                    for c in range(nk):
                        P.pe(lambda e, c=c, dbk=dbk, i=i, hf_=hf_: e.matmul(psv(dbk)[:, :], lhsT=lhs_fn(c, i), rhs=rhs_fn(c, hf_), start=(c == 0), stop=(c == nk - 1)))
                    evp = P.sig_last("pe")
                    s = dblk[0] % 2
                    dblk[0] += 1
                    P.wait("dve", [evp, dtm_free[s]])
                    gsc = gate_fn(i)
                    if gsc is None:
                        P.dve(lambda e, dbk=dbk, r=r, hf_=hf_, s=s: e.tensor_tensor(out=dtm[s][:, :], in0=psv(dbk)[:, :], in1=g2[r][:, hf_ * 512:(hf_ + 1) * 512], op=ALU.mult))
                    else:
                        P.dve(lambda e, dbk=dbk, r=r, hf_=hf_, s=s, gsc=gsc: e.scalar_tensor_tensor(out=dtm[s][:, :], in0=psv(dbk)[:, :], scalar=gsc, in1=g2[r][:, hf_ * 512:(hf_ + 1) * 512], op0=ALU.mult, op1=ALU.mult))
                    evd = P.sig_last("dve")
                    ringD.done(di, evd)
                    P.wait("pool", evd)
                    P.pool(lambda e, i=i, hf_=hf_, s=s: e.tensor_tensor(out=X[:, i, hf_ * 512:(hf_ + 1) * 512], in0=X[:, i, hf_ * 512:(hf_ + 1) * 512], in1=dtm[s][:, :], op=ALU.add))
                    dtm_free[s] = P.sig_last("pool")

        P.dve(lambda e: e.memset(wd_u8[:, 0:7168], 0.0))
        evz = P.sig_last("dve")
        P.wait("sp", evz)
        nwe = exp_b_gu.shape[1]
        evb = [P.dma("sp", bgall[0:nwe, :], exp_b_gu[l, :, :], "ld8b"), P.dma("sp", bdn[0:nwe, :], exp_b_down[l, :, :], "ld8b")]
        P.wait("pe", evb)
        P.wait("dve", evb)
        for c in range(16):
            P.pe(lambda e, c=c: e.transpose(out=psv(c // 4)[:, (c % 4) * 128:(c % 4 + 1) * 128], in_=bgall[:, c * 128:(c + 1) * 128], identity=ident_f[:, :]))
        evp = P.sig_last("pe")
        P.wait("dve", evp)
        for c4 in range(4):
            P.dve(lambda e, c4=c4: e.tensor_copy(out=bguT[:, 4 * c4:4 * c4 + 4, :], in_=psv(c4).rearrange("p (c x) -> p c x", c=4)[:, :, 0:NE]))
        P.dve(lambda e: e.tensor_copy(out=bdnb, in_=bdn))
        evd = P.sig_last("dve")
        P.wait("pe", evd)
        down_accum(lambda c, i: GTu[:, i, :], lambda c, hf_: bdnb[:, hf_ * 512:(hf_ + 1) * 512], 1, lambda i: None, range(ntm))
        wdn_free = P.sig_last("pe")
        act_free = None

        pre = {}

        def issue_wgu(ex_j, grp_j):
            ws_ = grp_j % 2
            P.wait("pool", wgu_free[ws_])
            e1 = P.dma_k("pool", wgu[ws_][:, :, 0:128], exp_w_gu[l, ex_j, :, grp_j * 128:(grp_j + 1) * 128].rearrange("(k p) n -> p k n", p=128), "ld8u%d_%d" % (ws_, l))
            e2 = P.dma_k("pool", wgu[ws_][:, :, 128:256], exp_w_gu[l, ex_j, :, D + grp_j * 128:D + (grp_j + 1) * 128].rearrange("(k p) n -> p k n", p=128), "ld8u%d_%d" % (ws_, l))
            return e1, e2

        for ex_i in range(moe_experts):
            evwd = None
            for grp in range(8):
                ws = grp % 2
                if (ex_i, grp) in pre:
                    evg1, evg2 = pre.pop((ex_i, grp))
                else:
                    evg1, evg2 = issue_wgu(ex_i, grp)
                if grp == 2:
                    P.wait("pool", wdn_free)
                    evwd = P.dma_k("pool", wdn[:, :, :], exp_w_down[l, ex_i].rearrange("(k p) n -> p k n", p=128), "ld8d")
                P.wait("pe", [evg1, evg2])
                ch = grp
                for (t0, tn) in tsl_m:
                    gi, gbk = ringGU.next(P)
                    ui, ubk = ringGU.next(P)
                    for k in range(8):
                        P.pe(lambda e, k=k, gbk=gbk, ws=ws, t0=t0, tn=tn: e.matmul(psv(gbk)[:, 0:tn], lhsT=wgu[ws][:, k, 0:128], rhs=h2T[:, k, t0:t0 + tn], start=(k == 0), stop=(k == 7)))
                    for k in range(8):
                        P.pe(lambda e, k=k, ubk=ubk, ws=ws, t0=t0, tn=tn: e.matmul(psv(ubk)[:, 0:tn], lhsT=wgu[ws][:, k, 128:256], rhs=h2T[:, k, t0:t0 + tn], start=(k == 0), stop=(k == 7)))
                    evp = P.sig_last("pe")
                    s = blk % 2
                    blk += 1
                    P.wait("dve", [evp, tmp_free[s], act_free])
                    P.wait("act", [evp, tmp_free[s]])
                    P.dve(lambda e, gbk=gbk, s=s, ch=ch, tn=tn, ex_i=ex_i: e.tensor_scalar(out=gcb[s][:, 0:tn], in0=psv(gbk)[:, 0:tn], scalar1=bguT[:, ch, ex_i:ex_i + 1], scalar2=7.0, op0=ALU.add, op1=ALU.min))
                    evd1 = P.sig_last("dve")
                    P.act(lambda e, ubk=ubk, s=s, ch=ch, tn=tn, ex_i=ex_i: e.activation(out=u1b[s][:, 0:tn], in_=psv(ubk)[:, 0:tn], func=AF.Identity, bias=bguT[:, 8 + ch, ex_i:ex_i + 1]))
                    P.wait("act", evd1)
                    P.act(lambda e, s=s, tn=tn: e.activation(out=gsb[s][:, 0:tn], in_=gcb[s][:, 0:tn], func=AF.Silu, scale=1.702))
                    eva = P.sig_last("act")
                    ringGU.done(gi, evd1)
                    ringGU.done(ui, eva)
                    P.wait("dve", eva)
                    P.dve(lambda e, s=s, tn=tn: e.tensor_scalar(out=u1b[s][:, 0:tn], in0=u1b[s][:, 0:tn], scalar1=-7.0, scalar2=7.0, op0=ALU.max, op1=ALU.min))
                    P.dve(lambda e, s=s, ch=ch, t0=t0, tn=tn: e.scalar_tensor_tensor(out=actT[:, ch, t0:t0 + tn], in0=u1b[s][:, 0:tn], scalar=1.0, in1=gsb[s][:, 0:tn], op0=ALU.add, op1=ALU.mult))
                    tmp_free[s] = P.sig_last("dve")
                wgu_free[ws] = P.sig_last("pe")
            act_written = P.sig_last("dve")
            if ex_i + 1 < moe_experts:
                for g_ in range(2):
                    pre[(ex_i + 1, g_)] = issue_wgu(ex_i + 1, g_)
            P.wait("pe", [act_written, evwd])
            down_accum(lambda c, i: actT[:, c, i * 128:(i + 1) * 128], lambda c, hf_: wdn[:, c, hf_ * 512:(hf_ + 1) * 512], 8,
                       lambda i, ex_i=ex_i: Gs[:, i, ex_i:ex_i + 1], range(ntm))
            wdn_free = P.sig_last("pe")
            act_free = wdn_free
        P.barrier()
        if l == 0:
            dump("x_l0", X[:, :, :].rearrange("p i d -> p (i d)"))

    return finish_prog()


def _consts():
    ident = np.eye(128, dtype=np.float32)
    t = np.arange(L)
    row = (t // 64).astype(np.float32)
    col = (t % 64).astype(np.float32)
    inv = (10000.0 ** (-np.arange(0, 32, 2, dtype=np.float32) / 32)).astype(np.float32)
    ang = np.concatenate([row[:, None] * inv, row[:, None] * inv, col[:, None] * inv, col[:, None] * inv], axis=-1).astype(np.float32)
    cos = np.cos(ang).astype(np.float32)
    sin = np.sin(ang).astype(np.float32)
    sgn = np.concatenate([-np.ones(16), np.ones(16), -np.ones(16), np.ones(16)]).astype(np.float32)
    rope = np.concatenate([cos, sin * sgn[None, :]], axis=-1).reshape(NXT, 128, 128).transpose(1, 0, 2)
    j = np.arange(128)[:, None]
    i = np.arange(128)[None, :]
    mask = np.stack([(i <= j), (j <= i)], axis=1).astype(np.float32)
    fix = np.ones((128, 2, 2, 8), np.float32)
    pinv = np.zeros((128, 2), np.float32)
    for p in range(128):
        for ct in range(2):
            w = POOLW[2 * ct + p // 64]
            pinv[p, ct] = 1.0 / w
            for tt in range(w // 2):
                fix[p, ct, 0, tt] = 1.0 / (tt + w // 2)
                fix[p, ct, 1, tt] = 1.0 / (w - tt)
    return {"c_ident": ident, "c_rope": np.ascontiguousarray(rope, dtype=np.float32), "c_mask": np.ascontiguousarray(mask),
            "c_poolfix": fix, "c_poolinv": pinv}


WEIGHT_KEYS = ["w_mod", "b_mod", "norm1_w", "norm2_w", "w_in", "q_norm_w", "k_norm_w", "attn_sink", "ssm_a_re", "ssm_a_im",
               "ssm_log_dt", "ssm_b_re", "ssm_b_im", "ssm_c_re", "ssm_c_im", "ssm_d", "glu_w", "glu_b", "pool_w", "pool_scale",
               "out_norm_w", "w_out", "router_w", "router_b", "exp_w_gu", "exp_b_gu", "exp_w_down", "exp_b_down"]


def make_in_map(inputs, b, consts, wd=DEPTH, we=NE):
    m = {k: np.ascontiguousarray(np.asarray(inputs[k][:wd, :we] if k.startswith('exp_') else inputs[k][:wd], dtype=np.float32)) for k in WEIGHT_KEYS}
    m["x"] = np.ascontiguousarray(np.asarray(inputs["x"][b], dtype=np.float32))
    m["ctx"] = np.ascontiguousarray(np.asarray(inputs["ctx"][b], dtype=np.float32))
    m["cc"] = np.ascontiguousarray(np.stack([np.asarray(inputs["c"][b]), np.asarray(inputs["c_ctx"])]).astype(np.float32))
    m.update(consts)
    return m


def kernel(**inputs):
    nc = build_program()
    consts = _consts()
    in_maps = [make_in_map(inputs, b, consts) for b in range(8)]
    res = run_bass_kernel_spmd(nc, in_maps, core_ids=list(range(8)))
    return np.stack([np.asarray(r["out"], dtype=np.float32) for r in res.results], axis=0)
```

```python
from contextlib import ExitStack
import math
import numpy as np
import ml_dtypes
import concourse.bass as bass
import concourse.mybir as mybir
from concourse.bass_utils import run_bass_kernel_spmd

F32 = mybir.dt.float32
BF16 = mybir.dt.bfloat16
AF = mybir.ActivationFunctionType
ALU = mybir.AluOpType
AX = mybir.AxisListType

D = 1024
L = 2048
LC = 256
T = L + LC
NT = T // 128
NXT = L // 128
DEPTH = 4
NE = 32
EPS = 1e-6
ATT_SCALE = 64 ** -0.5
KSW = 1.0 / 1.702
ENGS = ("pe", "act", "dve", "pool", "sp")
TSL = [(0, 512), (512, 512), (1024, 512), (1536, 512), (2048, 256)]
POOLW = (2, 4, 8, 16)


class _Probe:
    def __init__(self):
        self.outs = []

    def __getattr__(self, name):
        def f(*a, **kw):
            o = kw.get("out", None)
            if o is None and a:
                o = a[0]
            self.outs.append(o)
            if kw.get("accum_out", None) is not None:
                self.outs.append(kw["accum_out"])
            return self
        return f

    def small(self):
        for o in self.outs:
            try:
                n = int(np.prod(o.shape[1:]))
            except Exception:
                n = 1 << 20
            if n < 128:
                return True
        return False


class Prog:
    def __init__(self, nc):
        self.nc = nc
        self.q = {e: [] for e in ENGS}
        self.cnt = {e: 0 for e in ENGS}
        self.waited = {e: {} for e in ENGS}
        self.sems = {}
        self.dcnt = {}
        self.last_sig = {}
        self.epoch = 0
        self.stack = ExitStack()
        self.sb_off = 16512
        self.sb_n = 0

    def sb(self, shape, dtype, name=None):
        nbytes = int(np.prod(shape[1:])) * mybir.dt.size(dtype)
        nbytes = (nbytes + 31) // 32 * 32
        off = self.sb_off
        self.sb_off += nbytes
        assert self.sb_off <= 229376, ("SBUF overflow", self.sb_off, name)
        self.sb_n += 1
        return self.nc.alloc_sbuf_tensor_at(f"sb{self.sb_n}_{name or ''}", list(shape), dtype, offset=off)

    def mark(self):
        return self.sb_off

    def release(self, m):
        self.sb_off = m

    def sem(self, name):
        if name not in self.sems:
            self.sems[name] = self.stack.enter_context(self.nc.semaphore(name))
        return self.sems[name]

    def emit(self, eng, fn, sig=False):
        self.fence_last[eng] = False
        ent = [fn, None]
        self.q[eng].append(ent)
        if eng in ("act", "dve", "pool"):
            pr = _Probe()
            fn(pr)
            if pr.small():
                ev = self._sig(eng, ent)
                sm = self.sem(ev[3])
                self.q[eng].append([lambda e, sm=sm, v=ev[2]: e.wait_ge(sm, v), None, "wait"])
                return ev
        if sig:
            return self._sig(eng, ent)
        return None

    def _sig(self, eng, ent):
        self.cnt[eng] += 1
        nm = "p_%s_%d" % (eng, self.epoch)
        ent[1] = self.sem(nm)
        self.last_sig[eng] = ("e", eng, self.cnt[eng], nm)
        return self.last_sig[eng]

    def init_fences(self):
        self.fz = {e: self.sb([128, 64], F32, "fz_" + e) for e in ("act", "dve", "pool")}
        self.fence_last = {e: False for e in ENGS}

    def sig_last(self, eng):
        last = None
        for ent in reversed(self.q[eng]):
            if len(ent) == 3:
                continue
            last = ent
            break
        if last is None:
            return None
        if eng in self.fz:
            if self.fence_last[eng] and eng in self.last_sig:
                return self.last_sig[eng]
            fz = self.fz[eng]
            if eng == "act":
                ent = [lambda e: e.copy(out=fz[:, :], in_=fz[:, :]), None]
            else:
                ent = [lambda e: e.tensor_copy(out=fz[:, :], in_=fz[:, :]), None]
            self.q[eng].append(ent)
            self.fence_last[eng] = True
            return self._sig(eng, ent)
        if last[1] is None:
            return self._sig(eng, last)
        if eng in self.last_sig:
            return self.last_sig[eng]
        return None

    def wait(self, eng, ev):
        if ev is None:
            return
        if isinstance(ev, list):
            for x in ev:
                self.wait(eng, x)
            return
        kind, key, val = ev[0], ev[1], ev[2]
        if kind == "e" and key == eng:
            return
        semname = ev[3] if kind == "e" else key
        if self.waited[eng].get(semname, 0) >= val:
            return
        self.waited[eng][semname] = val
        s = self.sem(semname)
        self.q[eng].append([lambda e, s=s, val=val: e.wait_ge(s, val), None, "wait"])

    def dma(self, qeng, out, in_, semname, **kw):
        s = self.sem(semname)
        self.dcnt[semname] = self.dcnt.get(semname, 0) + 16
        self.q[qeng].append([lambda e, out=out, in_=in_, s=s, kw=kw: e.dma_start(out=out, in_=in_, **kw).then_inc(s, 16), None, "dma"])
        return ("d", semname, self.dcnt[semname])

    def dma_k(self, qeng, out, in_, semname, **kw):
        ev = None
        for k in range(out.shape[1]):
            ev = self.dma(qeng, out[:, k, :], in_[:, k, :], semname, **kw)
        return ev

    def new_epoch(self):
        self.barrier()
        self.epoch += 1
        self.cnt = {e: 0 for e in ENGS}
        self.fence_last = {e: False for e in ENGS}
        self.last_sig = {}

    def barrier(self, engs=ENGS):
        evs = [self.sig_last(e) for e in engs]
        for e in engs:
            self.wait(e, evs)

    def pe(self, fn, sig=False):
        return self.emit("pe", fn, sig)

    def act(self, fn, sig=False):
        return self.emit("act", fn, sig)

    def dve(self, fn, sig=False):
        return self.emit("dve", fn, sig)

    def pool(self, fn, sig=False):
        return self.emit("pool", fn, sig)

    def finish(self):
        nc = self.nc
        q = self.q

        def run(e, lst):
            for ent in lst:
                ins = ent[0](e)
                if ent[1] is not None:
                    ins.then_inc(ent[1], 1)

        with nc.Block() as block:
            @block.tensor
            def _(e):
                run(e, q["pe"])

            @block.scalar
            def _(e):
                run(e, q["act"])

            @block.vector
            def _(e):
                run(e, q["dve"])

            @block.gpsimd
            def _(e):
                run(e, q["pool"])

            @block.sync
            def _(e):
                run(e, q["sp"])
        self.stack.close()


class Ring:
    def __init__(self, banks):
        self.banks = banks
        self.free = [None] * len(banks)
        self.i = 0

    def next(self, P, eng="pe"):
        i = self.i
        self.i = (self.i + 1) % len(self.banks)
        P.wait(eng, self.free[i])
        return i, self.banks[i]

    def done(self, i, ev):
        self.free[i] = ev


def bcast_row(ap_row, n=128):
    return ap_row.partition_broadcast(n)[:, 0, :]


def build_program(n_layers=DEPTH, dbg=None, moe_experts=NE, stop=None, we=NE):
    dbg = dbg or {}
    nc = bass.Bass("TRN2", target_bir_lowering=False)
    P = Prog(nc)

    def din(name, shape, dt=F32):
        return nc.dram_tensor(name, list(shape), dt, kind="ExternalInput").ap()

    WD = n_layers
    x_in = din("x", [L, D])
    ctx_in = din("ctx", [LC, D])
    cc_in = din("cc", [2, D])
    w_mod = din("w_mod", [WD, D, 6 * D])
    b_mod = din("b_mod", [WD, 6 * D])
    norm1_w = din("norm1_w", [WD, D])
    norm2_w = din("norm2_w", [WD, D])
    w_in = din("w_in", [WD, D, 1280])
    q_norm_w = din("q_norm_w", [WD, 64])
    k_norm_w = din("k_norm_w", [WD, 64])
    attn_sink = din("attn_sink", [WD, 8])
    ssm_a_re = din("ssm_a_re", [WD, 2, 16, 64])
    ssm_a_im = din("ssm_a_im", [WD, 2, 16, 64])
    ssm_log_dt = din("ssm_log_dt", [WD, 2, 16])
    ssm_b_re = din("ssm_b_re", [WD, 2, 16, 64, 16])
    ssm_b_im = din("ssm_b_im", [WD, 2, 16, 64, 16])
    ssm_c_re = din("ssm_c_re", [WD, 2, 16, 16, 64])
    ssm_c_im = din("ssm_c_im", [WD, 2, 16, 16, 64])
    ssm_d = din("ssm_d", [WD, 256])
    glu_w = din("glu_w", [WD, 256, 256])
    glu_b = din("glu_b", [WD, 256])
    pool_w = din("pool_w", [WD, 4, 64, 64])
    pool_scale = din("pool_scale", [WD, 256])
    out_norm_w = din("out_norm_w", [WD, 768])
    w_out = din("w_out", [WD, D, D])
    router_w = din("router_w", [WD, D, NE])
    router_b = din("router_b", [WD, NE])
    exp_w_gu = din("exp_w_gu", [WD, we, D, 2 * D])
    exp_b_gu = din("exp_b_gu", [WD, we, 2 * D])
    exp_w_down = din("exp_w_down", [WD, we, D, D])
    exp_b_down = din("exp_b_down", [WD, we, D])
    c_ident = din("c_ident", [128, 128])
    c_rope = din("c_rope", [128, NXT, 128])
    c_mask = din("c_mask", [128, 2, 128])
    c_poolfix = din("c_poolfix", [128, 2, 2, 8])
    c_poolinv = din("c_poolinv", [128, 2])
    out = nc.dram_tensor("out", [L, D], F32, kind="ExternalOutput").ap()
    modscr = nc.dram_tensor("modscr", [DEPTH, 2, 6 * D], F32, kind="Internal").ap()
    dbg_out = {k: nc.dram_tensor("dbg_" + k, list(shp), F32, kind="ExternalOutput").ap() for k, shp in dbg.items()}

    psb = [nc.alloc_psum_tensor(f"ps{i}", [128, 512], F32) for i in range(8)]

    def psv(i, dt=F32):
        return psb[i][:, :] if dt == F32 else psb[i][:, :].bitcast(dt)

    P.init_fences()
    X = P.sb([128, NT, D], F32, "X")
    ident_f = P.sb([128, 128], F32, "identf")
    ident_b = P.sb([128, 128], BF16, "identb")
    ones_b = P.sb([128, 128], BF16, "onesb")
    masks = P.sb([128, 2, 128], BF16, "masks")
    poolfix = P.sb([128, 2, 2, 8], F32, "poolfix")
    poolinv = P.sb([128, 2], F32, "poolinv")
    epsc = P.sb([128, 1], F32, "epsc")
    halfpi = P.sb([128, 1], F32, "halfpi")
    base_mark = P.mark()

    def dump(name, src_ap, dst_ap=None):
        if name not in dbg_out:
            return
        P.barrier()
        ev = P.dma("pool", dbg_out[name] if dst_ap is None else dst_ap, src_ap, "dbgsem")
        for en in ENGS:
            P.wait(en, ev)
        P.barrier()

    def finish_prog():
        P.barrier()
        evs_o = [P.dma("sp", out[i * 128:(i + 1) * 128, :], X[:, i, :], "st_out") for i in range(NXT)]
        P.wait("sp", evs_o)
        P.finish()
        return nc

    m0 = P.mark()
    mstage = P.sb([128, 2, 128], F32, "mstage")
    evs = [P.dma("sp", ident_f[:, :], c_ident[:, :], "ld0"),
           P.dma("sp", mstage[:, :, :], c_mask[:, :, :], "ld0"),
           P.dma("sp", poolfix[:, :, :, :], c_poolfix[:, :, :, :], "ld0"),
           P.dma("sp", poolinv[:, :], c_poolinv[:, :], "ld0")]
    for i in range(NXT):
        evs.append(P.dma("sp", X[:, i, :], x_in[i * 128:(i + 1) * 128, :], "ld0"))
    for i in range(2):
        evs.append(P.dma("sp", X[:, NXT + i, :], ctx_in[i * 128:(i + 1) * 128, :], "ld0"))
    P.wait("dve", evs)
    for fe, ft in P.fz.items():
        P.emit(fe if fe != "act" else "dve", lambda e, ft=ft: e.memset(ft[:, :], 0.0))
    P.dve(lambda e: e.tensor_copy(out=ident_b[:, :], in_=ident_f[:, :]))
    P.dve(lambda e: e.tensor_copy(out=masks[:, :, :], in_=mstage[:, :, :]))
    P.dve(lambda e: e.memset(ones_b[:, :], 1.0))
    P.dve(lambda e: e.memset(epsc[:, :], EPS))
    P.dve(lambda e: e.memset(halfpi[:, :], math.pi / 2))
    P.barrier()
    P.release(m0)

    if stop == 'S0':
        return finish_prog()
    m0 = P.mark()
    ccs = P.sb([128, D], F32, "ccs")
    sil = P.sb([128, 8, 128], F32, "sil")
    wst = [P.sb([128, 8, 512], F32, f"wst{i}") for i in range(2)]
    brow = P.sb([2, 6 * D], F32, "brow")
    mrow = P.sb([2, 512], F32, "mrow")
    P.dve(lambda e: e.memset(ccs[:, :], 0.0))
    P.dve(lambda e: e.memset(sil[:, :, :], 0.0))
    evz = P.sig_last("dve")
    P.wait("sp", evz)
    ev = P.dma("sp", ccs[0:2, :], cc_in[:, :], "ld1")
    P.wait("act", ev)
    P.act(lambda e: e.activation(out=ccs[0:2, :], in_=ccs[0:2, :], func=AF.Silu))
    ev_a = P.sig_last("act")
    P.wait("pe", ev_a)
    for k in range(8):
        P.pe(lambda e, k=k: e.transpose(out=psv(k // 4)[:, (k % 4) * 128:(k % 4 + 1) * 128], in_=ccs[:, k * 128:(k + 1) * 128], identity=ident_f[:, :]))
    ev = P.sig_last("pe")
    P.wait("dve", ev)
    for hh in range(2):
        P.dve(lambda e, hh=hh: e.tensor_copy(out=sil[:, 4 * hh:4 * hh + 4, 0:2], in_=psv(hh).rearrange("p (k c) -> p k c", k=4)[:, :, 0:2]))
    ev_sil = P.sig_last("dve")
    P.wait("pe", ev_sil)
    if stop == 'S1a':
        return finish_prog()
    ring = Ring([2, 3])
    wfree = [None, None]
    mrow_free = None
    it = 0
    pend = None
    def flush(pend_):
        evd_, l_, ct_ = pend_
        P.wait("sp", evd_)
        e0_ = P.dma("sp", modscr[l_, 0:2, ct_ * 512:(ct_ + 1) * 512], mrow[0:2, :], "st1")
        P.wait("sp", e0_)
        return e0_
    for l in range(n_layers):
        if pend is not None:
            mrow_free = flush(pend)
            pend = None
        evb0 = [P.dma("sp", brow[r:r + 1, :], b_mod[l:l + 1, :], "ld1b") for r in range(2)]
        for ct in range(12):
            s = it % 2
            it += 1
            P.wait("sp", wfree[s])
            evw = P.dma_k("sp", wst[s][:, :, :], w_mod[l, :, ct * 512:(ct + 1) * 512].rearrange("(k p) n -> p k n", p=128), f"ld1w{s}")
            if pend is not None:
                mrow_free = flush(pend)
                pend = None
            bi, b = ring.next(P)
            P.wait("pe", evw)
            for k in range(8):
                P.pe(lambda e, k=k, s=s, b=b: e.matmul(psv(b)[:, :], lhsT=sil[:, k, :], rhs=wst[s][:, k, :], start=(k == 0), stop=(k == 7)))
            evp = P.sig_last("pe")
            wfree[s] = evp
            P.wait("dve", [evp, mrow_free] + evb0)
            P.dve(lambda e, b=b, ct=ct: e.tensor_tensor(out=mrow[0:2, :], in0=psv(b)[0:2, :], in1=brow[0:2, ct * 512:(ct + 1) * 512], op=ALU.add))
            evd = P.sig_last("dve")
            ring.done(bi, evd)
            pend = (evd, l, ct)
    if pend is not None:
        mrow_free = flush(pend)
    P.barrier()
    P.release(m0)

    if stop == 'S1':
        return finish_prog()
    def norm_mod_transpose(l, nw, shift_idx, scale_idx, hT, ntiles, router=None):
        m = P.mark()
        A = [P.sb([128, D], F32, "A0"), P.sb([128, D], F32, "A1")]
        S = [P.sb([128, D], F32, "S0"), P.sb([128, D], F32, "S1")]
        nwb = P.sb([128, D], F32, "nwb")
        junk = P.sb([128, D], BF16, "junk")
        hf = [P.sb([128, D], F32, f"hf{i}") for i in range(2)]
        st = P.sb([128, NT, 4], F32, "st")
        evs = [P.dma("sp", nwb[:, :], bcast_row(nw[l:l + 1, :]), "ldn")]
        for r in range(2):
            evs.append(P.dma("sp", A[r][:, :], bcast_row(modscr[l, r:r + 1, scale_idx * D:(scale_idx + 1) * D]), "ldn"))
            evs.append(P.dma("sp", S[r][:, :], bcast_row(modscr[l, r:r + 1, shift_idx * D:(shift_idx + 1) * D]), "ldn"))
        P.wait("dve", evs)
        for r in range(2):
            P.dve(lambda e, r=r: e.scalar_tensor_tensor(out=A[r][:, :], in0=A[r][:, :], scalar=1.0, in1=nwb[:, :], op0=ALU.add, op1=ALU.mult))
        if router is not None:
            rw32, rbb, LG, h32 = router
        ring_t = Ring([0, 2])
        hfree = [None, None]
        lg_ring = Ring([6, 7])
        for i in range(ntiles):
            r = 0 if i < NXT else 1
            s = i % 2
            P.act(lambda e, i=i: e.activation(out=junk[:, :], in_=X[:, i, :], func=AF.Square, accum_out=st[:, i, 0:1]))
            P.act(lambda e, i=i: e.activation(out=st[:, i, 1:2], in_=st[:, i, 0:1], func=AF.Sqrt, bias=epsc[:, 0:1], scale=1.0 / D))
            eva = P.sig_last("act")
            P.wait("dve", [eva, hfree[s]])
            P.dve(lambda e, i=i: e.reciprocal(out=st[:, i, 2:3], in_=st[:, i, 1:2]))
            P.dve(lambda e, i=i, r=r, s=s: e.scalar_tensor_tensor(out=hf[s][:, :], in0=X[:, i, :], scalar=st[:, i, 2:3], in1=A[r][:, :], op0=ALU.mult, op1=ALU.mult))
            P.dve(lambda e, r=r, s=s: e.tensor_tensor(out=hf[s][:, :], in0=hf[s][:, :], in1=S[r][:, :], op=ALU.add))
            evd = P.sig_last("dve")
            bi, b = ring_t.next(P)
            P.wait("pe", evd)
            for k in range(8):
                P.pe(lambda e, k=k, b=b, s=s: e.transpose(out=psv(b + k // 4)[:, (k % 4) * 128:(k % 4 + 1) * 128], in_=hf[s][:, k * 128:(k + 1) * 128], identity=ident_f[:, :]))
            evp = P.sig_last("pe")
            hfree[s] = evp
            P.wait("act", evp)
            for hh in range(2):
                P.act(lambda e, b=b, hh=hh, i=i: e.copy(out=hT[:, 4 * hh:4 * hh + 4, i * 128:(i + 1) * 128],
                                                       in_=psv(b + hh).rearrange("p (k t) -> p k t", k=4)))
            if router is not None:
                P.wait("act", h32_free[0])
                for hh in range(2):
                    P.act(lambda e, b=b, hh=hh: e.copy(out=h32[:, 4 * hh:4 * hh + 4, :], in_=psv(b + hh).rearrange("p (k t) -> p k t", k=4)))
            evc = P.sig_last("act")
            ring_t.done(bi, evc)
            if router is not None:
                li, lb = lg_ring.next(P)
                P.wait("pe", evc)
                for k in range(8):
                    P.pe(lambda e, k=k, lb=lb: e.matmul(psv(lb)[:, 0:NE], lhsT=h32[:, k, :], rhs=rw32[:, k, :], start=(k == 0), stop=(k == 7)))
                evl = P.sig_last("pe")
                h32_free[0] = evl
                P.wait("dve", evl)
                P.dve(lambda e, lb=lb, i=i: e.tensor_tensor(out=LG[:, i, :], in0=psv(lb)[:, 0:NE], in1=rbb[:, :], op=ALU.add))
                lg_ring.done(li, P.sig_last("dve"))
        P.barrier()
        P.release(m)

    h32_free = [None]
    pl_free = [None]
    pool_ev = [None]
    dve_step_ev = [None]

    R1 = (P.mark() + 31) // 32 * 32
    R2 = R1 + 36864
    R3 = R2 + 9216
    PW = 8 + L + 8 + 8 + LC + 8
    R4 = R3 + 2 * PW * 4
    R5 = R4 + 18432 + 18432 + 4704
    for l in range(n_layers):
        last = l == DEPTH - 1
        P.new_epoch()
        P.release(R1)
        hT = P.sb([128, 8, T], BF16, "hT")
        mergedT = hT
        uT = P.sb([128, 2, T], BF16, "uT")
        poolP = P.sb([128, 2, PW], F32, "poolP")
        qT = P.sb([128, 4, T], BF16, "qT")
        kT2 = P.sb([128, 2, 2, T], BF16, "kT2")
        Vaug = P.sb([128, NT, 2, 65], BF16, "Vaug")
        assert P.mark() <= R5, (P.mark(), R5)
        P.release(R4)
        norm_mod_transpose(l, norm1_w, 0, 1, hT, NT)
        if l == 0:
            dump("hT", hT[:, :, :].rearrange("p k t -> p (k t)"))
        if stop == 'L1':
            return finish_prog()
        P.release(R5)
        win = P.sb([128, 8, 768], BF16, "win")
        ropet = [P.sb([128, 128], F32, f"ropet{i}") for i in range(2)]
        NW = P.sb([128, 10, 64], F32, "NW")
        QK = [P.sb([128, 1024], BF16, f"QK{i}") for i in range(2)]
        tq = [P.sb([128, 640], F32, f"tq{i}") for i in range(2)]
        tr = P.sb([128, 640], F32, "tr")
        ss = P.sb([128, NT, 32], F32, "ss")
        evw = P.dma_k("pool", win[:, :, :], w_in[l, :, 0:768].rearrange("(k p) n -> p k n", p=128), "ldw_in")
        evr = []
        for h in range(10):
            src = q_norm_w if h < 8 else k_norm_w
            evr.append(P.dma("sp", NW[:, h, :], bcast_row(src[l:l + 1, :]), "ld2"))
        P.dve(lambda e: e.memset(Vaug[:, :, :, 64:65], 1.0))
        P.dve(lambda e: e.memset(poolP[:, :, :], 0.0))
        for qq in QK:
            P.dve(lambda e, qq=qq: e.memset(qq[:, :], 0.0))
        P.wait("pe", evw)
        P.wait("dve", evr)
        ringA = Ring([0, 1])
        ringB = Ring([2, 3])
        ringT = Ring([4, 5])
        qkfree = [None, None]
        ropefree = [None, None]
        for i in range(NT):
            s = i % 2
            if i < NXT:
                P.wait("sp", ropefree[s])
                evrope = P.dma("sp", ropet[s][:, :], c_rope[:, i, :], "ld2r%d" % s)
            ai, a = ringA.next(P)
            bi, b = ringB.next(P)
            for k in range(8):
                P.pe(lambda e, k=k, a=a, i=i: e.matmul(psv(a)[:, :], lhsT=hT[:, k, i * 128:(i + 1) * 128], rhs=win[:, k, 0:512], start=(k == 0), stop=(k == 7)))
            for k in range(8):
                P.pe(lambda e, k=k, b=b, i=i: e.matmul(psv(b)[:, 0:256], lhsT=hT[:, k, i * 128:(i + 1) * 128], rhs=win[:, k, 512:768], start=(k == 0), stop=(k == 7)))
            evp = P.sig_last("pe")
            P.wait("act", evp)
            P.act(lambda e, b=b, i=i: e.copy(out=Vaug[:, i, :, 0:64], in_=psv(b)[:, 128:256].rearrange("p (g d) -> p g d", g=2)))
            P.wait("dve", [evp, qkfree[s]])
            t = tq[s]
            P.dve(lambda e, a=a, t=t: e.tensor_copy(out=t[:, 0:512], in_=psv(a)[:, :]))
            P.dve(lambda e, b=b, t=t: e.tensor_copy(out=t[:, 512:640], in_=psv(b)[:, 0:128]))
            evcp = P.sig_last("dve")
            P.dve(lambda e, t=t: e.tensor_tensor(out=tr[:, :], in0=t[:, :], in1=t[:, :], op=ALU.mult))
            P.dve(lambda e, i=i: e.tensor_reduce(out=ss[:, i, 0:10], in_=tr[:, :].rearrange("p (h d) -> p h d", d=64), axis=AX.X, op=ALU.add))
            evd = P.sig_last("dve")
            P.wait("act", evd)
            P.act(lambda e, i=i: e.activation(out=ss[:, i, 10:20], in_=ss[:, i, 0:10], func=AF.Sqrt, bias=epsc[:, 0:1], scale=1.0 / 64))
            eva = P.sig_last("act")
            ringA.done(ai, evcp)
            ringB.done(bi, eva)
            P.wait("dve", eva)
            P.dve(lambda e, i=i: e.reciprocal(out=ss[:, i, 20:30], in_=ss[:, i, 10:20]))
            t3 = t[:, :].rearrange("p (h d) -> p h d", d=64)
            P.dve(lambda e, i=i, t3=t3: e.tensor_tensor(out=t3, in0=t3, in1=ss[:, i, 20:30].unsqueeze(2).broadcast_to([128, 10, 64]), op=ALU.mult))
            P.dve(lambda e, t3=t3: e.tensor_tensor(out=t3, in0=t3, in1=NW[:, :, :], op=ALU.mult))
            qk = QK[s]
            if i < NXT:
                P.wait("dve", evrope)
                rp = ropet[s]
                t5 = t[:, :].rearrange("p (h x f d) -> p h x f d", h=10, x=2, f=2)
                r5 = tr[:, :].rearrange("p (h x f d) -> p h x f d", h=10, x=2, f=2)
                sn = rp[:, 64:128].rearrange("p (x f d) -> p x f d", x=2, f=2)
                cs = rp[:, 0:64]
                for f in range(2):
                    P.dve(lambda e, f=f, t5=t5, r5=r5, sn=sn: e.tensor_tensor(out=r5[:, :, :, f, :], in0=t5[:, :, :, 1 - f, :],
                                                                             in1=sn[:, :, f, :].unsqueeze(1).broadcast_to([128, 10, 2, 16]), op=ALU.mult))
                P.dve(lambda e, t3=t3, cs=cs: e.tensor_tensor(out=t3, in0=t3, in1=cs.unsqueeze(1).broadcast_to([128, 10, 64]), op=ALU.mult))
                P.dve(lambda e, t=t, qk=qk: e.tensor_tensor(out=qk[:, 0:512], in0=t[:, 0:512], in1=tr[:, 0:512], op=ALU.add))
                for g in range(2):
                    for dup in range(2):
                        P.dve(lambda e, t=t, qk=qk, g=g, dup=dup: e.tensor_tensor(out=qk[:, 512 + g * 256 + dup * 192:512 + g * 256 + dup * 192 + 64],
                                                                               in0=t[:, 512 + g * 64:576 + g * 64], in1=tr[:, 512 + g * 64:576 + g * 64], op=ALU.add))
                ropefree[s] = P.sig_last("dve")
            else:
                P.dve(lambda e, t=t, qk=qk: e.tensor_copy(out=qk[:, 0:512], in_=t[:, 0:512]))
                for g in range(2):
                    for dup in range(2):
                        P.dve(lambda e, t=t, qk=qk, g=g, dup=dup: e.tensor_copy(out=qk[:, 512 + g * 256 + dup * 192:512 + g * 256 + dup * 192 + 64],
                                                                             in_=t[:, 512 + g * 64:576 + g * 64]))
            evq = P.sig_last("dve")
            ti, tb = ringT.next(P)
            P.wait("pe", evq)
            tp = psv(tb, BF16)
            for c in range(8):
                P.pe(lambda e, c=c, tp=tp, qk=qk: e.transpose(out=tp[:, c * 128:(c + 1) * 128], in_=qk[:, c * 128:(c + 1) * 128], identity=ident_b[:, :]))
            evt = P.sig_last("pe")
            qkfree[s] = evt
            P.wait("act", evt)
            P.act(lambda e, tp=tp, i=i: e.copy(out=qT[:, :, i * 128:(i + 1) * 128], in_=tp[:, 0:512].rearrange("p (c t) -> p c t", c=4)))
            P.act(lambda e, tp=tp, i=i: e.copy(out=kT2[:, :, :, i * 128:(i + 1) * 128], in_=tp[:, 512:1024].rearrange("p (g h t) -> p g h t", g=2, h=2)))
            ringT.done(ti, P.sig_last("act"))
        evpe = P.sig_last("pe")
        P.wait("pool", evpe)
        evw = P.dma_k("pool", win[:, :, 0:512], w_in[l, :, 768:1280].rearrange("(k p) n -> p k n", p=128), "ldw_in")
        P.wait("pe", evw)
        ringU = Ring([6, 7])
        for m_ in range(4):
            for (t0, tn) in TSL:
                ui, ub = ringU.next(P)
                for k in range(8):
                    P.pe(lambda e, k=k, ub=ub, m_=m_, t0=t0, tn=tn: e.matmul(psv(ub)[:, 0:tn], lhsT=win[:, k, m_ * 128:(m_ + 1) * 128],
                                                                          rhs=hT[:, k, t0:t0 + tn], start=(k == 0), stop=(k == 7)))
                evp = P.sig_last("pe")
                P.wait("act", evp)
                if m_ < 2:
                    P.act(lambda e, ub=ub, m_=m_, t0=t0, tn=tn: e.copy(out=uT[:, m_, t0:t0 + tn], in_=psv(ub)[:, 0:tn]))
                else:
                    po = 8 + t0 if t0 < L else 8 + L + 8 + 8
                    P.act(lambda e, ub=ub, m_=m_, po=po, tn=tn: e.copy(out=poolP[:, m_ - 2, po:po + tn], in_=psv(ub)[:, 0:tn]))
                ringU.done(ui, P.sig_last("act"))
        P.barrier()
        if l == 0:
            dump("qT", qT[:, :, :].rearrange("p k t -> p (k t)"))
            dump("kT2", kT2[:, :, :, :].rearrange("p g h t -> p (g h t)"))
            dump("uT", uT[:, :, :].rearrange("p k t -> p (k t)"))
            dump("poolP", poolP[:, :, :].rearrange("p k t -> p (k t)"))

        if stop == 'L2':
            return finish_prog()
        P.release(R5)
        esink = P.sb([128, 8], F32, "esink")
        ONW = P.sb([128, 512], F32, "ONW")
        PT = [P.sb([128, 512], BF16, f"PT{i}") for i in range(6)]
        Otm = P.sb([128, 512], F32, "Otm")
        Ob = P.sb([128, 512], BF16, "Ob")
        junk3 = P.sb([128, 512], BF16, "junk3")
        den = P.sb([128, 8], F32, "den")
        st3 = P.sb([128, NT, 4], F32, "st3")
        ev1 = P.dma("sp", esink[:, :], bcast_row(attn_sink[l:l + 1, :]), "ld3")
        ev2 = P.dma("sp", ONW[:, :], bcast_row(out_norm_w[l:l + 1, 0:512]), "ld3")
        P.wait("act", ev1)
        P.act(lambda e: e.activation(out=esink[:, :], in_=esink[:, :], func=AF.Exp))
        ev_es = P.sig_last("act")
        P.wait("dve", [ev_es, ev2])
        ringS = Ring([0, 1, 2])
        ringO = Ring([3, 4])
        ringT = Ring([5, 6])
        ptfree = [None] * 6
        pti = 0
        o_free = None
        ob_free = None
        qtiles = list(range(NXT)) + ([] if last else [NXT, NXT + 1])
        for n in qtiles:
            if n < NXT:
                kbs = ([(n - 1, 0)] if n > 0 else []) + [(n, None)] + ([(n + 1, 1)] if n < NXT - 1 else []) + [(NXT, None), (NXT + 1, None)]
            else:
                kbs = [(NXT, None), (NXT + 1, None)]
            for g in range(2):
                pts = []
                for (kb, mk) in kbs:
                    si, sbk = ringS.next(P)
                    for hp in range(2):
                        P.pe(lambda e, hp=hp, sbk=sbk, g=g, kb=kb, n=n: e.matmul(psv(sbk)[:, hp * 256:(hp + 1) * 256],
                                                                                lhsT=kT2[:, g, hp, kb * 128:(kb + 1) * 128],
                                                                                rhs=qT[:, 2 * g:2 * g + 2, n * 128:(n + 1) * 128],
                                                                                start=True, stop=True))
                    evp = P.sig_last("pe")
                    pslot = pti % 6
                    pti += 1
                    P.wait("act", [evp, ptfree[pslot]])
                    pt = PT[pslot]
                    P.act(lambda e, pt=pt, sbk=sbk: e.activation(out=pt[:, :], in_=psv(sbk)[:, :], func=AF.Exp, scale=ATT_SCALE))
                    eva = P.sig_last("act")
                    ringS.done(si, eva)
                    if mk is not None:
                        P.wait("dve", eva)
                        P.dve(lambda e, pt=pt, mk=mk: e.tensor_tensor(out=pt[:, :].rearrange("p (s q) -> p s q", s=4), in0=pt[:, :].rearrange("p (s q) -> p s q", s=4),
                                                                      in1=masks[:, mk, :].unsqueeze(1).broadcast_to([128, 4, 128]), op=ALU.mult))
                        eva = P.sig_last("dve")
                    pts.append((pslot, pt, kb, eva))
                oi, ob = ringO.next(P)
                ops = psv(ob)[:, 0:260].rearrange("p (s d) -> p s d", d=65)
                for s_ in range(4):
                    for j, (pslot, pt, kb, eva) in enumerate(pts):
                        P.wait("pe", eva)
                        P.pe(lambda e, s_=s_, pt=pt, kb=kb, g=g, j=j, ops=ops, npt=len(pts): e.matmul(ops[:, s_, :], lhsT=pt[:, s_ * 128:(s_ + 1) * 128],
                                                                                                     rhs=Vaug[:, kb, g, :], start=(j == 0), stop=(j == npt - 1)))
                evo = P.sig_last("pe")
                for (pslot, pt, kb, eva) in pts:
                    ptfree[pslot] = evo
                P.wait("dve", [evo, o_free])
                es_v = esink[:, :].rearrange("p (g pl hp) -> p g hp pl", g=2, pl=2)[:, g]
                P.dve(lambda e, ops=ops, es_v=es_v: e.tensor_tensor(out=den[:, 0:4].rearrange("p (a b) -> p a b", a=2), in0=ops[:, :, 64].rearrange("p (a b) -> p a b", a=2), in1=es_v, op=ALU.add))
                P.dve(lambda e: e.reciprocal(out=den[:, 4:8], in_=den[:, 0:4]))
                o_v = Otm[:, :].rearrange("p (g pl hp d) -> p g hp pl d", g=2, pl=2, hp=2)[:, g]
                for hp in range(2):
                    P.dve(lambda e, hp=hp, ops=ops, o_v=o_v: e.tensor_tensor(out=o_v[:, hp], in0=ops[:, 2 * hp:2 * hp + 2, 0:64],
                                                                          in1=den[:, 4 + 2 * hp:6 + 2 * hp].unsqueeze(2).broadcast_to([128, 2, 64]), op=ALU.mult))
                ringO.done(oi, P.sig_last("dve"))
            evd = P.sig_last("dve")
            P.wait("act", evd)
            P.act(lambda e, n=n: e.activation(out=junk3[:, :], in_=Otm[:, :], func=AF.Square, accum_out=st3[:, n, 0:1]))
            P.act(lambda e, n=n: e.activation(out=st3[:, n, 1:2], in_=st3[:, n, 0:1], func=AF.Sqrt, bias=epsc[:, 0:1], scale=1.0 / 512))
            eva = P.sig_last("act")
            P.wait("dve", [eva, ob_free])
            P.dve(lambda e, n=n: e.reciprocal(out=st3[:, n, 2:3], in_=st3[:, n, 1:2]))
            P.dve(lambda e, n=n: e.scalar_tensor_tensor(out=Ob[:, :], in0=Otm[:, :], scalar=st3[:, n, 2:3], in1=ONW[:, :], op0=ALU.mult, op1=ALU.mult))
            evd = P.sig_last("dve")
            o_free = evd
            ti, tb = ringT.next(P)
            P.wait("pe", evd)
            tp = psv(tb, BF16)
            for c in range(4):
                P.pe(lambda e, c=c, tp=tp: e.transpose(out=tp[:, c * 128:(c + 1) * 128], in_=Ob[:, c * 128:(c + 1) * 128], identity=ident_b[:, :]))
            evt = P.sig_last("pe")
            ob_free = evt
            P.wait("act", evt)
            P.act(lambda e, tp=tp, n=n: e.copy(out=mergedT[:, 0:4, n * 128:(n + 1) * 128], in_=tp[:, 0:512].rearrange("p (c t) -> p c t", c=4)))
            ringT.done(ti, P.sig_last("act"))
        P.barrier()

        if l == 0:
            dump("mT3", mergedT[:, :, :].rearrange("p k t -> p (k t)"))
        if stop == 'L3':
            return finish_prog()
        P.release(R5)
        pa = P.sb([128, PW], F32, "pa")
        pb_ = P.sb([128, PW], F32, "pb")
        dlt = P.sb([128, 2, T], BF16, "dlt")
        dfx = P.sb([128, 16], F32, "dfx")
        pwst = P.sb([128, 2, 128], F32, "pwst")
        pwb = P.sb([128, 2, 128], BF16, "pwb")
        psc = P.sb([128, 2], F32, "psc")
        P.dve(lambda e: e.memset(pwst[:, :, :], 0.0))
        evz = P.sig_last("dve")
        P.wait("sp", evz)
        evs = []
        for ct in range(2):
            for hh in range(2):
                evs.append(P.dma("sp", pwst[hh * 64:(hh + 1) * 64, ct, hh * 64:(hh + 1) * 64], pool_w[l, 2 * ct + hh, :, :], "ld4"))
            evs.append(P.dma("sp", psc[:, ct:ct + 1], pool_scale[l, ct * 128:(ct + 1) * 128].rearrange("(p o) -> p o", o=1), "ld4"))
        P.wait("dve", evs)
        P.wait("act", evs)
        P.dve(lambda e: e.tensor_copy(out=pwb[:, :, :], in_=pwst[:, :, :]))
        segs = [(8, L, 0), (8 + L + 8 + 8, LC, L)]
        for ct in range(2):
            for hh in range(2):
                w = POOLW[2 * ct + hh]
                pr = slice(hh * 64, (hh + 1) * 64)
                for (po, ln, tok0) in segs:
                    lo = po - 8
                    n_all = ln + 16
                    src = poolP[pr, ct, lo:lo + n_all]
                    bufs = [pa[pr, 0:n_all], pb_[pr, 0:n_all]]
                    cur = src
                    sh = 1
                    bi = 0
                    while sh < w:
                        dst = bufs[bi]
                        P.dve(lambda e, dst=dst, cur=cur, sh=sh, n_all=n_all: e.tensor_tensor(out=dst[:, sh:n_all], in0=cur[:, sh:n_all], in1=cur[:, 0:n_all - sh], op=ALU.add))
                        cur = dst
                        bi ^= 1
                        sh *= 2
                    o0 = 8 + w // 2 - 1
                    P.dve(lambda e, cur=cur, o0=o0, ln=ln, w=w, ct=ct, pr=pr, po=po, tok0=tok0: e.scalar_tensor_tensor(
                        out=dlt[pr, ct, tok0:tok0 + ln], in0=cur[:, o0:o0 + ln], scalar=1.0 / w, in1=poolP[pr, ct, po:po + ln], op0=ALU.mult, op1=ALU.subtract))
                    hw = w // 2
                    for side in range(2):
                        tb0 = 0 if side == 0 else ln - hw
                        P.dve(lambda e, cur=cur, o0=o0, tb0=tb0, hw=hw, pr=pr, ct=ct, side=side: e.tensor_tensor(
                            out=dfx[pr, 0:hw], in0=cur[:, o0 + tb0:o0 + tb0 + hw], in1=poolfix[pr, ct, side, 0:hw], op=ALU.mult))
                        P.dve(lambda e, tb0=tb0, hw=hw, pr=pr, ct=ct, po=po, tok0=tok0: e.tensor_tensor(
                            out=dlt[pr, ct, tok0 + tb0:tok0 + tb0 + hw], in0=dfx[pr, 0:hw], in1=poolP[pr, ct, po + tb0:po + tb0 + hw], op=ALU.subtract))
        evd = P.sig_last("dve")
        P.wait("pe", evd)
        ringP = Ring([0, 1])
        for ct in range(2):
            for (t0, tn) in TSL:
                pi, pbk = ringP.next(P)
                P.pe(lambda e, ct=ct, t0=t0, tn=tn, pbk=pbk: e.matmul(psv(pbk)[:, 0:tn], lhsT=pwb[:, ct, :], rhs=dlt[:, ct, t0:t0 + tn], start=True, stop=True))
                evp = P.sig_last("pe")
                P.wait("act", evp)
                P.act(lambda e, ct=ct, t0=t0, tn=tn, pbk=pbk: e.activation(out=mergedT[:, 6 + ct, t0:t0 + tn], in_=psv(pbk)[:, 0:tn], func=AF.Copy, scale=psc[:, ct:ct + 1]))
                ringP.done(pi, P.sig_last("act"))
        P.barrier()

        if l == 0:
            dump("mT4", mergedT[:, :, :].rearrange("p k t -> p (k t)"))
        if stop == 'L4':
            return finish_prog()
        P.release(R3)
        prm = P.sb([128, 12, 2, 8], F32, "prm")
        upw = P.sb([128, 2, 8, 3, 12], F32, "upw")
        rcol = P.sb([128, 2, 8], F32, "rcol")
        TP = P.sb([128, 2, 16, 16], F32, "TP")
        tt1 = P.sb([128, 16, 8], F32, "tt1")
        tt2 = P.sb([128, 16, 8], F32, "tt2")
        Cs = P.sb([128, 2, 2, 8, 16], F32, "Cs")
        bbz = P.sb([128, 2, 128], BF16, "bbz")
        BzT = P.sb([128, 2, 2, 128], BF16, "BzT")
        Cz = P.sb([128, 2, 2, 128], BF16, "Cz")
        pl = [P.sb([128, T], F32, f"pl{i}") for i in range(6)]
        sbf = mergedT[:, 4:6, :]
        bst = pl[5][:, 0:512].rearrange("p (d r j q) -> p d r j q", d=2, r=2, j=8)
        yT = P.sb([128, 2, T], F32, "yT")
        dcol = P.sb([128, 8], F32, "dcol")
        gst = P.sb([128, 2, 256], F32, "gst")
        gwb = P.sb([128, 2, 256], BF16, "gwb")
        bbr = P.sb([128, 2, 8, 16], F32, "bbr")
        bbi = P.sb([128, 2, 8, 16], F32, "bbi")
        btmp = pl[4][:, 0:256].rearrange("p (d j q) -> p d j q", d=2, j=8)
        evs = []
        for gl in range(2):
            pr = slice(gl * 64, (gl + 1) * 64)
            for d in range(2):
                evs.append(P.dma("sp", prm[pr, 0, d, :], ssm_a_re[l, d].rearrange("(j gl) n -> gl n j", gl=2)[gl], "ld5", allow_slow_non_contiguous=True))
                evs.append(P.dma("sp", prm[pr, 1, d, :], ssm_a_im[l, d].rearrange("(j gl) n -> gl n j", gl=2)[gl], "ld5", allow_slow_non_contiguous=True))
                evs.append(P.dma("sp", prm[pr, 2, d, :], ssm_log_dt[l, d:d + 1, :].rearrange("o (j gl) -> gl o j", gl=2)[gl].partition_broadcast(64)[:, 0, :],
                                 "ld5", allow_slow_non_contiguous=True))
                for ri, src in enumerate((ssm_b_re, ssm_b_im)):
                    evs.append(P.dma("sp", bst[pr, d, ri, :, :], src[l, d].rearrange("(j gl) n p -> gl n j p", gl=2)[gl], "ld5"))
                for ri, src in enumerate((ssm_c_re, ssm_c_im)):
                    for jj in range(8):
                        evs.append(P.dma("sp", Cs[pr, d, ri, jj, :], src[l, d, 2 * jj + gl].rearrange("p n -> n p"), "ld5c_%d" % l, allow_slow_non_contiguous=True))
        for m_ in range(2):
            evs.append(P.dma("sp", dcol[:, m_:m_ + 1], ssm_d[l, m_ * 128:(m_ + 1) * 128].rearrange("(p o) -> p o", o=1), "ld5"))
            evs.append(P.dma("sp", dcol[:, 2 + m_:3 + m_], glu_b[l, m_ * 128:(m_ + 1) * 128].rearrange("(p o) -> p o", o=1), "ld5"))
            evs.append(P.dma("sp", dcol[:, 4 + m_:5 + m_], out_norm_w[l, 512 + m_ * 128:512 + (m_ + 1) * 128].rearrange("(p o) -> p o", o=1), "ld5"))
        evs.append(P.dma_k("sp", gst[:, :, :], glu_w[l].rearrange("(k p) n -> p k n", p=128), "ld5"))
        P.wait("act", evs)
        P.wait("dve", evs)
        if stop == 'L5a':
            return finish_prog()
        f2 = lambda s_: prm[:, s_, :, :].rearrange("p d j -> p (d j)")
        a_re, a_im, ldt = f2(0), f2(1), f2(2)
        dt_, mag, th, cc_, sn_, ar, ai, t1, t2 = f2(3), f2(4), f2(5), f2(6), f2(7), f2(8), f2(9), f2(10), f2(11)
        P.act(lambda e: e.activation(out=dt_, in_=ldt, func=AF.Exp))
        eva = P.sig_last("act")
        P.wait("dve", eva)
        P.dve(lambda e: e.tensor_scalar(out=Cs[:, :, 1, :, :], in0=Cs[:, :, 1, :, :], scalar1=-1.0, scalar2=None, op0=ALU.mult))
        P.dve(lambda e: e.tensor_tensor(out=mag, in0=a_re, in1=dt_, op=ALU.mult))
        P.dve(lambda e: e.tensor_tensor(out=th, in0=a_im, in1=dt_, op=ALU.mult))
        evd = P.sig_last("dve")
        P.wait("act", evd)
        P.act(lambda e: e.activation(out=mag, in_=mag, func=AF.Exp))
        P.act(lambda e: e.activation(out=sn_, in_=th, func=AF.Sin, scale=1.0 / 16))
        P.act(lambda e: e.activation(out=cc_, in_=th, func=AF.Sin, scale=1.0 / 16, bias=halfpi[:, 0:1]))
        eva = P.sig_last("act")
        P.wait("dve", eva)
        for _ in range(4):
            P.dve(lambda e: e.tensor_tensor(out=t1, in0=cc_, in1=sn_, op=ALU.mult))
            P.dve(lambda e: e.tensor_tensor(out=cc_, in0=cc_, in1=cc_, op=ALU.mult))
            P.dve(lambda e: e.tensor_tensor(out=t2, in0=sn_, in1=sn_, op=ALU.mult))
            P.dve(lambda e: e.tensor_tensor(out=cc_, in0=cc_, in1=t2, op=ALU.subtract))
            P.dve(lambda e: e.tensor_scalar(out=sn_, in0=t1, scalar1=2.0, scalar2=None, op0=ALU.mult))
        P.dve(lambda e: e.tensor_tensor(out=t1, in0=cc_, in1=cc_, op=ALU.mult))
        P.dve(lambda e: e.tensor_tensor(out=t2, in0=sn_, in1=sn_, op=ALU.mult))
        P.dve(lambda e: e.tensor_tensor(out=t1, in0=t1, in1=t2, op=ALU.add))
        evd = P.sig_last("dve")
        P.wait("act", evd)
        P.act(lambda e: e.activation(out=ar, in_=t1, func=AF.Sqrt))
        eva = P.sig_last("act")
        P.wait("dve", eva)
        P.dve(lambda e: e.reciprocal(out=t2, in_=ar))
        P.dve(lambda e: e.tensor_tensor(out=ar, in0=t2, in1=t2, op=ALU.mult))
        P.dve(lambda e: e.tensor_tensor(out=ar, in0=ar, in1=t1, op=ALU.mult))
        P.dve(lambda e: e.tensor_scalar(out=ar, in0=ar, scalar1=-0.5, scalar2=1.5, op0=ALU.mult, op1=ALU.add))
        P.dve(lambda e: e.tensor_tensor(out=t2, in0=t2, in1=ar, op=ALU.mult))
        P.dve(lambda e: e.tensor_tensor(out=cc_, in0=cc_, in1=t2, op=ALU.mult))
        P.dve(lambda e: e.tensor_tensor(out=sn_, in0=sn_, in1=t2, op=ALU.mult))
        P.dve(lambda e: e.tensor_tensor(out=ar, in0=mag, in1=cc_, op=ALU.mult))
        P.dve(lambda e: e.tensor_tensor(out=ai, in0=mag, in1=sn_, op=ALU.mult))
        upv = lambda c, k: upw[:, :, :, c, k].rearrange("p d j -> p (d j)")
        P.dve(lambda e: e.tensor_copy(out=rcol[:, :, :].rearrange("p d j -> p (d j)"), in_=mag))
        P.dve(lambda e: e.tensor_copy(out=upv(0, 0), in_=cc_))
        P.dve(lambda e: e.tensor_copy(out=upv(1, 0), in_=sn_))
        for k in range(1, 12):
            P.dve(lambda e, k=k: e.tensor_tensor(out=t1, in0=upv(0, k - 1), in1=upv(0, k - 1), op=ALU.mult))
            P.dve(lambda e, k=k: e.tensor_tensor(out=t2, in0=upv(1, k - 1), in1=upv(1, k - 1), op=ALU.mult))
            P.dve(lambda e, k=k: e.tensor_tensor(out=upv(0, k), in0=t1, in1=t2, op=ALU.subtract))
            P.dve(lambda e, k=k: e.tensor_tensor(out=t1, in0=upv(0, k - 1), in1=upv(1, k - 1), op=ALU.mult))
            P.dve(lambda e, k=k: e.tensor_scalar(out=upv(1, k), in0=t1, scalar1=2.0, scalar2=None, op0=ALU.mult))
        P.dve(lambda e: e.tensor_scalar(out=upw[:, :, :, 2, :], in0=upw[:, :, :, 1, :], scalar1=-1.0, scalar2=None, op0=ALU.mult))
        P.dve(lambda e: e.memset(TP[:, 0, :, 0:1], 1.0))
        P.dve(lambda e: e.memset(TP[:, 1, :, 0:1], 0.0))
        for k in range(4):
            m = 1 << k
            ucb = upv(0, k).unsqueeze(2).broadcast_to([128, 16, m])
            usb = upv(1, k).unsqueeze(2).broadcast_to([128, 16, m])
            c_old, s_old = TP[:, 0, :, 0:m], TP[:, 1, :, 0:m]
            P.dve(lambda e, m=m, ucb=ucb, c_old=c_old: e.tensor_tensor(out=tt1[:, :, 0:m], in0=c_old, in1=ucb, op=ALU.mult))
            P.dve(lambda e, m=m, usb=usb, s_old=s_old: e.tensor_tensor(out=tt2[:, :, 0:m], in0=s_old, in1=usb, op=ALU.mult))
            P.dve(lambda e, m=m: e.tensor_tensor(out=TP[:, 0, :, m:2 * m], in0=tt1[:, :, 0:m], in1=tt2[:, :, 0:m], op=ALU.subtract))
            P.dve(lambda e, m=m, ucb=ucb, s_old=s_old: e.tensor_tensor(out=tt1[:, :, 0:m], in0=s_old, in1=ucb, op=ALU.mult))
            P.dve(lambda e, m=m, usb=usb, c_old=c_old: e.tensor_tensor(out=tt2[:, :, 0:m], in0=c_old, in1=usb, op=ALU.mult))
            P.dve(lambda e, m=m: e.tensor_tensor(out=TP[:, 1, :, m:2 * m], in0=tt1[:, :, 0:m], in1=tt2[:, :, 0:m], op=ALU.add))
        P.dve(lambda e: e.tensor_tensor(out=t1, in0=a_re, in1=a_re, op=ALU.mult))
        P.dve(lambda e: e.tensor_tensor(out=t2, in0=a_im, in1=a_im, op=ALU.mult))
        P.dve(lambda e: e.tensor_tensor(out=t1, in0=t1, in1=t2, op=ALU.add))
        P.dve(lambda e: e.reciprocal(out=dt_, in_=t1))
        P.dve(lambda e: e.tensor_scalar(out=cc_, in0=ar, scalar1=-1.0, scalar2=None, op0=ALU.add))
        P.dve(lambda e: e.tensor_tensor(out=t1, in0=cc_, in1=a_re, op=ALU.mult))
        P.dve(lambda e: e.tensor_tensor(out=t2, in0=ai, in1=a_im, op=ALU.mult))
        P.dve(lambda e: e.tensor_tensor(out=t1, in0=t1, in1=t2, op=ALU.add))
        P.dve(lambda e: e.tensor_tensor(out=mag, in0=t1, in1=dt_, op=ALU.mult))
        P.dve(lambda e: e.tensor_tensor(out=t1, in0=ai, in1=a_re, op=ALU.mult))
        P.dve(lambda e: e.tensor_tensor(out=t2, in0=cc_, in1=a_im, op=ALU.mult))
        P.dve(lambda e: e.tensor_tensor(out=t1, in0=t1, in1=t2, op=ALU.subtract))
        P.dve(lambda e: e.tensor_tensor(out=th, in0=t1, in1=dt_, op=ALU.mult))
        P.dve(lambda e: e.tensor_copy(out=gwb[:, :, :], in_=gst[:, :, :]))
        qr3 = prm[:, 4, :, :]
        qi3 = prm[:, 5, :, :]
        bq = lambda q3: q3.unsqueeze(3).broadcast_to([128, 2, 8, 16])
        P.dve(lambda e: e.tensor_tensor(out=bbr[:, :, :, :], in0=bst[:, :, 0, :, :], in1=bq(qr3), op=ALU.mult))
        P.dve(lambda e: e.tensor_tensor(out=btmp[:, :, :, :], in0=bst[:, :, 1, :, :], in1=bq(qi3), op=ALU.mult))
        P.dve(lambda e: e.tensor_tensor(out=bbr[:, :, :, :], in0=bbr[:, :, :, :], in1=btmp[:, :, :, :], op=ALU.subtract))
        P.dve(lambda e: e.tensor_tensor(out=bbi[:, :, :, :], in0=bst[:, :, 1, :, :], in1=bq(qr3), op=ALU.mult))
        P.dve(lambda e: e.tensor_tensor(out=btmp[:, :, :, :], in0=bst[:, :, 0, :, :], in1=bq(qi3), op=ALU.mult))
        P.dve(lambda e: e.tensor_tensor(out=bbi[:, :, :, :], in0=bbi[:, :, :, :], in1=btmp[:, :, :, :], op=ALU.add))
        if stop == 'L5b':
            return finish_prog()
        ringX = Ring([0, 1, 2, 3])
        ringY = Ring([4, 5])
        ringZ = Ring([6, 7])
        bz_free = None
        sbf_free = None
        y_started = [False, False]
        for j in range(8):
            mt = j // 4
            P.wait("dve", bz_free)
            for d in range(2):
                P.dve(lambda e: e.memset(bbz[:, :, :], 0.0))
                for ri, bb in enumerate((bbr, bbi)):
                    for gl in range(2):
                        c0 = 16 * ((2 * j + gl) % 8)
                        P.dve(lambda e, gl=gl, c0=c0, bb=bb, d=d, ri=ri, j=j: e.tensor_copy(out=bbz[gl * 64:(gl + 1) * 64, ri, c0:c0 + 16], in_=bb[gl * 64:(gl + 1) * 64, d, j, :]))
                evd = P.sig_last("dve")
                zi, zb = ringZ.next(P)
                P.wait("pe", evd)
                for ri in range(2):
                    P.pe(lambda e, ri=ri, zb=zb: e.transpose(out=psv(zb, BF16)[:, ri * 128:(ri + 1) * 128], in_=bbz[:, ri, :], identity=ident_b[:, :]))
                evp = P.sig_last("pe")
                P.wait("dve", evp)
                P.dve(lambda e, zb=zb, d=d: e.tensor_copy(out=BzT[:, d, :, :], in_=psv(zb, BF16)[:, 0:256].rearrange("p (r s) -> p r s", r=2)))
                ringZ.done(zi, P.sig_last("dve"))
                P.dve(lambda e, d=d: e.memset(Cz[:, d, :, :], 0.0))
                for ri in range(2):
                    for gl in range(2):
                        c0 = 16 * ((2 * j + gl) % 8)
                        P.dve(lambda e, gl=gl, c0=c0, d=d, ri=ri, j=j: e.tensor_copy(out=Cz[gl * 64:(gl + 1) * 64, d, ri, c0:c0 + 16], in_=Cs[gl * 64:(gl + 1) * 64, d, ri, j, :]))
            ev_mats = P.sig_last("dve")
            P.wait("pe", ev_mats)
            for d in range(2):
                for (t0, tn) in TSL:
                    c0 = (t0 + LC if t0 < L else 0) if d == 0 else t0
                    for ri in range(2):
                        xi, xb = ringX.next(P)
                        P.pe(lambda e, ri=ri, xb=xb, d=d, t0=t0, tn=tn, mt=mt: e.matmul(psv(xb)[:, 0:tn], lhsT=BzT[:, d, ri, :], rhs=uT[:, mt, t0:t0 + tn], start=True, stop=True))
                        evp = P.sig_last("pe")
                        P.wait("act", [evp] + (pl_free[0] or []))
                        P.act(lambda e, ri=ri, xb=xb, c0=c0, tn=tn: e.copy(out=pl[ri][:, c0:c0 + tn], in_=psv(xb)[:, 0:tn]))
                        ringX.done(xi, P.sig_last("act"))
                eva = P.sig_last("act")
                P.wait("dve", [eva, sbf_free])
                V = (lambda ap: ap) if d == 0 else (lambda ap: ap[:, ::-1])
                Tc, Ts, P5, P6 = pl[2], pl[3], pl[4], pl[5]
                xr, xi = V(pl[0][:, :]), V(pl[1][:, :])
                cmb = d * 8 + j
                P.dve(lambda e, cmb=cmb: e.tensor_copy(out=Tc[:, 0:16], in_=TP[:, 0, cmb, :]))
                P.dve(lambda e, cmb=cmb: e.tensor_copy(out=Ts[:, 0:16], in_=TP[:, 1, cmb, :]))
                for k in range(4, 12):
                    m = 1 << k
                    n = min(m, T - m)
                    uc = upw[:, d, j, 0, k:k + 1]
                    us = upw[:, d, j, 1, k:k + 1]
                    nus = upw[:, d, j, 2, k:k + 1]
                    P.dve(lambda e, m=m, n=n, uc=uc: e.tensor_scalar(out=Tc[:, m:m + n], in0=Tc[:, 0:n], scalar1=uc, scalar2=None, op0=ALU.mult))
                    P.dve(lambda e, m=m, n=n, nus=nus: e.scalar_tensor_tensor(out=Tc[:, m:m + n], in0=Ts[:, 0:n], scalar=nus, in1=Tc[:, m:m + n], op0=ALU.mult, op1=ALU.add))
                    P.dve(lambda e, m=m, n=n, uc=uc: e.tensor_scalar(out=Ts[:, m:m + n], in0=Ts[:, 0:n], scalar1=uc, scalar2=None, op0=ALU.mult))
                    P.dve(lambda e, m=m, n=n, us=us: e.scalar_tensor_tensor(out=Ts[:, m:m + n], in0=Tc[:, 0:n], scalar=us, in1=Ts[:, m:m + n], op0=ALU.mult, op1=ALU.add))
                P.dve(lambda e, xr=xr: e.tensor_tensor(out=P5[:, :], in0=Tc[:, :], in1=xr, op=ALU.mult))
                P.dve(lambda e, xi=xi: e.tensor_tensor(out=P6[:, :], in0=Ts[:, :], in1=xi, op=ALU.mult))
                P.dve(lambda e: e.tensor_tensor(out=P5[:, :], in0=P5[:, :], in1=P6[:, :], op=ALU.add))
                P.dve(lambda e, xr=xr: e.tensor_tensor(out=P6[:, :], in0=Ts[:, :], in1=xr, op=ALU.mult))
                P.dve(lambda e, xi=xi: e.tensor_tensor(out=xi, in0=Tc[:, :], in1=xi, op=ALU.mult))
                P.dve(lambda e, xi=xi: e.tensor_tensor(out=xi, in0=xi, in1=P6[:, :], op=ALU.subtract))
                rb = rcol[:, d, j:j + 1].broadcast_to([128, T])
                zr, zi = pl[0], P6
                P.dve(lambda e, rb=rb: e.tensor_tensor_scan(out=zr[:, :], data0=rb, data1=P5[:, :], initial=0.0, op0=ALU.mult, op1=ALU.add))
                P.dve(lambda e, rb=rb, xi=xi: e.tensor_tensor_scan(out=zi[:, :], data0=rb, data1=xi, initial=0.0, op0=ALU.mult, op1=ALU.add))
                tmp = pl[1]
                P.dve(lambda e: e.tensor_tensor(out=P5[:, :], in0=Tc[:, :], in1=zr[:, :], op=ALU.mult))
                P.dve(lambda e: e.tensor_tensor(out=tmp[:, :], in0=Ts[:, :], in1=zi[:, :], op=ALU.mult))
                so_re, so_im = V(sbf[:, 0, :]), V(sbf[:, 1, :])
                P.dve(lambda e, so_re=so_re: e.tensor_tensor(out=so_re, in0=P5[:, :], in1=tmp[:, :], op=ALU.subtract))
                P.dve(lambda e: e.tensor_tensor(out=P5[:, :], in0=Ts[:, :], in1=zr[:, :], op=ALU.mult))
                P.dve(lambda e: e.tensor_tensor(out=tmp[:, :], in0=Tc[:, :], in1=zi[:, :], op=ALU.mult))
                P.dve(lambda e, so_im=so_im: e.tensor_tensor(out=so_im, in0=P5[:, :], in1=tmp[:, :], op=ALU.add))
                pl_free[0] = [P.sig_last("dve")]
                P.wait("pe", pl_free[0])
                for (t0, tn) in TSL:
                    c0 = (t0 + LC if t0 < L else 0) if d == 0 else t0
                    yi, yb = ringY.next(P)
                    for ri in range(2):
                        P.pe(lambda e, d=d, ri=ri, yb=yb, c0=c0, tn=tn: e.matmul(psv(yb)[:, 0:tn], lhsT=Cz[:, d, ri, :], rhs=sbf[:, ri, c0:c0 + tn], start=(ri == 0), stop=(ri == 1)))
                    evp = P.sig_last("pe")
                    P.wait("dve", evp)
                    if not y_started[mt]:
                        P.dve(lambda e, yb=yb, t0=t0, tn=tn, mt=mt: e.tensor_copy(out=yT[:, mt, t0:t0 + tn], in_=psv(yb)[:, 0:tn]))
                    else:
                        P.dve(lambda e, yb=yb, t0=t0, tn=tn, mt=mt: e.tensor_tensor(out=yT[:, mt, t0:t0 + tn], in0=yT[:, mt, t0:t0 + tn], in1=psv(yb)[:, 0:tn], op=ALU.add))
                    ringY.done(yi, P.sig_last("dve"))
                y_started[mt] = True
                sbf_free = P.sig_last("pe")
            bz_free = P.sig_last("pe")
        P.barrier()
        if l == 0:
            dump("Tc", pl[2][:, :])
            dump("Ts", pl[3][:, :])
            dump("zr", pl[0][:, :])
        if stop == 'L5c':
            return finish_prog()
        if l == 0:
            dump("yT", yT[:, :, :].rearrange("p k t -> p (k t)"))
        g32 = pl[0]
        tA = pl[1]
        gb = pl[2][:, :].bitcast(BF16).rearrange("p (k t) -> p k t", k=2)
        sq = pl[3][:, :].bitcast(BF16).rearrange("p (k t) -> p k t", k=2)
        for m_ in range(2):
            y_ = yT[:, m_, :]
            P.dve(lambda e, m_=m_, y_=y_: e.scalar_tensor_tensor(out=y_, in0=uT[:, m_, :], scalar=dcol[:, m_:m_ + 1], in1=y_, op0=ALU.mult, op1=ALU.add))
            P.dve(lambda e, y_=y_: e.tensor_tensor(out=tA[:, :], in0=y_, in1=y_, op=ALU.mult))
            P.dve(lambda e: e.tensor_scalar(out=tA[:, :], in0=tA[:, :], scalar1=0.044715, scalar2=1.0, op0=ALU.mult, op1=ALU.add))
            P.dve(lambda e, y_=y_: e.tensor_tensor(out=tA[:, :], in0=tA[:, :], in1=y_, op=ALU.mult))
            evd = P.sig_last("dve")
            P.wait("act", evd)
            P.act(lambda e: e.activation(out=tA[:, :], in_=tA[:, :], func=AF.Sigmoid, scale=1.5957691216057308))
            eva = P.sig_last("act")
            P.wait("dve", eva)
            P.dve(lambda e, y_=y_: e.tensor_tensor(out=y_, in0=y_, in1=tA[:, :], op=ALU.mult))
            P.dve(lambda e, y_=y_, m_=m_: e.tensor_copy(out=gb[:, m_, :], in_=y_))
        evd = P.sig_last("dve")
        P.wait("pe", evd)
        P.wait("act", evd)
        ringG = Ring([0, 1])
        for m_ in range(2):
            for (t0, tn) in TSL:
                gi, gbk = ringG.next(P)
                for kt in range(2):
                    P.pe(lambda e, kt=kt, m_=m_, gbk=gbk, t0=t0, tn=tn: e.matmul(psv(gbk)[:, 0:tn], lhsT=gwb[:, kt, m_ * 128:(m_ + 1) * 128], rhs=gb[:, kt, t0:t0 + tn], start=(kt == 0), stop=(kt == 1)))
                evp = P.sig_last("pe")
                P.wait("act", evp)
                P.act(lambda e, m_=m_, gbk=gbk, t0=t0, tn=tn: e.activation(out=tA[:, t0:t0 + tn], in_=psv(gbk)[:, 0:tn], func=AF.Sigmoid, bias=dcol[:, 2 + m_:3 + m_]))
                eva = P.sig_last("act")
                ringG.done(gi, eva)
                P.wait("dve", eva)
                P.dve(lambda e, m_=m_, t0=t0, tn=tn: e.tensor_tensor(out=yT[:, m_, t0:t0 + tn], in0=yT[:, m_, t0:t0 + tn], in1=tA[:, t0:t0 + tn], op=ALU.mult))
                P.dve(lambda e, m_=m_, t0=t0, tn=tn: e.tensor_tensor(out=sq[:, m_, t0:t0 + tn], in0=yT[:, m_, t0:t0 + tn], in1=yT[:, m_, t0:t0 + tn], op=ALU.mult))
                evd = P.sig_last("dve")
                P.wait("act", evd)
        evd = P.sig_last("dve")
        P.wait("pe", evd)
        if l == 0:
            dump("ssm", yT[:, :, :].rearrange("p k t -> p (k t)"))
        ringG = Ring([2, 3])
        for (t0, tn) in TSL:
            gi, gbk = ringG.next(P)
            for kt in range(2):
                P.pe(lambda e, kt=kt, gbk=gbk, t0=t0, tn=tn: e.matmul(psv(gbk)[:, 0:tn], lhsT=ones_b[:, :], rhs=sq[:, kt, t0:t0 + tn], start=(kt == 0), stop=(kt == 1)))
            evp = P.sig_last("pe")
            P.wait("act", evp)
            P.act(lambda e, gbk=gbk, t0=t0, tn=tn: e.activation(out=g32[:, t0:t0 + tn], in_=psv(gbk)[:, 0:tn], func=AF.Sqrt, bias=epsc[:, 0:1], scale=1.0 / 256))
            eva = P.sig_last("act")
            ringG.done(gi, eva)
            P.wait("dve", eva)
            P.dve(lambda e, t0=t0, tn=tn: e.reciprocal(out=g32[:, t0:t0 + tn], in_=g32[:, t0:t0 + tn]))
            for m_ in range(2):
                P.dve(lambda e, m_=m_, t0=t0, tn=tn: e.scalar_tensor_tensor(out=mergedT[:, 4 + m_, t0:t0 + tn], in0=yT[:, m_, t0:t0 + tn], scalar=dcol[:, 4 + m_:5 + m_],
                                                                           in1=g32[:, t0:t0 + tn], op0=ALU.mult, op1=ALU.mult))
        P.barrier()
        if l == 0:
            dump("mergedT", mergedT[:, :, :].rearrange("p k t -> p (k t)"))

        if stop == 'L5':
            return finish_prog()
        P.release(R2)
        wob = P.sb([128, 8, D], BF16, "wob")
        g1 = [P.sb([128, D], F32, f"g1_{r}") for r in range(2)]
        tmp6 = [P.sb([128, 512], F32, f"tmp6_{i}") for i in range(2)]
        evw = P.dma_k("pool", wob[:, :, :], w_out[l].rearrange("(k p) n -> p k n", p=128), "ldw_out")
        evg = [P.dma("sp", g1[r][:, :], bcast_row(modscr[l, r:r + 1, 2 * D:3 * D]), "ld6") for r in range(2)]
        P.wait("pe", evw)
        P.wait("dve", evg)
        ring6 = Ring([0, 1, 2, 3])
        tfree = [None, None]
        it = 0
        for i in range(NXT if last else NT):
            r = 0 if i < NXT else 1
            for hf_ in range(2):
                oi, obk = ring6.next(P)
                for c in range(8):
                    P.pe(lambda e, c=c, obk=obk, i=i, hf_=hf_: e.matmul(psv(obk)[:, :], lhsT=mergedT[:, c, i * 128:(i + 1) * 128], rhs=wob[:, c, hf_ * 512:(hf_ + 1) * 512], start=(c == 0), stop=(c == 7)))
                evp = P.sig_last("pe")
                s = it % 2
                it += 1
                P.wait("dve", [evp, tfree[s]])
                P.dve(lambda e, obk=obk, r=r, hf_=hf_, s=s: e.tensor_tensor(out=tmp6[s][:, :], in0=psv(obk)[:, :], in1=g1[r][:, hf_ * 512:(hf_ + 1) * 512], op=ALU.mult))
                evd = P.sig_last("dve")
                ring6.done(oi, evd)
                P.wait("pool", evd)
                P.pool(lambda e, i=i, hf_=hf_, s=s: e.tensor_tensor(out=X[:, i, hf_ * 512:(hf_ + 1) * 512], in0=X[:, i, hf_ * 512:(hf_ + 1) * 512], in1=tmp6[s][:, :], op=ALU.add))
                tfree[s] = P.sig_last("pool")
        P.barrier()
        if l == 0:
            dump("xmix0", X[:, :, :].rearrange("p i d -> p (i d)"))
        if moe_experts == 0:
            continue

        P.new_epoch()
        ntm = NXT if last else NT
        P.release(R1)
        h2T = P.sb([128, 8, T], BF16, "h2T")
        Gs = P.sb([128, NT, NE], F32, "Gs")
        g2 = [P.sb([128, D], F32, f"g2_{r}") for r in range(2)]
        GTu = P.sb([128, NT, 128], BF16, "GTu")
        OFF_M = P.mark()
        LG = P.sb([128, NT, NE], F32, "LG")
        rwst = P.sb([128, 8, NE], F32, "rwst")
        rbb = P.sb([128, NE], F32, "rbb")
        h32 = P.sb([128, 8, 128], F32, "h32")
        m8 = P.sb([128, 8], F32, "m8")
        msk = P.sb([128, NE], F32, "msk")
        ex = P.sb([128, NE], F32, "ex")
        gu_ = P.sb([128, 128], F32, "gu_")
        st7 = P.sb([128, 4], F32, "st7")
        evs = [P.dma_k("sp", rwst[:, :, :], router_w[l].rearrange("(k p) n -> p k n", p=128), "ld7"),
               P.dma("sp", rbb[:, :], bcast_row(router_b[l:l + 1, :]), "ld7")]
        evs += [P.dma("sp", g2[r][:, :], bcast_row(modscr[l, r:r + 1, 5 * D:6 * D]), "ld7") for r in range(2)]
        P.wait("pe", evs)
        P.wait("dve", evs)
        h32_free[0] = None
        norm_mod_transpose(l, norm2_w, 3, 4, h2T, ntm, router=(rwst, rbb, LG, h32))
        ringG = Ring([0, 1])
        P.dve(lambda e: e.memset(gu_[:, :], 0.0))
        for i in range(ntm):
            P.dve(lambda e, i=i: e.max(out=m8[:, :], in_=LG[:, i, :]))
            P.dve(lambda e, i=i: e.tensor_scalar(out=msk[:, :], in0=LG[:, i, :], scalar1=m8[:, 3:4], scalar2=None, op0=ALU.is_ge))
            P.dve(lambda e: e.tensor_scalar(out=st7[:, 0:1], in0=m8[:, 0:1], scalar1=-1.0, scalar2=None, op0=ALU.mult))
            evd = P.sig_last("dve")
            P.wait("act", evd)
            P.act(lambda e, i=i: e.activation(out=ex[:, :], in_=LG[:, i, :], func=AF.Exp, bias=st7[:, 0:1]))
            eva = P.sig_last("act")
            P.wait("dve", eva)
            P.dve(lambda e: e.tensor_tensor(out=ex[:, :], in0=ex[:, :], in1=msk[:, :], op=ALU.mult))
            P.dve(lambda e: e.tensor_reduce(out=st7[:, 1:2], in_=ex[:, :], axis=AX.X, op=ALU.add))
            P.dve(lambda e: e.reciprocal(out=st7[:, 2:3], in_=st7[:, 1:2]))
            P.dve(lambda e: e.tensor_scalar(out=gu_[:, 0:NE], in0=ex[:, :], scalar1=st7[:, 2:3], scalar2=None, op0=ALU.mult))
            P.dve(lambda e, i=i: e.tensor_scalar(out=Gs[:, i, :], in0=gu_[:, 0:NE], scalar1=KSW, scalar2=None, op0=ALU.mult))
            evd = P.sig_last("dve")
            gi, gbk = ringG.next(P)
            P.wait("pe", evd)
            P.pe(lambda e, gbk=gbk: e.transpose(out=psv(gbk)[:, 0:128], in_=gu_[:, :], identity=ident_f[:, :]))
            evp = P.sig_last("pe")
            P.wait("act", evp)
            P.act(lambda e, gbk=gbk, i=i: e.copy(out=GTu[:, i, :], in_=psv(gbk)[:, 0:128]))
            eva = P.sig_last("act")
            ringG.done(gi, eva)
            P.wait("dve", eva)
        P.barrier()
        if l == 0:
            dump("gates0", Gs[:, :, :].rearrange("p i e -> p (i e)"))

        P.release(OFF_M)
        actT = P.sb([128, 8, T], BF16, "actT")
        wgu = [P.sb([128, 8, 256], BF16, f"wgu{i}") for i in range(2)]
        wdn = P.sb([128, 8, D], BF16, "wdn")
        bguT = P.sb([128, 16, NE], F32, "bguT")
        gcb = [P.sb([128, 512], F32, f"gc{i}") for i in range(2)]
        gsb = [P.sb([128, 512], F32, f"gs{i}") for i in range(2)]
        u1b = [P.sb([128, 512], F32, f"u1{i}") for i in range(2)]
        dtm = [P.sb([128, 512], F32, f"dt{i}") for i in range(2)]
        wd_u8 = wdn[:, :, :].rearrange("p k n -> p (k n)")
        bgall = wd_u8[:, 0:4096].bitcast(F32)
        bdn = wd_u8[:, 4096:6144].bitcast(F32)
        bdnb = wd_u8[:, 6144:7168]
        ringGU = Ring([0, 1, 2, 3])
        ringD = Ring([4, 5, 6, 7])
        wgu_free = [None, None]
        tmp_free = [None, None]
        dtm_free = [None, None]
        tsl_m = TSL if not last else TSL[:4]
        gq = 0
        blk = 0
        dblk = [0]

        def down_accum(lhs_fn, rhs_fn, nk, gate_fn, i_tiles):
            for i in i_tiles:
                r = 0 if i < NXT else 1
                for hf_ in range(2):
                    di, dbk = ringD.next(P)
                    for c in range(nk):
                        P.pe(lambda e, c=c, dbk=dbk, i=i, hf_=hf_: e.matmul(psv(dbk)[:, :], lhsT=lhs_fn(c, i), rhs=rhs_fn(c, hf_), start=(c == 0), stop=(c == nk - 1)))
                    evp = P.sig_last("pe")
                    s = dblk[0] % 2
                    dblk[0] += 1
                    P.wait("dve", [evp, dtm_free[s]])
                    gsc = gate_fn(i)
                    if gsc is None:
                        P.dve(lambda e, dbk=dbk, r=r, hf_=hf_, s=s: e.tensor_tensor(out=dtm[s][:, :], in0=psv(dbk)[:, :], in1=g2[r][:, hf_ * 512:(hf_ + 1) * 512], op=ALU.mult))
                    else:
                        P.dve(lambda e, dbk=dbk, r=r, hf_=hf_, s=s, gsc=gsc: e.scalar_tensor_tensor(out=dtm[s][:, :], in0=psv(dbk)[:, :], scalar=gsc, in1=g2[r][:, hf_ * 512:(hf_ + 1) * 512], op0=ALU.mult, op1=ALU.mult))
                    evd = P.sig_last("dve")
                    ringD.done(di, evd)
                    P.wait("pool", evd)
                    P.pool(lambda e, i=i, hf_=hf_, s=s: e.tensor_tensor(out=X[:, i, hf_ * 512:(hf_ + 1) * 512], in0=X[:, i, hf_ * 512:(hf_ + 1) * 512], in1=dtm[s][:, :], op=ALU.add))
                    dtm_free[s] = P.sig_last("pool")

        P.dve(lambda e: e.memset(wd_u8[:, 0:7168], 0.0))
        evz = P.sig_last("dve")
        P.wait("sp", evz)
        nwe = exp_b_gu.shape[1]
        evb = [P.dma("sp", bgall[0:nwe, :], exp_b_gu[l, :, :], "ld8b"), P.dma("sp", bdn[0:nwe, :], exp_b_down[l, :, :], "ld8b")]
        P.wait("pe", evb)
        P.wait("dve", evb)
        for c in range(16):
            P.pe(lambda e, c=c: e.transpose(out=psv(c // 4)[:, (c % 4) * 128:(c % 4 + 1) * 128], in_=bgall[:, c * 128:(c + 1) * 128], identity=ident_f[:, :]))
        evp = P.sig_last("pe")
        P.wait("dve", evp)
        for c4 in range(4):
            P.dve(lambda e, c4=c4: e.tensor_copy(out=bguT[:, 4 * c4:4 * c4 + 4, :], in_=psv(c4).rearrange("p (c x) -> p c x", c=4)[:, :, 0:NE]))
        P.dve(lambda e: e.tensor_copy(out=bdnb, in_=bdn))
        evd = P.sig_last("dve")
        P.wait("pe", evd)
        down_accum(lambda c, i: GTu[:, i, :], lambda c, hf_: bdnb[:, hf_ * 512:(hf_ + 1) * 512], 1, lambda i: None, range(ntm))
        wdn_free = P.sig_last("pe")
        act_free = None

        pre = {}

        def issue_wgu(ex_j, grp_j):
            ws_ = grp_j % 2
            P.wait("pool", wgu_free[ws_])
            e1 = P.dma_k("pool", wgu[ws_][:, :, 0:128], exp_w_gu[l, ex_j, :, grp_j * 128:(grp_j + 1) * 128].rearrange("(k p) n -> p k n", p=128), "ld8u%d_%d" % (ws_, l))
            e2 = P.dma_k("pool", wgu[ws_][:, :, 128:256], exp_w_gu[l, ex_j, :, D + grp_j * 128:D + (grp_j + 1) * 128].rearrange("(k p) n -> p k n", p=128), "ld8u%d_%d" % (ws_, l))
            return e1, e2

        for ex_i in range(moe_experts):
            evwd = None
            for grp in range(8):
                ws = grp % 2
                if (ex_i, grp) in pre:
                    evg1, evg2 = pre.pop((ex_i, grp))
                else:
                    evg1, evg2 = issue_wgu(ex_i, grp)
                if grp == 2:
                    P.wait("pool", wdn_free)
                    evwd = P.dma_k("pool", wdn[:, :, :], exp_w_down[l, ex_i].rearrange("(k p) n -> p k n", p=128), "ld8d")
                P.wait("pe", [evg1, evg2])
                ch = grp
                for (t0, tn) in tsl_m:
                    gi, gbk = ringGU.next(P)
                    ui, ubk = ringGU.next(P)
                    for k in range(8):
                        P.pe(lambda e, k=k, gbk=gbk, ws=ws, t0=t0, tn=tn: e.matmul(psv(gbk)[:, 0:tn], lhsT=wgu[ws][:, k, 0:128], rhs=h2T[:, k, t0:t0 + tn], start=(k == 0), stop=(k == 7)))
                    for k in range(8):
                        P.pe(lambda e, k=k, ubk=ubk, ws=ws, t0=t0, tn=tn: e.matmul(psv(ubk)[:, 0:tn], lhsT=wgu[ws][:, k, 128:256], rhs=h2T[:, k, t0:t0 + tn], start=(k == 0), stop=(k == 7)))
                    evp = P.sig_last("pe")
                    s = blk % 2
                    blk += 1
                    P.wait("dve", [evp, tmp_free[s], act_free])
                    P.wait("act", [evp, tmp_free[s]])
                    P.dve(lambda e, gbk=gbk, s=s, ch=ch, tn=tn, ex_i=ex_i: e.tensor_scalar(out=gcb[s][:, 0:tn], in0=psv(gbk)[:, 0:tn], scalar1=bguT[:, ch, ex_i:ex_i + 1], scalar2=7.0, op0=ALU.add, op1=ALU.min))
                    evd1 = P.sig_last("dve")
                    P.act(lambda e, ubk=ubk, s=s, ch=ch, tn=tn, ex_i=ex_i: e.activation(out=u1b[s][:, 0:tn], in_=psv(ubk)[:, 0:tn], func=AF.Identity, bias=bguT[:, 8 + ch, ex_i:ex_i + 1]))
                    P.wait("act", evd1)
                    P.act(lambda e, s=s, tn=tn: e.activation(out=gsb[s][:, 0:tn], in_=gcb[s][:, 0:tn], func=AF.Silu, scale=1.702))
                    eva = P.sig_last("act")
                    ringGU.done(gi, evd1)
                    ringGU.done(ui, eva)
                    P.wait("dve", eva)
                    P.dve(lambda e, s=s, tn=tn: e.tensor_scalar(out=u1b[s][:, 0:tn], in0=u1b[s][:, 0:tn], scalar1=-7.0, scalar2=7.0, op0=ALU.max, op1=ALU.min))
                    P.dve(lambda e, s=s, ch=ch, t0=t0, tn=tn: e.scalar_tensor_tensor(out=actT[:, ch, t0:t0 + tn], in0=u1b[s][:, 0:tn], scalar=1.0, in1=gsb[s][:, 0:tn], op0=ALU.add, op1=ALU.mult))
                    tmp_free[s] = P.sig_last("dve")
                wgu_free[ws] = P.sig_last("pe")
            act_written = P.sig_last("dve")
            if ex_i + 1 < moe_experts:
                for g_ in range(2):
                    pre[(ex_i + 1, g_)] = issue_wgu(ex_i + 1, g_)
            P.wait("pe", [act_written, evwd])
            down_accum(lambda c, i: actT[:, c, i * 128:(i + 1) * 128], lambda c, hf_: wdn[:, c, hf_ * 512:(hf_ + 1) * 512], 8,
                       lambda i, ex_i=ex_i: Gs[:, i, ex_i:ex_i + 1], range(ntm))
            wdn_free = P.sig_last("pe")
            act_free = wdn_free
        P.barrier()
        if l == 0:
            dump("x_l0", X[:, :, :].rearrange("p i d -> p (i d)"))

    return finish_prog()


def _consts():
    ident = np.eye(128, dtype=np.float32)
    t = np.arange(L)
    row = (t // 64).astype(np.float32)
    col = (t % 64).astype(np.float32)
    inv = (10000.0 ** (-np.arange(0, 32, 2, dtype=np.float32) / 32)).astype(np.float32)
    ang = np.concatenate([row[:, None] * inv, row[:, None] * inv, col[:, None] * inv, col[:, None] * inv], axis=-1).astype(np.float32)
    cos = np.cos(ang).astype(np.float32)
    sin = np.sin(ang).astype(np.float32)
    sgn = np.concatenate([-np.ones(16), np.ones(16), -np.ones(16), np.ones(16)]).astype(np.float32)
    rope = np.concatenate([cos, sin * sgn[None, :]], axis=-1).reshape(NXT, 128, 128).transpose(1, 0, 2)
    j = np.arange(128)[:, None]
    i = np.arange(128)[None, :]
    mask = np.stack([(i <= j), (j <= i)], axis=1).astype(np.float32)
    fix = np.ones((128, 2, 2, 8), np.float32)
    pinv = np.zeros((128, 2), np.float32)
    for p in range(128):
        for ct in range(2):
            w = POOLW[2 * ct + p // 64]
            pinv[p, ct] = 1.0 / w
            for tt in range(w // 2):
                fix[p, ct, 0, tt] = 1.0 / (tt + w // 2)
                fix[p, ct, 1, tt] = 1.0 / (w - tt)
    return {"c_ident": ident, "c_rope": np.ascontiguousarray(rope, dtype=np.float32), "c_mask": np.ascontiguousarray(mask),
            "c_poolfix": fix, "c_poolinv": pinv}


WEIGHT_KEYS = ["w_mod", "b_mod", "norm1_w", "norm2_w", "w_in", "q_norm_w", "k_norm_w", "attn_sink", "ssm_a_re", "ssm_a_im",
               "ssm_log_dt", "ssm_b_re", "ssm_b_im", "ssm_c_re", "ssm_c_im", "ssm_d", "glu_w", "glu_b", "pool_w", "pool_scale",
               "out_norm_w", "w_out", "router_w", "router_b", "exp_w_gu", "exp_b_gu", "exp_w_down", "exp_b_down"]


def make_in_map(inputs, b, consts, wd=DEPTH, we=NE):
    m = {k: np.ascontiguousarray(np.asarray(inputs[k][:wd, :we] if k.startswith('exp_') else inputs[k][:wd], dtype=np.float32)) for k in WEIGHT_KEYS}
    m["x"] = np.ascontiguousarray(np.asarray(inputs["x"][b], dtype=np.float32))
    m["ctx"] = np.ascontiguousarray(np.asarray(inputs["ctx"][b], dtype=np.float32))
    m["cc"] = np.ascontiguousarray(np.stack([np.asarray(inputs["c"][b]), np.asarray(inputs["c_ctx"])]).astype(np.float32))
    m.update(consts)
    return m


def kernel(**inputs):
    nc = build_program()
    consts = _consts()
    in_maps = [make_in_map(inputs, b, consts) for b in range(8)]
    res = run_bass_kernel_spmd(nc, in_maps, core_ids=list(range(8)))
    return np.stack([np.asarray(r["out"], dtype=np.float32) for r in res.results], axis=0)
```
